# Optimizing a Trainium2 kernel written in Bass

```python
import jax, jax.numpy as jnp
from jax import lax
import numpy as np

D_MODEL = 2048
BATCH = 2
SEQ = 4096
DEPTH = 1

CHUNK = 64
QBLOCK = 128
D_PLE = 256
HEAD_DIM = 128
N_HEADS_FOX = 8
N_HEADS_SB = 8
W_FOX = N_HEADS_FOX * HEAD_DIM
W_SB = N_HEADS_SB * HEAD_DIM
N_GROUPS = 4
EXPERTS_PER_GROUP = 8
N_EXPERTS = N_GROUPS * EXPERTS_PER_GROUP
TOP_K_IN_GROUP = 2
D_EXPERT = 512
MOE_BLOCK = 128
EPS = 1e-6
D_IN = 3 * W_FOX + N_HEADS_FOX + 3 * W_SB + 2 * D_MODEL

kernel_name = 'hybrid_fox_stickbreaking_hiermoe_block'


def rmsnorm(x, g):
    xf = x.astype(jnp.float32)
    y = xf * lax.rsqrt(jnp.mean(xf * xf, axis=-1, keepdims=True) + EPS)
    return (y * g.astype(jnp.float32)).astype(x.dtype)


def to_blocks(t):
    b, s, h, d = t.shape
    return t.reshape(b, s // QBLOCK, QBLOCK, h, d).transpose(1, 0, 3, 2, 4)


def from_blocks(o):
    nb, b, h, qb, d = o.shape
    return o.transpose(1, 0, 3, 2, 4).reshape(b, nb * qb, h * d)


def forgetting_attention(q, k, v, log_f):
    b, s, h, d = q.shape
    nb = s // QBLOCK
    c = jnp.cumsum(log_f, axis=1).transpose(0, 2, 1)
    c_blocks = c.reshape(b, h, nb, QBLOCK).transpose(2, 0, 1, 3)
    kh = k.transpose(0, 2, 1, 3)
    vh = v.transpose(0, 2, 1, 3)
    key_pos = jnp.arange(s)
    scale = d ** -0.5

    def block(args):
        qb, cqb, i = args
        logits = jnp.einsum('bhqd,bhkd->bhqk', qb, kh).astype(jnp.float32) * scale
        logits = logits + cqb[..., :, None] - c[:, :, None, :]
        qpos = i * QBLOCK + jnp.arange(QBLOCK)
        mask = key_pos[None, :] <= qpos[:, None]
        probs = jax.nn.softmax(jnp.where(mask, logits, -jnp.inf), axis=-1)
        return jnp.einsum('bhqk,bhkd->bhqd', probs.astype(vh.dtype), vh)

    o = lax.map(block, (to_blocks(q), c_blocks, jnp.arange(nb)))
    return from_blocks(o)


def stick_breaking_attention(q, k, v):
    b, s, h, d = q.shape
    nb = s // QBLOCK
    kh = k.transpose(0, 2, 1, 3)
    vh = v.transpose(0, 2, 1, 3)
    key_pos = jnp.arange(s)
    scale = d ** -0.5

    def block(args):
        qb, i = args
        z = jnp.einsum('bhqd,bhkd->bhqk', qb, kh).astype(jnp.float32) * scale
        qpos = i * QBLOCK + jnp.arange(QBLOCK)
        mask = key_pos[None, :] < qpos[:, None]
        neg = jnp.where(mask, jax.nn.log_sigmoid(-z), 0.0)
        after = lax.cumsum(neg, axis=3, reverse=True) - neg
        a = jnp.where(mask, jnp.exp(jax.nn.log_sigmoid(z) + after), 0.0)
        return jnp.einsum('bhqk,bhkd->bhqd', a.astype(vh.dtype), vh)

    o = lax.map(block, (to_blocks(q), jnp.arange(nb)))
    return from_blocks(o)


def hierarchical_moe(h, w_group, w_expert, w_gate, w_up, w_down):
    b, s, d = h.shape
    t = b * s
    xt = h.reshape(t, d)
    g_logits = (xt @ w_group).astype(jnp.float32)
    g_prob = jax.nn.softmax(g_logits, axis=-1)
    g_sel = jnp.argmax(g_logits, axis=-1).astype(jnp.int32)
    p_group = jnp.take_along_axis(g_prob, g_sel[:, None], axis=1)
    e_logits = (xt @ w_expert).astype(jnp.float32).reshape(t, N_GROUPS, EXPERTS_PER_GROUP)
    e_logits = jnp.take_along_axis(e_logits, g_sel[:, None, None], axis=1)[:, 0]
    top_val, top_idx = lax.top_k(e_logits, TOP_K_IN_GROUP)
    gate = jax.nn.softmax(top_val, axis=-1) * p_group
    expert_id = g_sel[:, None] * EXPERTS_PER_GROUP + top_idx.astype(jnp.int32)

    m = t * TOP_K_IN_GROUP
    flat_e = expert_id.reshape(m)
    flat_tok = jnp.repeat(jnp.arange(t, dtype=jnp.int32), TOP_K_IN_GROUP)
    flat_w = gate.reshape(m)
    order = jnp.argsort(flat_e)
    se = flat_e[order]
    stok = flat_tok[order]
    sw = flat_w[order]
    counts = jnp.bincount(flat_e, length=N_EXPERTS)
    padded = (counts + MOE_BLOCK - 1) // MOE_BLOCK * MOE_BLOCK
    pad_end = jnp.cumsum(padded)
    pad_start = pad_end - padded
    start = jnp.cumsum(counts) - counts
    dest = pad_start[se] + jnp.arange(m) - start[se]
    n_blocks = m // MOE_BLOCK + N_EXPERTS
    p_rows = n_blocks * MOE_BLOCK
    row_tok = jnp.full((p_rows,), t, dtype=jnp.int32).at[dest].set(stok)
    row_w = jnp.zeros((p_rows,), dtype=gate.dtype).at[dest].set(sw)
    block_expert = jnp.minimum(
        jnp.searchsorted(pad_end, jnp.arange(n_blocks) * MOE_BLOCK, side='right'),
        N_EXPERTS - 1).astype(jnp.int32)
    x_pad = jnp.concatenate([xt, jnp.zeros((1, d), xt.dtype)], axis=0)
    xb = x_pad[row_tok].reshape(n_blocks, MOE_BLOCK, d)

    def expert_block(args):
        xe, e = args
        hid = jax.nn.silu(xe @ w_gate[e]) * (xe @ w_up[e])
        return hid @ w_down[e]

    yb = lax.map(expert_block, (xb, block_expert))
    y = jax.ops.segment_sum(yb.reshape(p_rows, d) * row_w[:, None].astype(yb.dtype),
                            row_tok, num_segments=t + 1)[:t]
    return y.reshape(b, s, d)


def setup_inputs(seed: int = 0) -> dict:
    key = jax.random.key(seed)
    ks = jax.random.split(key, 20)
    f32 = jnp.float32

    def nrm(k, shape, fan_in):
        return jax.random.normal(k, shape, f32) * (fan_in ** -0.5)

    def gain(k, shape):
        return 1.0 + 0.02 * jax.random.normal(k, shape, f32)

    return {
        'x': jax.random.normal(ks[0], (BATCH, SEQ, D_MODEL), f32),
        'p': jax.random.normal(ks[1], (DEPTH, BATCH, SEQ, D_PLE), f32),
        'w_in': nrm(ks[2], (DEPTH, D_MODEL, D_IN), D_MODEL),
        'b_forget': 1.0 + 3.0 * jax.random.uniform(ks[3], (DEPTH, N_HEADS_FOX), f32),
        'w_branch_fox': nrm(ks[4], (DEPTH, W_FOX, D_MODEL), W_FOX),
        'w_branch_sb': nrm(ks[5], (DEPTH, W_SB, D_MODEL), W_SB),
        'w_mix_out': nrm(ks[6], (DEPTH, D_MODEL, D_MODEL), D_MODEL),
        'g_mix': gain(ks[7], (DEPTH, D_MODEL)),
        'g_ffn': gain(ks[8], (DEPTH, D_MODEL)),
        'w_group': nrm(ks[9], (DEPTH, D_MODEL, N_GROUPS), D_MODEL),
        'w_expert': nrm(ks[10], (DEPTH, D_MODEL, N_EXPERTS), D_MODEL),
        'w_gate': nrm(ks[11], (DEPTH, N_EXPERTS, D_MODEL, D_EXPERT), D_MODEL),
        'w_up': nrm(ks[12], (DEPTH, N_EXPERTS, D_MODEL, D_EXPERT), D_MODEL),
        'w_down': nrm(ks[13], (DEPTH, N_EXPERTS, D_EXPERT, D_MODEL), D_EXPERT),
        'g_ple': gain(ks[14], (DEPTH, D_MODEL)),
        'w_ple_proj': nrm(ks[15], (DEPTH, D_PLE, D_MODEL), D_PLE),
        'w_ple_gate': nrm(ks[16], (DEPTH, D_MODEL, D_MODEL), D_MODEL),
        'g_final': gain(ks[17], (D_MODEL,)),
    }


def reference(x, p, w_in, b_forget, w_branch_fox, w_branch_sb, w_mix_out, g_mix, g_ffn,
              w_group, w_expert, w_gate, w_up, w_down, g_ple, w_ple_proj, w_ple_gate, g_final):
    b, s, _ = x.shape
    sizes = [W_FOX, W_FOX, W_FOX, N_HEADS_FOX, W_SB, W_SB, W_SB, D_MODEL]
    splits = [int(v) for v in np.cumsum(sizes)]
    for i in range(DEPTH):
        h = rmsnorm(x, g_mix[i])
        proj = h @ w_in[i]
        q_a, k_a, v_a, f_a, q_b, k_b, v_b, gate_a, gate_b = jnp.split(proj, splits, axis=-1)
        heads_a = (b, s, N_HEADS_FOX, HEAD_DIM)
        heads_b = (b, s, N_HEADS_SB, HEAD_DIM)
        log_f = jax.nn.log_sigmoid(f_a.astype(jnp.float32) + b_forget[i].astype(jnp.float32))
        o_a = forgetting_attention(q_a.reshape(heads_a), k_a.reshape(heads_a),
                                   v_a.reshape(heads_a), log_f)
        o_b = stick_breaking_attention(q_b.reshape(heads_b), k_b.reshape(heads_b),
                                       v_b.reshape(heads_b))
        mixed = (jax.nn.sigmoid(gate_a) * (o_a @ w_branch_fox[i])
                 + jax.nn.sigmoid(gate_b) * (o_b @ w_branch_sb[i]))
        x = x + mixed @ w_mix_out[i]
        x = x + hierarchical_moe(rmsnorm(x, g_ffn[i]), w_group[i], w_expert[i],
                                 w_gate[i], w_up[i], w_down[i])
        ple = p[i] @ w_ple_proj[i]
        x = x + jax.nn.sigmoid(rmsnorm(x, g_ple[i]) @ w_ple_gate[i]) * ple
    return rmsnorm(x, g_final)
```

```python
import numpy as np
from contextlib import ExitStack
import concourse.bass as bass
import concourse.mybir as mybir
from concourse.bass_utils import run_bass_kernel_spmd

F32 = mybir.dt.float32
BF16 = mybir.dt.bfloat16
I32 = mybir.dt.int32
U32 = mybir.dt.uint32
U8 = mybir.dt.uint8
AF = mybir.ActivationFunctionType
ALU = mybir.AluOpType
AX = mybir.AxisListType

D = 2048
S = 4096
NB = 32
NSLOT = 8
TOWN = 1024
DIN = 10248
NE = 32
DEXP = 512
NBLK = 48
EPS = 1e-6
SCALE = 128 ** -0.5
NK = [4, 8, 12, 16, 20, 24, 28, 32]


class Tl:
    __slots__ = ("name", "writers", "readers", "sem", "dma_cnt")

    def __init__(self, name):
        self.name = name
        self.writers = []
        self.readers = []
        self.sem = {}
        self.dma_cnt = {}


class Op:
    __slots__ = ("eng", "emit", "deps", "marked", "semval", "idx", "dma")

    def __init__(self, eng, emit):
        self.eng = eng
        self.emit = emit
        self.deps = []
        self.marked = False
        self.semval = 0
        self.idx = 0
        self.dma = None


class Prog:
    ENGS = ("pe", "act", "dve", "pool", "sp")

    def __init__(self, nc, stack):
        self.nc = nc
        self.stack = stack
        self.ops = {e: [] for e in self.ENGS}
        self.nops = 0
        self.esem = {e: stack.enter_context(nc.semaphore("es_" + e)) for e in self.ENGS}
        self.dma_tokens = []
        self.out_tokens = []
        self.nsem = 0

    def tile(self, name):
        return Tl(name)

    def _tsem(self, t, kind):
        if kind not in t.sem:
            t.sem[kind] = self.stack.enter_context(self.nc.semaphore("ds_%d" % self.nsem))
            t.dma_cnt[kind] = 0
            self.nsem += 1
        return t.sem[kind]

    def I(self, eng, fname, *args, reads=(), writes=(), **kw):
        return self.op(eng, (fname, args, kw), reads=reads, writes=writes)

    def DMA(self, eng, out, in_, reads, wt, output=False, sem_tile=None, **kw):
        return self.op(eng, ("dma_start", (), dict(out=out, in_=in_, **kw)), reads=reads, writes=[wt], dma_out=wt, is_output=output,
                       sem_tile=sem_tile)

    def op(self, eng, emit, reads=(), writes=(), dma_out=None, is_output=False, sem_tile=None):
        o = Op(eng, emit)
        o.idx = self.nops
        self.nops += 1
        deps = []
        for t in reads:
            deps.extend(t.writers)
        for t in writes:
            deps.extend(t.readers)
            if dma_out is not None:
                deps.extend(tk for tk in t.writers if tk[0] != "dma")
            else:
                deps.extend(t.writers)
        o.deps = deps
        if dma_out is not None:
            kind = "sw" if eng == "pool" else "hw"
            st = sem_tile if sem_tile is not None else dma_out
            sem = self._tsem(st, kind)
            st.dma_cnt[kind] += 16
            tok = ("dma", sem, st.dma_cnt[kind])
            o.dma = sem
            self.dma_tokens.append(tok)
            if is_output:
                self.out_tokens.append(tok)
        else:
            tok = ("op", o)
        for t in reads:
            t.readers.append(tok)
        for t in writes:
            if t.readers:
                t.writers = [tok]
                t.readers = []
            else:
                t.writers.append(tok)
                if len(t.writers) > 64:
                    t.writers = self._compact(t.writers)
        for t in reads:
            if len(t.readers) > 64:
                t.readers = self._compact(t.readers)
        self.ops[eng].append(o)
        return o

    @staticmethod
    def _compact(toks):
        best = {}
        for tk in toks:
            if tk[0] == "op":
                k = ("op", tk[1].eng)
                if k not in best or best[k][1].idx < tk[1].idx:
                    best[k] = tk
            else:
                k = ("dma", id(tk[1]))
                if k not in best or best[k][2] < tk[2]:
                    best[k] = tk
        return list(best.values())

    def barrier(self):
        toks = list(self.dma_tokens)
        for e in self.ENGS:
            if self.ops[e]:
                last = self.ops[e][-1]
                if last.dma is None and (last.emit is not None or last.deps):
                    toks.append(("op", last))
        self.dma_tokens = []
        for e in self.ENGS:
            o = Op(e, None)
            o.idx = self.nops
            self.nops += 1
            o.deps = [t for t in toks if not (t[0] == "op" and t[1].eng == e and e in ("pe", "sp"))]
            self.ops[e].append(o)

    def finish(self):
        o = Op("sp", None)
        o.deps = list(self.out_tokens)
        self.ops["sp"].append(o)
        for e in self.ENGS:
            for o in self.ops[e]:
                for tk in o.deps:
                    if tk[0] == "op":
                        p = tk[1]
                        if p.eng == "pe" and e == "pe":
                            continue
                        p.marked = True
        for e in self.ENGS:
            c = 0
            for o in self.ops[e]:
                if o.marked:
                    c += 1
                    o.semval = c
        nc = self.nc
        hmap = {"pe": nc.tensor, "act": nc.scalar, "dve": nc.vector, "pool": nc.gpsimd, "sp": nc.sync}
        with nc.Block() as block:
            def run(e, h):
                waited = {}
                regcache = {}
                for o in self.ops[e]:
                    need = {}
                    for tk in o.deps:
                        if tk[0] == "op":
                            p = tk[1]
                            if p.eng == "pe" and e == "pe":
                                continue
                            sem = self.esem[p.eng]
                            val = p.semval
                        else:
                            sem = tk[1]
                            val = tk[2]
                        k = id(sem)
                        if waited.get(k, (None, 0))[1] >= val:
                            continue
                        if k not in need or need[k][1] < val:
                            need[k] = (sem, val)
                    for k, (sem, val) in need.items():
                        h.wait_ge(sem, val)
                        waited[k] = (sem, val)
                    ins = None
                    if o.emit is not None:
                        fname, args, kw = o.emit
                        if "bounds_check" in kw and isinstance(kw["bounds_check"], int):
                            bv = kw["bounds_check"]
                            if bv not in regcache:
                                regcache[bv] = h.to_reg(bv)
                            kw = dict(kw, bounds_check=regcache[bv])
                        try:
                            ins = getattr(h, fname)(*args, **kw)
                        except Exception:
                            print("EMIT FAIL", e, fname, {k: (getattr(v, "shape", v)) for k, v in kw.items()})
                            raise
                    elif o.marked:
                        ins = h.nop()
                    if ins is not None:
                        if o.dma is not None:
                            ins.then_inc(o.dma, 16)
                        elif o.marked:
                            ins.then_inc(self.esem[e], 1)

            @block.tensor
            def _(h):
                run("pe", h)

            @block.scalar
            def _(h):
                run("act", h)

            @block.vector
            def _(h):
                run("dve", h)

            @block.gpsimd
            def _(h):
                run("pool", h)

            @block.sync
            def _(h):
                run("sp", h)


class Arena:
    def __init__(self, nc, nbytes):
        self.big = nc.alloc_sbuf_tensor("arena", [128, nbytes], U8)
        self.nbytes = nbytes
        self.off = 0

    def mark(self):
        return self.off

    def reset(self, off):
        self.off = off

    def alloc(self, shape_free, dtype, nparts=128):
        esz = {F32: 4, BF16: 2, I32: 4, U32: 4}[dtype]
        n = 1
        for s in shape_free:
            n *= s
        nb = (n * esz + 63) // 64 * 64
        assert self.off + nb <= self.nbytes, ("sbuf overflow", self.off, nb)
        ap = self.big[:, self.off:self.off + n * esz].bitcast(dtype)
        self.off += nb
        if len(shape_free) == 2:
            ap = ap.rearrange("p (a b) -> p a b", a=shape_free[0])
        elif len(shape_free) == 3:
            ap = ap.rearrange("p (a b c) -> p a b c", a=shape_free[0], b=shape_free[1])
        return ap


def build_nc(stage=99):
    nc = bass.Bass("TRN2", target_bir_lowering=False)
    dt = nc.dram_tensor
    xall = dt("xall", [S, D], F32, kind="ExternalInput").ap()
    xown = dt("xown", [TOWN, D], F32, kind="ExternalInput").ap()
    blk128 = dt("blk128", [128, NSLOT], F32, kind="ExternalInput").ap()
    w_in = dt("w_in", [D, DIN], F32, kind="ExternalInput").ap()
    b_forget = dt("b_forget", [1, 8], F32, kind="ExternalInput").ap()
    g_mix = dt("g_mix", [1, D], F32, kind="ExternalInput").ap()
    if stage >= 3:
        w_bfox = dt("w_branch_fox", [1024, D], F32, kind="ExternalInput").ap()
        w_bsb = dt("w_branch_sb", [1024, D], F32, kind="ExternalInput").ap()
        w_mix = dt("w_mix_out", [D, D], F32, kind="ExternalInput").ap()
    if stage >= 4:
        g_ffn = dt("g_ffn", [1, D], F32, kind="ExternalInput").ap()
        w_rout = dt("w_rout", [D, 36], F32, kind="ExternalInput").ap()
        w_gate = dt("w_gate", [NE * 512, 2048], F32, kind="ExternalInput").ap()
        w_up = dt("w_up", [NE * 512, 2048], F32, kind="ExternalInput").ap()
        w_down = dt("w_down", [NE * 512, 2048], F32, kind="ExternalInput").ap()
    if stage >= 5:
        pown = dt("pown", [TOWN, 256], F32, kind="ExternalInput").ap()
        g_ple = dt("g_ple", [1, D], F32, kind="ExternalInput").ap()
        w_pp = dt("w_ple_proj", [256, D], F32, kind="ExternalInput").ap()
        w_pg = dt("w_ple_gate", [D, D], F32, kind="ExternalInput").ap()
        g_final = dt("g_final", [1, D], F32, kind="ExternalInput").ap()
    y = dt("y", [TOWN, D], F32, kind="ExternalOutput").ap()

    hT_d = dt("hT_d", [16, 128, S], BF16).ap()
    kaT_d = dt("kaT_d", [8, 128, S], BF16).ap()
    kbT_d = dt("kbT_d", [8, 128, S], BF16).ap()
    va_d = dt("va_d", [S, 1024], BF16).ap()
    vb_d = dt("vb_d", [S, 1024], BF16).ap()
    x1_d = dt("x1_d", [TOWN, D], F32).ap()
    xb_d = dt("xb_d", [NBLK * 128, D], BF16).ap()
    yb_d = dt("yb_d", [NBLK * 128, D], F32).ap()
    wb_d = [dt("wb%d_d" % i, [NE * 512, 2048], BF16).ap() for i in range(3)]

    stack = ExitStack()
    with stack:
        P = Prog(nc, stack)
        A = Arena(nc, 207 * 1024)
        ps = [nc.alloc_psum_tensor("ps%d" % i, [128, 512], F32) for i in range(8)]
        pst = [P.tile("ps%d" % i) for i in range(8)]

        def psf(i):
            return ps[i][:]

        def psb(i):
            return ps[i][:].bitcast(BF16)

        def bcast_row(ap1d):
            return ap1d.partition_broadcast(128).rearrange("p a n -> p (a n)")

        c_dmat = A.alloc([128], F32)
        c_identb = A.alloc([128], BF16)
        c_identf = A.alloc([128], F32)
        c_trif = A.alloc([128], F32)
        c_strif = A.alloc([128], F32)
        c_onesf = A.alloc([128], F32)
        c_ntri = A.alloc([128], BF16)
        c_nones = A.alloc([128], BF16)
        c_ones = A.alloc([128], BF16)
        c_j128 = A.alloc([NBLK], F32)
        c_pidx = A.alloc([1], F32)
        c_blk = A.alloc([NSLOT], F32)
        c_onehot = A.alloc([NSLOT, NB], F32)
        c_bf = A.alloc([8], F32)
        c_eps = A.alloc([1], F32)
        core_mark = A.mark()
        m_le = A.alloc([32, 128], BF16)
        m_lt = A.alloc([32, 128], BF16)
        t_const = P.tile("const")
        TC = [t_const]
        P.I("pool", "iota", c_dmat, [[1, 128]], base=0, channel_multiplier=-1, allow_small_or_imprecise_dtypes=True, writes=TC)
        P.I("pool", "iota", c_j128, [[128, NBLK]], base=0, channel_multiplier=0, allow_small_or_imprecise_dtypes=True, writes=TC)
        P.I("pool", "iota", c_pidx, [[0, 1]], base=0, channel_multiplier=1, allow_small_or_imprecise_dtypes=True, writes=TC)
        P.DMA("sp", c_blk, blk128, [], t_const)
        P.DMA("sp", c_bf, bcast_row(b_forget), [], t_const)

        def dv(fname, *args, reads=(), writes=(), **kw):
            return P.I("dve", fname, *args, reads=reads, writes=writes, **kw)

        def act(fname, *args, reads=(), writes=(), **kw):
            return P.I("act", fname, *args, reads=reads, writes=writes, **kw)

        def pe(fname, *args, reads=(), writes=(), **kw):
            return P.I("pe", fname, *args, reads=reads, writes=writes, **kw)

        dv("tensor_single_scalar", c_identb, c_dmat, 0.0, ALU.is_equal, reads=TC, writes=TC)
        dv("tensor_single_scalar", c_identf, c_dmat, 0.0, ALU.is_equal, reads=TC, writes=TC)
        dv("tensor_single_scalar", c_trif, c_dmat, 0.0, ALU.is_ge, reads=TC, writes=TC)
        dv("tensor_single_scalar", c_strif, c_dmat, 0.0, ALU.is_gt, reads=TC, writes=TC)
        dv("memset", c_onesf, 1.0, writes=TC)
        dv("memset", c_ones, 1.0, writes=TC)
        dv("memset", c_nones, -1.0, writes=TC)
        dv("memset", c_eps, EPS, writes=TC)
        dv("tensor_scalar", c_ntri, c_dmat, 0.0, -1.0, ALU.is_le, ALU.mult, reads=TC, writes=TC)
        for k in range(NSLOT):
            dv("tensor_scalar", c_onehot[:, k, :], c_j128[:, 0:NB], c_blk[:, k:k + 1], None, ALU.is_equal, reads=TC, writes=TC)
        for g in range(NSLOT):
            for j in range(4):
                kb = 4 * g + j
                dv("tensor_scalar", m_le[:, 4 * g + j, :], c_dmat, c_blk[:, g:g + 1], 128.0 * kb, ALU.add, ALU.is_ge, reads=TC, writes=TC)
                dv("tensor_scalar", m_lt[:, 4 * g + j, :], c_dmat, c_blk[:, g:g + 1], 128.0 * kb + 1.0, ALU.add, ALU.is_ge, reads=TC, writes=TC)
        if stage == 0:
            return finish_debug(nc, P, dt, [("m_le", m_le, [128, 32, 128], BF16), ("onehot", c_onehot, [128, NSLOT, NB], F32),
                                            ("ntri", c_ntri, [128, 128], BF16), ("bf", c_bf, [128, 8], F32)], TC)
        t_xbd = P.tile("xb_d")
        t_xbz = P.tile("xb_z")

        gbc = A.alloc([D], F32)
        t_gbc = P.tile("gbc")
        P.DMA("sp", gbc, bcast_row(g_mix), [], t_gbc)
        cv_buf = [A.alloc([2048], BF16) for _ in range(3)]
        cv_t = [P.tile("cv%d" % i) for i in range(3)]
        zero_b = cv_buf[0]
        dv("memset", zero_b, 0.0, writes=TC)
        t_wb = P.tile("wb_d")
        cv_pieces = [(mat, e, m) for e in range(NE) for mat in range(3) for m in range(4)]
        cv_state = {"l": 0, "s": 0}
        cv_on = stage >= 4

        def conv_store():
            p_ = cv_state["s"]
            mat, e, m = cv_pieces[p_]
            r0 = e * 512 + m * 128
            P.DMA("sp", wb_d[mat][r0:r0 + 128, :], cv_buf[p_ % 3], [cv_t[p_ % 3]], t_wb, sem_tile=cv_t[p_ % 3])
            cv_state["s"] += 1

        def conv_step(k=1):
            if not cv_on:
                return
            wsrc = (w_gate, w_up, w_down)
            for _ in range(k):
                if cv_state["l"] >= len(cv_pieces):
                    if cv_state["s"] < cv_state["l"]:
                        conv_store()
                    continue
                if cv_state["l"] - cv_state["s"] >= 2:
                    conv_store()
                p_ = cv_state["l"]
                mat, e, m = cv_pieces[p_]
                r0 = e * 512 + m * 128
                P.DMA("pool", cv_buf[p_ % 3], wsrc[mat][r0:r0 + 128, :], [], cv_t[p_ % 3])
                cv_state["l"] += 1

        def conv_flush():
            if not cv_on:
                return
            while cv_state["l"] < len(cv_pieces):
                conv_step(1)
            while cv_state["s"] < cv_state["l"]:
                conv_store()

        c_all = A.alloc([8, NB], F32)
        cbias = A.alloc([8, NSLOT], F32)
        t_c = P.tile("c")
        persist_mark = A.mark()

        def norm_rstd(xst, xst_t, junk, junk_t, ss, ss_t):
            act("activation", out=junk, in_=xst, func=AF.Square, accum_out=ss[:, 0:1], reads=[xst_t], writes=[junk_t, ss_t])
            act("activation", out=ss[:, 1:2], in_=ss[:, 0:1], func=AF.Sqrt, bias=c_eps[:, 0:1], scale=1.0 / D,
                reads=[ss_t, t_const], writes=[ss_t])
            dv("reciprocal", ss[:, 2:3], ss[:, 1:2], reads=[ss_t], writes=[ss_t])

        def transpose16(src_bf, src_t, dst, dst_t, col0, banks, stride=1, evac=("act", "dve")):
            for half in range(2):
                b = banks[half]
                for q in range(8):
                    kc = half * 8 + q
                    pe("transpose", psb(b)[:, q * 128:(q + 1) * 128], src_bf[:, kc * 128:(kc + 1) * 128], c_identb,
                       reads=[src_t, t_const], writes=[pst[b]])
                o_ = dst[:, half * 8:(half + 1) * 8, col0:col0 + 128]
                i_ = psb(b).rearrange("p (a b) -> p a b", a=8)
                if evac[half] == "act":
                    act("activation", out=o_, in_=i_, func=AF.Copy, reads=[pst[b]], writes=[dst_t])
                else:
                    dv("tensor_copy", out=o_, in_=i_, reads=[pst[b]], writes=[dst_t])

        def norm_transpose(x_src_ap, gb, gb_t, xst, xst_t, xn, xn_t, ss, ss_t, hT_dst, hT_t, col0, banks):
            P.DMA("sp", xst, x_src_ap, [], xst_t)
            norm_rstd(xst, xst_t, xn, xn_t, ss, ss_t)
            dv("scalar_tensor_tensor", out=xn, in0=xst, scalar=ss[:, 2:3], in1=gb, op0=ALU.mult, op1=ALU.mult,
               reads=[xst_t, ss_t, gb_t], writes=[xn_t])
            transpose16(xn, xn_t, hT_dst, hT_t, col0, banks)

        kv_tiles = []
        t_hd = P.tile("hd")
        for pss in range(2):
            A.reset(persist_mark)
            W = A.alloc([16, 2048], BF16)
            t_W = P.tile("W")
            c0 = 1024 if pss == 0 else 4104
            for kc4 in range(4):
                P.DMA("pool", W[:, kc4 * 4:(kc4 + 1) * 4, :],
                      w_in[kc4 * 512:(kc4 + 1) * 512, c0:c0 + 2048].rearrange("(kc p) n -> p kc n", p=128), [], t_W)
            if pss == 0:
                Wf32 = A.alloc([16, 8], F32)
                Wf = A.alloc([16, 8], BF16)
                fl_all = A.alloc([NB * 8], F32)
                t_fl = P.tile("fl")
                t_Wf = P.tile("Wf")
                P.DMA("sp", Wf32, w_in[:, 3072:3080].rearrange("(kc p) n -> p kc n", p=128), [], t_Wf)
                dv("tensor_copy", out=Wf, in_=Wf32, reads=[t_Wf], writes=[t_Wf])
            hT = [A.alloc([16, 512], BF16) for _ in range(2)]
            hT_t = [P.tile("hT%d" % i) for i in range(2)]
            kst = [A.alloc([512], BF16) for _ in range(4)]
            kst_t = [P.tile("kst%d" % i) for i in range(4)]
            vst = [A.alloc([1024], BF16) for _ in range(3)]
            vst_t = [P.tile("vst%d" % i) for i in range(3)]
            kT_d = kaT_d if pss == 0 else kbT_d
            v_d = va_d if pss == 0 else vb_d
            t_kd = P.tile("kd%d" % pss)
            t_vd = P.tile("vd%d" % pss)
            kv_tiles.extend([t_kd, t_vd])
            xn4 = [A.alloc([D], BF16) for _ in range(4)] if pss == 0 else None
            xn4_t = [P.tile("xn4_%d" % i) for i in range(4)]
            ss4 = [A.alloc([4], F32) for _ in range(4)] if pss == 0 else None
            ss4_t = [P.tile("ss4_%d" % i) for i in range(4)]

            xst4 = [A.alloc([D], F32) for _ in range(4)] if pss == 0 else None
            xst4_t = [P.tile("xst4_%d" % i) for i in range(4)]

            def load_group(G):
                for tt in range(4):
                    r0 = (G * 4 + tt) * 128
                    P.DMA("sp", xst4[tt], xall[r0:r0 + 128, :], [], xst4_t[tt])

            def norm_tile(G, tt):
                norm_rstd(xst4[tt], xst4_t[tt], xn4[tt], xn4_t[tt], ss4[tt], ss4_t[tt])
                dv("scalar_tensor_tensor", out=xn4[tt], in0=xst4[tt], scalar=ss4[tt][:, 2:3], in1=gbc, op0=ALU.mult, op1=ALU.mult,
                   reads=[xst4_t[tt], ss4_t[tt], t_gbc], writes=[xn4_t[tt]])

            def norm_group(G):
                load_group(G)
                for tt in range(4):
                    norm_tile(G, tt)

            def transpose_group(G):
                for tt in range(4):
                    transpose16(xn4[tt], xn4_t[tt], hT[G % 2], hT_t[G % 2], tt * 128, (0, 1))

            if pss == 0:
                norm_group(0)
                transpose_group(0)
            for G in range(8):
                hb = hT[G % 2]
                hbt = hT_t[G % 2]
                if pss == 0:
                    if G + 1 < 8:
                        load_group(G + 1)
                    P.DMA("sp", hT_d[:, :, G * 512:(G + 1) * 512].rearrange("kc p t -> p kc t"), hb, [hbt], t_hd, sem_tile=hbt)
                else:
                    P.DMA("sp", hb, hT_d[:, :, G * 512:(G + 1) * 512].rearrange("kc p t -> p kc t"), [t_hd], hbt)
                for hd in range(8):
                    b = (2, 3, 7)[(G * 8 + hd) % 3]
                    for kc in range(16):
                        pe("matmul", psf(b), lhsT=W[:, kc, hd * 128:(hd + 1) * 128], rhs=hb[:, kc, :], start=(kc == 0), stop=(kc == 15),
                           reads=[t_W, hbt], writes=[pst[b]])
                    ks = kst[(G * 8 + hd) % 4]
                    kst_ = kst_t[(G * 8 + hd) % 4]
                    dv("tensor_copy", out=ks, in_=psf(b), reads=[pst[b]], writes=[kst_])
                    P.DMA("sp", kT_d[hd, :, G * 512:(G + 1) * 512], ks, [kst_], t_kd, sem_tile=kst_)
                    if pss == 1:
                        conv_step(1)
                    if pss == 0 and G + 1 < 8 and hd % 2 == 1:
                        norm_tile(G + 1, hd // 2)
                if pss == 0 and G + 1 < 8:
                    transpose_group(G + 1)
                if pss == 0 and G == 1 and stage >= 4:
                    for blk in range(NBLK):
                        P.DMA("sp", xb_d[blk * 128:(blk + 1) * 128, :], zero_b, TC, t_xbz)
                for tt in range(4):
                    vs = vst[(G * 4 + tt) % 3]
                    vs_t = vst_t[(G * 4 + tt) % 3]
                    for cg in range(2):
                        b = 4 + cg
                        for kc in range(16):
                            pe("matmul", psf(b), lhsT=hb[:, kc, tt * 128:(tt + 1) * 128], rhs=W[:, kc, 1024 + cg * 512:1024 + (cg + 1) * 512],
                               start=(kc == 0), stop=(kc == 15), reads=[t_W, hbt], writes=[pst[b]])
                        act("activation", out=vs[:, cg * 512:(cg + 1) * 512], in_=psf(b), func=AF.Copy, reads=[pst[b]], writes=[vs_t])
                    r0 = (G * 4 + tt) * 128
                    P.DMA("sp", v_d[r0:r0 + 128, :], vs, [vs_t], t_vd, sem_tile=vs_t)
                    if pss == 1:
                        conv_step(1)
                    if pss == 0:
                        for kc in range(16):
                            pe("matmul", psf(6)[:, 0:8], lhsT=hb[:, kc, tt * 128:(tt + 1) * 128], rhs=Wf[:, kc, :],
                               start=(kc == 0), stop=(kc == 15), reads=[t_Wf, hbt], writes=[pst[6]])
                        j = G * 4 + tt
                        dv("tensor_tensor", out=fl_all[:, j * 8:(j + 1) * 8], in0=psf(6)[:, 0:8], in1=c_bf, op=ALU.add,
                           reads=[pst[6], t_const], writes=[t_fl])
            if pss == 0:
                act("activation", out=fl_all, in_=fl_all, func=AF.Sigmoid, reads=[t_fl], writes=[t_fl])
                act("activation", out=fl_all, in_=fl_all, func=AF.Ln, reads=[t_fl], writes=[t_fl])
                pe("matmul", psf(6)[:, 0:256], lhsT=c_trif, rhs=fl_all, start=True, stop=True, reads=[t_fl, t_const], writes=[pst[6]])
                pe("matmul", psf(7)[:, 0:256], lhsT=c_onesf, rhs=fl_all, start=True, stop=True, reads=[t_fl, t_const], writes=[pst[7]])
                ctot = A.alloc([8, NB], F32)
                cpre = A.alloc([8, NB], F32)
                cm = A.alloc([8, NB], F32)
                ctmp = A.alloc([8, NB], F32)
                dv("tensor_copy", out=ctot, in_=psf(7)[:, 0:256].rearrange("p (j h) -> p h j", h=8), reads=[pst[7]], writes=[t_c])
                for hd in range(8):
                    dv("tensor_tensor_scan", out=cpre[:, hd, :], data0=c_onesf[:, 0:NB], data1=ctot[:, hd, :], initial=0.0,
                       op0=ALU.mult, op1=ALU.add, reads=[t_c, t_const], writes=[t_c])
                dv("tensor_sub", out=cpre, in0=cpre, in1=ctot, reads=[t_c], writes=[t_c])
                dv("tensor_tensor", out=c_all, in0=psf(6)[:, 0:256].rearrange("p (j h) -> p h j", h=8), in1=cpre, op=ALU.add,
                   reads=[pst[6], t_c], writes=[t_c])
                dv("scalar_tensor_tensor", out=cm, in0=ctot, scalar=0.5, in1=cpre, op0=ALU.mult, op1=ALU.add, reads=[t_c], writes=[t_c])
                for k in range(NSLOT):
                    dv("tensor_tensor", out=ctmp, in0=cm, in1=c_onehot[:, k:k + 1, :].broadcast_to([128, 8, NB]), op=ALU.mult,
                       reads=[t_c, t_const], writes=[t_c])
                    dv("tensor_reduce", out=cbias[:, :, k], in_=ctmp, axis=AX.X, op=ALU.add, reads=[t_c], writes=[t_c])
            P.barrier()

        if stage == 1:
            return finish_debug(nc, P, dt, [("c_all", c_all, [128, 8, NB], F32), ("cbias", cbias, [128, 8, NSLOT], F32),
                                            ("m_le", m_le, [128, 32, 128], BF16),
                                            ("kaT", kaT_d[0], [128, S], BF16), ("kbT", kbT_d[7], [128, S], BF16),
                                            ("va", va_d[0:512, :], [512, 1024], BF16),
                                            ("vb", vb_d[3584:4096, :], [512, 1024], BF16)], [t_const, t_c] + kv_tiles)

        A.reset(persist_mark)
        hTo = A.alloc([16, TOWN], BF16)
        t_hTo = P.tile("hTo")
        oaT = A.alloc([8, TOWN], BF16)
        obT = A.alloc([8, TOWN], BF16)
        t_oa = P.tile("oaT")
        t_ob = P.tile("obT")
        mid_mark = A.mark()
        QT = A.alloc([16, TOWN], BF16)
        t_QT = P.tile("QT")
        att_mark = A.mark()
        Wq = A.alloc([16, 1024], BF16)
        t_Wq = P.tile("Wq")
        xst = [A.alloc([D], F32) for _ in range(2)]
        xst_t = [P.tile("xst%d" % i) for i in range(2)]
        xn = [A.alloc([D], BF16) for _ in range(2)]
        xn_t = [P.tile("xn%d" % i) for i in range(2)]
        ssb = [A.alloc([4], F32) for _ in range(2)]
        ss_t = [P.tile("ss%d" % i) for i in range(2)]
        for i in range(8):
            norm_transpose(xown[i * 128:(i + 1) * 128, :], gbc, t_gbc, xst[i % 2], xst_t[i % 2], xn[i % 2], xn_t[i % 2], ssb[i % 2], ss_t[i % 2],
                           hTo, t_hTo, i * 128, (0, 1))
        for part, c0 in ((0, 0), (1, 3080)):
            for kc8 in range(2):
                P.DMA("pool", Wq[:, kc8 * 8:(kc8 + 1) * 8, :],
                      w_in[kc8 * 1024:(kc8 + 1) * 1024, c0:c0 + 1024].rearrange("(kc p) n -> p kc n", p=128), [], t_Wq)
            for hh in range(8):
                hd = part * 8 + hh
                for half in range(2):
                    b = 2 + (hd * 2 + half) % 4
                    for kc in range(16):
                        pe("matmul", psf(b), lhsT=Wq[:, kc, hh * 128:(hh + 1) * 128], rhs=hTo[:, kc, half * 512:(half + 1) * 512],
                           start=(kc == 0), stop=(kc == 15), reads=[t_Wq, t_hTo], writes=[pst[b]])
                    conv_step(1)
                    if half == 0:
                        act("activation", out=QT[:, hd, 0:512], in_=psf(b), func=AF.Copy, scale=SCALE, reads=[pst[b]], writes=[t_QT])
                    else:
                        dv("tensor_scalar", QT[:, hd, 512:1024], psf(b), SCALE, None, ALU.mult, reads=[pst[b]], writes=[t_QT])
        P.barrier()

        A.reset(att_mark)
        KT = [A.alloc([S], BF16) for _ in range(2)]
        KT_t = [P.tile("KT%d" % i) for i in range(2)]
        Vt = [A.alloc([NB, 128], BF16) for _ in range(2)]
        V_t = [P.tile("V%d" % i) for i in range(2)]
        Pt = [A.alloc([TOWN], BF16) for _ in range(2)]
        Pt_t = [P.tile("Pt%d" % i) for i in range(2)]
        clampt = A.alloc([128], F32)
        t_clamp = P.tile("clamp")
        Lr = A.alloc([TOWN], F32)
        t_Lr = P.tile("Lr")
        e1 = [A.alloc([TOWN], F32) for _ in range(2)]
        e1_t = [P.tile("e1%d" % i) for i in range(2)]
        spb = [A.alloc([TOWN], BF16) for _ in range(2)]
        sp_t = [P.tile("sp%d" % i) for i in range(2)]
        Nsum = A.alloc([TOWN], BF16)
        t_Ns = P.tile("Nsum")

        def load_kv(hd, kT_d, v_d, slot):
            P.DMA("sp", KT[slot], kT_d[hd], list(kv_tiles), KT_t[slot])
            P.DMA("sp", Vt[slot], v_d[:, hd * 128:(hd + 1) * 128].rearrange("(kb p) d -> p kb d", p=128), list(kv_tiles), V_t[slot])

        def col_splits(c0):
            out = []
            if c0 < 512:
                out.append((0, c0, 512))
                out.append((1, 512, 1024))
            else:
                out.append((1, c0, 1024))
            return out

        negc = [A.alloc([NB], F32) for _ in range(2)]
        negc_t = [P.tile("negc%d" % i) for i in range(2)]
        clampb = [clampt, A.alloc([128], F32)]
        clamp_t = [t_clamp, P.tile("clamp1")]

        def load_head(H):
            if H >= 16:
                return
            if H < 8:
                load_kv(H, kaT_d, va_d, H % 2)
            else:
                load_kv(H - 8, kbT_d, vb_d, H % 2)

        load_head(0)
        load_head(1)

        fox_iters = [(hd, kb) for hd in range(8) for kb in range(NB)]

        Pacc = A.alloc([TOWN], F32)
        t_Pacc = P.tile("Pacc")

        crow = [A.alloc([TOWN], BF16) for _ in range(2)]
        crow_t = [P.tile("crow%d" % i) for i in range(2)]

        def fox_A(j):
            hd, kb = fox_iters[j]
            sl = hd % 2
            if kb == 0:
                dv("tensor_scalar", negc[sl], c_all[:, hd, :], -1.0, None, ALU.mult, reads=[t_c], writes=[negc_t[sl]])
                dv("tensor_copy", out=crow[sl][0:1, :].rearrange("p (k c) -> p k c", k=NSLOT),
                   in_=cbias[0:1, hd, :].rearrange("p (k o) -> p k o", o=1).broadcast_to([1, NSLOT, 128]),
                   reads=[t_c], writes=[crow_t[sl]])
            c0 = (kb // 4) * 128
            sb = (j % 3) * 2
            for (bh, lo, hi) in col_splits(c0):
                dst = psf(sb + bh)[:, lo - bh * 512:hi - bh * 512]
                pe("matmul", dst, lhsT=KT[sl][:, kb * 128:(kb + 1) * 128], rhs=QT[:, hd, lo:hi],
                   start=True, stop=False, skip_group_check=True, reads=[KT_t[sl], t_QT], writes=[pst[sb + bh]])
                pe("matmul", dst, lhsT=c_ones[0:1, :], rhs=crow[sl][0:1, lo:hi],
                   start=False, stop=True, skip_group_check=True, reads=[t_const, crow_t[sl]], writes=[pst[sb + bh]])

        def fox_E1(j):
            hd, kb = fox_iters[j]
            sl = hd % 2
            g = kb // 4
            c0 = g * 128
            sb = (j % 3) * 2
            pt, ptt = Pt[j % 2], Pt_t[j % 2]
            cl, clt = clampb[j % 2], clamp_t[j % 2]
            bh = g // 4
            src = psf(sb + bh)[:, (g % 4) * 128:(g % 4 + 1) * 128]
            dv("tensor_scalar", cl, src, negc[sl][:, kb:kb + 1], 60.0, ALU.add, ALU.min, reads=[pst[sb + bh], negc_t[sl]], writes=[clt])
            act("activation", out=pt[:, c0:c0 + 128], in_=cl, func=AF.Exp, reads=[clt], writes=[ptt])
            if c0 + 128 < TOWN:
                for (bh2, lo, hi) in col_splits(c0 + 128):
                    act("activation", out=pt[:, lo:hi], in_=psf(sb + bh2)[:, lo - bh2 * 512:hi - bh2 * 512], func=AF.Exp,
                        bias=negc[sl][:, kb:kb + 1], reads=[pst[sb + bh2], negc_t[sl]], writes=[ptt])

        def fox_E2(j):
            hd, kb = fox_iters[j]
            c0 = (kb // 4) * 128
            pt, ptt = Pt[j % 2], Pt_t[j % 2]
            dv("tensor_mul", out=pt[:, c0:c0 + 128], in0=pt[:, c0:c0 + 128], in1=m_le[:, kb, :], reads=[ptt, t_const], writes=[ptt])
            if kb == 0:
                dv("tensor_copy", out=Pacc[:, 0:512], in_=pt[:, 0:512], reads=[ptt], writes=[t_Pacc])
                P.I("pool", "tensor_copy", out=Pacc[:, 512:TOWN], in_=pt[:, 512:TOWN], reads=[ptt], writes=[t_Pacc])
            else:
                if c0 < 512:
                    dv("tensor_tensor", out=Pacc[:, c0:512], in0=Pacc[:, c0:512], in1=pt[:, c0:512], op=ALU.add, reads=[ptt, t_Pacc], writes=[t_Pacc])
                lo2 = max(c0, 512)
                P.I("pool", "tensor_tensor", out=Pacc[:, lo2:TOWN], in0=Pacc[:, lo2:TOWN], in1=pt[:, lo2:TOWN], op=ALU.add,
                    reads=[ptt, t_Pacc], writes=[t_Pacc])

        def fox_C(j):
            hd, kb = fox_iters[j]
            sl = hd % 2
            c0 = (kb // 4) * 128
            pt, ptt = Pt[j % 2], Pt_t[j % 2]
            for (bh, lo, hi) in col_splits(c0):
                pe("matmul", psf(6 + bh)[:, lo - bh * 512:hi - bh * 512], lhsT=Vt[sl][:, kb, :], rhs=pt[:, lo:hi],
                   start=(kb == 0), stop=(kb == NB - 1), skip_group_check=True, reads=[V_t[sl], ptt], writes=[pst[6 + bh]])
            if kb == NB - 1:
                for bh in range(2):
                    lb = ((j + 1) % 3) * 2 + bh
                    pe("matmul", psf(lb), lhsT=c_onesf, rhs=Pacc[:, bh * 512:(bh + 1) * 512], start=True, stop=True,
                       reads=[t_const, t_Pacc], writes=[pst[lb]])
                    dv("reciprocal", Lr[:, bh * 512:(bh + 1) * 512], psf(lb), reads=[pst[lb]], writes=[t_Lr])
                    dv("tensor_tensor", out=oaT[:, hd, bh * 512:(bh + 1) * 512], in0=psf(6 + bh), in1=Lr[:, bh * 512:(bh + 1) * 512], op=ALU.mult,
                       reads=[pst[6 + bh], t_Lr], writes=[t_oa])
                load_head(hd + 2)

        nfx = len(fox_iters)
        fox_A(0)
        fox_A(1)
        fox_E1(0)
        for j in range(nfx):
            if j + 2 < nfx:
                fox_A(j + 2)
            if j + 1 < nfx:
                fox_E1(j + 1)
            fox_E2(j)
            fox_C(j)

        sb_iters = [(hd, kb) for hd in range(8) for kb in range(NB - 1, -1, -1)]
        first_o = {}

        def sb_A(j):
            hd, kb = sb_iters[j]
            sl = hd % 2
            c0 = (kb // 4) * 128
            sb = (j % 3) * 2
            for (bh, lo, hi) in col_splits(c0):
                pe("matmul", psf(sb + bh)[:, lo - bh * 512:hi - bh * 512], lhsT=KT[sl][:, kb * 128:(kb + 1) * 128], rhs=QT[:, 8 + hd, lo:hi],
                   start=True, stop=False, skip_group_check=True, reads=[KT_t[sl], t_QT], writes=[pst[sb + bh]])

        def sb_S(j):
            hd, kb = sb_iters[j]
            c0 = (kb // 4) * 128
            sb = (j % 3) * 2
            ef, eft = e1[j % 2], e1_t[j % 2]
            spk, spt = spb[j % 2], sp_t[j % 2]
            for (bh, lo, hi) in col_splits(c0):
                act("activation", out=ef[:, lo:hi], in_=psf(sb + bh)[:, lo - bh * 512:hi - bh * 512], func=AF.Exp,
                    reads=[pst[sb + bh]], writes=[eft])
            act("activation", out=spk[:, c0:1024], in_=ef[:, c0:1024], func=AF.Ln, bias=1.0, reads=[eft], writes=[spt])
            dv("tensor_mul", out=spk[:, c0:c0 + 128], in0=spk[:, c0:c0 + 128], in1=m_lt[:, kb, :], reads=[spt, t_const], writes=[spt])

        def sb_B(j):
            hd, kb = sb_iters[j]
            c0 = (kb // 4) * 128
            sb = (j % 3) * 2
            spk, spt = spb[j % 2], sp_t[j % 2]
            if kb == NB - 1:
                P.I("pool", "memset", Nsum, 0.0, writes=[t_Ns])
            for (bh, lo, hi) in col_splits(c0):
                dst = psf(sb + bh)[:, lo - bh * 512:hi - bh * 512]
                pe("matmul", dst, lhsT=c_ntri, rhs=spk[:, lo:hi], start=False, stop=False, skip_group_check=True,
                   reads=[t_const, spt], writes=[pst[sb + bh]])
                pe("matmul", dst, lhsT=c_nones, rhs=Nsum[:, lo:hi], start=False, stop=True, skip_group_check=True,
                   reads=[t_const, t_Ns], writes=[pst[sb + bh]])

        def sb_X(j):
            hd, kb = sb_iters[j]
            c0 = (kb // 4) * 128
            sb = (j % 3) * 2
            spk, spt = spb[j % 2], sp_t[j % 2]
            at, att = Pt[j % 2], Pt_t[j % 2]
            for (bh, lo, hi) in col_splits(c0):
                act("activation", out=at[:, lo:hi], in_=psf(sb + bh)[:, lo - bh * 512:hi - bh * 512], func=AF.Exp,
                    reads=[pst[sb + bh]], writes=[att])
            dv("tensor_mul", out=at[:, c0:c0 + 128], in0=at[:, c0:c0 + 128], in1=m_lt[:, kb, :], reads=[att, t_const], writes=[att])
            P.I("pool", "tensor_tensor", out=Nsum[:, c0:1024], in0=Nsum[:, c0:1024], in1=spk[:, c0:1024], op=ALU.add,
                reads=[t_Ns, spt], writes=[t_Ns])

        def sb_C(j):
            hd, kb = sb_iters[j]
            sl = hd % 2
            c0 = (kb // 4) * 128
            at, att = Pt[j % 2], Pt_t[j % 2]
            for (bh, lo, hi) in col_splits(c0):
                fo = first_o.get((hd, bh), True)
                pe("matmul", psf(6 + bh)[:, lo - bh * 512:hi - bh * 512], lhsT=Vt[sl][:, kb, :], rhs=at[:, lo:hi],
                   start=fo, stop=(kb == 0), skip_group_check=True, reads=[V_t[sl], att], writes=[pst[6 + bh]])
                first_o[(hd, bh)] = False
            if kb == 0:
                act("activation", out=obT[:, hd, 0:512], in_=psf(6), func=AF.Copy, reads=[pst[6]], writes=[t_ob])
                dv("tensor_copy", out=obT[:, hd, 512:1024], in_=psf(7), reads=[pst[7]], writes=[t_ob])
                load_head(8 + hd + 2)

        nsb = len(sb_iters)
        sb_A(0)
        sb_S(0)
        sb_A(1)
        for j in range(nsb):
            sb_B(j)
            if j + 2 < nsb:
                sb_A(j + 2)
            if j + 1 < nsb:
                sb_S(j + 1)
            if j >= 1:
                sb_C(j - 1)
            sb_X(j)
            if j % 2 == 0:
                conv_step(1)
        sb_C(nsb - 1)
        P.barrier()
        if stage == 2:
            return finish_debug(nc, P, dt, [("oaT", oaT, [128, 8, TOWN], BF16), ("obT", obT, [128, 8, TOWN], BF16)], [t_oa, t_ob])

        A.reset(mid_mark)
        mixedT = A.alloc([16, TOWN], BF16)
        t_mx = P.tile("mixedT")
        p6_mark = A.mark()
        Wga = [A.alloc([16, 256], BF16) for _ in range(2)]
        Wgb = [A.alloc([16, 256], BF16) for _ in range(2)]
        Wfx = [A.alloc([8, 256], BF16) for _ in range(2)]
        Wsb = [A.alloc([8, 256], BF16) for _ in range(2)]
        Wg_t = [P.tile("Wg6_%d" % i) for i in range(2)]
        sa = [A.alloc([512], F32) for _ in range(2)]
        sb_ = [A.alloc([512], F32) for _ in range(2)]
        sg_t = [P.tile("sg%d" % i) for i in range(2)]

        def load_w6(ng, s):
            P.DMA("pool", Wga[s], w_in[:, 6152 + ng * 256:6152 + (ng + 1) * 256].rearrange("(kc p) n -> p kc n", p=128), [], Wg_t[s])
            P.DMA("pool", Wgb[s], w_in[:, 8200 + ng * 256:8200 + (ng + 1) * 256].rearrange("(kc p) n -> p kc n", p=128), [], Wg_t[s])
            P.DMA("pool", Wfx[s], w_bfox[:, ng * 256:(ng + 1) * 256].rearrange("(kc p) n -> p kc n", p=128), [], Wg_t[s])
            P.DMA("pool", Wsb[s], w_bsb[:, ng * 256:(ng + 1) * 256].rearrange("(kc p) n -> p kc n", p=128), [], Wg_t[s])

        load_w6(0, 0)
        it6 = 0
        for ng in range(8):
            s = ng % 2
            if ng + 1 < 8:
                load_w6(ng + 1, (ng + 1) % 2)
            for nsub in range(2):
                nch = ng * 2 + nsub
                for half in range(2):
                    pb = (it6 % 2) * 4
                    sa_, sbb, sgt = sa[it6 % 2], sb_[it6 % 2], sg_t[it6 % 2]
                    it6 += 1
                    cs = slice(half * 512, (half + 1) * 512)
                    ns = slice(nsub * 128, (nsub + 1) * 128)
                    for kc in range(16):
                        pe("matmul", psf(pb), lhsT=Wga[s][:, kc, ns], rhs=hTo[:, kc, cs], start=(kc == 0), stop=(kc == 15),
                           reads=[Wg_t[s], t_hTo], writes=[pst[pb]])
                    for kc in range(16):
                        pe("matmul", psf(pb + 1), lhsT=Wgb[s][:, kc, ns], rhs=hTo[:, kc, cs], start=(kc == 0), stop=(kc == 15),
                           reads=[Wg_t[s], t_hTo], writes=[pst[pb + 1]])
                    for hc in range(8):
                        pe("matmul", psf(pb + 2), lhsT=Wfx[s][:, hc, ns], rhs=oaT[:, hc, cs], start=(hc == 0), stop=(hc == 7),
                           reads=[Wg_t[s], t_oa], writes=[pst[pb + 2]])
                    for hc in range(8):
                        pe("matmul", psf(pb + 3), lhsT=Wsb[s][:, hc, ns], rhs=obT[:, hc, cs], start=(hc == 0), stop=(hc == 7),
                           reads=[Wg_t[s], t_ob], writes=[pst[pb + 3]])
                    act("activation", out=sa_, in_=psf(pb), func=AF.Sigmoid, reads=[pst[pb]], writes=[sgt])
                    act("activation", out=sbb, in_=psf(pb + 1), func=AF.Sigmoid, reads=[pst[pb + 1]], writes=[sgt])
                    dv("tensor_tensor", out=sa_, in0=sa_, in1=psf(pb + 2), op=ALU.mult, reads=[sgt, pst[pb + 2]], writes=[sgt])
                    dv("tensor_tensor", out=sbb, in0=sbb, in1=psf(pb + 3), op=ALU.mult, reads=[sgt, pst[pb + 3]], writes=[sgt])
                    P.I("pool", "tensor_tensor", out=mixedT[:, nch, cs], in0=sa_, in1=sbb, op=ALU.add, reads=[sgt], writes=[t_mx])
                    conv_step(3)
        P.barrier()
        A.reset(p6_mark)
        Wm = [A.alloc([16, 512], BF16) for _ in range(2)]
        Wm_t = [P.tile("Wm%d" % i) for i in range(2)]
        xo = [A.alloc([512], F32) for _ in range(3)]
        xo_t = [P.tile("xo%d" % i) for i in range(3)]
        x1s = [A.alloc([512], F32) for _ in range(2)]
        x1s_t = [P.tile("x1s%d" % i) for i in range(2)]
        t_x1d = P.tile("x1_d")
        P.DMA("pool", Wm[0], w_mix[:, 0:512].rearrange("(kc p) n -> p kc n", p=128), [], Wm_t[0])
        mo_iters = [(cg, i) for cg in range(4) for i in range(8)]

        def xo_load(n):
            cg, i = mo_iters[n]
            P.DMA("sp", xo[n % 3], xown[i * 128:(i + 1) * 128, cg * 512:(cg + 1) * 512], [], xo_t[n % 3])

        xo_load(0)
        xo_load(1)
        for n, (cg, i) in enumerate(mo_iters):
            s_ = cg % 2
            if i == 0 and cg + 1 < 4:
                P.DMA("pool", Wm[(cg + 1) % 2], w_mix[:, (cg + 1) * 512:(cg + 2) * 512].rearrange("(kc p) n -> p kc n", p=128), [], Wm_t[(cg + 1) % 2])
            if n + 2 < len(mo_iters):
                xo_load(n + 2)
            b = n % 4
            u = n % 2
            for kc in range(16):
                pe("matmul", psf(b), lhsT=mixedT[:, kc, i * 128:(i + 1) * 128], rhs=Wm[s_][:, kc, :], start=(kc == 0), stop=(kc == 15),
                   reads=[t_mx, Wm_t[s_]], writes=[pst[b]])
            dv("tensor_tensor", out=x1s[u], in0=psf(b), in1=xo[n % 3], op=ALU.add, reads=[pst[b], xo_t[n % 3]], writes=[x1s_t[u]])
            P.DMA("sp", x1_d[i * 128:(i + 1) * 128, cg * 512:(cg + 1) * 512], x1s[u], [x1s_t[u]], t_x1d, sem_tile=x1s_t[u])
            conv_step(1)
        conv_flush()
        P.barrier()
        if stage == 3:
            return finish_debug(nc, P, dt, [("x1", x1_d, [TOWN, D], F32)], [t_x1d])
        A.reset(core_mark)
        Wr = A.alloc([16, 36], F32)
        t_Wr = P.tile("Wr")
        P.DMA("sp", Wr, w_rout.rearrange("(kc p) n -> p kc n", p=128), [], t_Wr)
        Eall = A.alloc([NSLOT, 2, 32], F32)
        gall = A.alloc([NSLOT, 2], F32)
        Esum = A.alloc([NSLOT, 32], F32)
        Call = A.alloc([NSLOT, 32], F32)
        dest_f = A.alloc([NSLOT, 2], F32)
        dest_i = A.alloc([NSLOT, 2], I32)
        idxw_f = A.alloc([NBLK, 4], F32)
        idxw_i = A.alloc([NBLK, 4], I32)
        t_rt = P.tile("route")
        moe_mark = A.mark()
        gbc = A.alloc([D], F32)
        t_gbc = P.tile("gbc7")
        P.DMA("sp", gbc, bcast_row(g_ffn), [], t_gbc)
        h2b = A.alloc([NSLOT, D], BF16)
        t_h2b = P.tile("h2b")
        xst = [A.alloc([D], F32) for _ in range(2)]
        xst_t = [P.tile("x1t%d" % i) for i in range(2)]
        junk = A.alloc([D], BF16)
        junk_t = P.tile("junk")
        ssb = [A.alloc([4], F32) for _ in range(2)]
        ss_t = [P.tile("ss%d" % i) for i in range(2)]
        h2f = A.alloc([D], F32)
        t_h2f = P.tile("h2f")
        h2fT = A.alloc([16, 128], F32)
        t_h2fT = P.tile("h2fT")
        lg = A.alloc([36], F32)
        rs = A.alloc([16], F32)
        goh = A.alloc([4], F32)
        j4 = A.alloc([4], F32)
        t48 = A.alloc([4, 8], F32)
        esel = A.alloc([8], F32)
        v8 = A.alloc([8], F32)
        oh1 = A.alloc([8], F32)
        oh2 = A.alloc([8], F32)
        t_r = P.tile("rtmp")
        TR = [t_r]
        for i in range(NSLOT):
            u = i % 2
            P.DMA("sp", xst[u], x1_d[i * 128:(i + 1) * 128, :], [t_x1d], xst_t[u])
            norm_rstd(xst[u], xst_t[u], junk, junk_t, ssb[u], ss_t[u])
            dv("scalar_tensor_tensor", out=h2f, in0=xst[u], scalar=ssb[u][:, 2:3], in1=gbc, op0=ALU.mult, op1=ALU.mult,
               reads=[xst_t[u], ss_t[u], t_gbc], writes=[t_h2f])
            act("activation", out=h2b[:, i, :], in_=h2f, func=AF.Copy, reads=[t_h2f], writes=[t_h2b])
            for q4 in range(4):
                for q in range(4):
                    kc = q4 * 4 + q
                    pe("transpose", psf(q4)[:, q * 128:(q + 1) * 128], h2f[:, kc * 128:(kc + 1) * 128], c_identf,
                       reads=[t_h2f, t_const], writes=[pst[q4]])
                o_ = h2fT[:, q4 * 4:(q4 + 1) * 4, :]
                i_ = psf(q4).rearrange("p (a b) -> p a b", a=4)
                if q4 % 2 == 0:
                    act("activation", out=o_, in_=i_, func=AF.Copy, reads=[pst[q4]], writes=[t_h2fT])
                else:
                    dv("tensor_copy", out=o_, in_=i_, reads=[pst[q4]], writes=[t_h2fT])
            for kc in range(16):
                pe("matmul", psf(4)[:, 0:36], lhsT=h2fT[:, kc, :], rhs=Wr[:, kc, :], start=(kc == 0), stop=(kc == 15),
                   reads=[t_h2fT, t_Wr], writes=[pst[4]])
            dv("tensor_copy", out=lg, in_=psf(4)[:, 0:36], reads=[pst[4]], writes=TR)
            dv("tensor_reduce", out=rs[:, 0:1], in_=lg[:, 0:4], axis=AX.X, op=ALU.max, reads=TR, writes=TR)
            dv("tensor_scalar", goh, lg[:, 0:4], rs[:, 0:1], None, ALU.is_equal, reads=TR, writes=TR)
            dv("tensor_scalar", rs[:, 1:2], rs[:, 0:1], -1.0, None, ALU.mult, reads=TR, writes=TR)
            act("activation", out=j4, in_=lg[:, 0:4], func=AF.Exp, bias=rs[:, 1:2], accum_out=rs[:, 2:3], reads=TR, writes=TR)
            dv("reciprocal", rs[:, 3:4], rs[:, 2:3], reads=TR, writes=TR)
            dv("tensor_tensor", out=t48, in0=lg[:, 4:36].rearrange("p (g e) -> p g e", g=4),
               in1=goh.rearrange("p (g o) -> p g o", o=1).broadcast_to([128, 4, 8]), op=ALU.mult, reads=TR, writes=TR)
            dv("tensor_reduce", out=esel, in_=t48.rearrange("p g e -> p e g"), axis=AX.X, op=ALU.add, reads=TR, writes=TR)
            dv("max", v8, esel, reads=TR, writes=TR)
            dv("tensor_scalar", oh1, esel, v8[:, 0:1], None, ALU.is_equal, reads=TR, writes=TR)
            dv("tensor_scalar", oh2, esel, v8[:, 1:2], None, ALU.is_equal, reads=TR, writes=TR)
            dv("tensor_sub", out=rs[:, 4:5], in0=v8[:, 1:2], in1=v8[:, 0:1], reads=TR, writes=TR)
            act("activation", out=rs[:, 5:6], in_=rs[:, 4:5], func=AF.Exp, reads=TR, writes=TR)
            dv("tensor_scalar", rs[:, 6:7], rs[:, 5:6], 1.0, None, ALU.add, reads=TR, writes=TR)
            dv("reciprocal", rs[:, 7:8], rs[:, 6:7], reads=TR, writes=TR)
            dv("tensor_mul", out=gall[:, i, 0:1], in0=rs[:, 7:8], in1=rs[:, 3:4], reads=TR, writes=[t_rt])
            dv("tensor_mul", out=gall[:, i, 1:2], in0=rs[:, 5:6], in1=gall[:, i, 0:1], reads=TR + [t_rt], writes=[t_rt])
            for k, oh in ((0, oh1), (1, oh2)):
                dv("tensor_tensor", out=Eall[:, i, k, :].rearrange("p (g e) -> p g e", g=4),
                   in0=goh.rearrange("p (g o) -> p g o", o=1).broadcast_to([128, 4, 8]),
                   in1=oh.rearrange("p (o e) -> p o e", o=1).broadcast_to([128, 4, 8]), op=ALU.mult, reads=TR, writes=[t_rt])
            dv("tensor_add", out=Esum[:, i, :], in0=Eall[:, i, 0, :], in1=Eall[:, i, 1, :], reads=[t_rt], writes=[t_rt])
        for i in range(NSLOT):
            b = 5 + i % 2
            for i2 in range(i):
                pe("matmul", psf(b)[:, 0:32], lhsT=c_onesf, rhs=Esum[:, i2, :], start=(i2 == 0), stop=False,
                   reads=[t_rt, t_const], writes=[pst[b]])
            pe("matmul", psf(b)[:, 0:32], lhsT=c_strif, rhs=Esum[:, i, :], start=(i == 0), stop=True,
               reads=[t_rt, t_const], writes=[pst[b]])
            dv("tensor_copy", out=Call[:, i, :], in_=psf(b)[:, 0:32], reads=[pst[b]], writes=[t_rt])
        for i in range(NSLOT):
            pe("matmul", psf(7)[:, 0:32], lhsT=c_onesf, rhs=Esum[:, i, :], start=(i == 0), stop=(i == NSLOT - 1),
               reads=[t_rt, t_const], writes=[pst[7]])
        cnt = A.alloc([32], F32)
        nbk = A.alloc([32], F32)
        pend = A.alloc([32], F32)
        pstart = A.alloc([32], F32)
        Dall = A.alloc([NSLOT, 32], F32)
        tmpE = A.alloc([NSLOT, 32], F32)
        tmpb = A.alloc([NBLK, 32], F32)
        bef = A.alloc([NBLK], F32)
        nef = A.alloc([NBLK], F32)
        dv("tensor_copy", out=cnt, in_=psf(7)[:, 0:32], reads=[pst[7]], writes=TR)
        dv("tensor_single_scalar", nbk, cnt, 0.0, ALU.is_gt, reads=TR, writes=TR)
        for j in range(1, 8):
            dv("scalar_tensor_tensor", out=nbk, in0=cnt, scalar=128.0 * j, in1=nbk, op0=ALU.is_gt, op1=ALU.add, reads=TR, writes=TR)
        dv("tensor_tensor_scan", out=pend, data0=c_onesf[:, 0:32], data1=nbk, initial=0.0, op0=ALU.mult, op1=ALU.add,
           reads=TR + [t_const], writes=TR)
        dv("tensor_scalar", pend, pend, 128.0, None, ALU.mult, reads=TR, writes=TR)
        dv("scalar_tensor_tensor", out=pstart, in0=nbk, scalar=-128.0, in1=pend, op0=ALU.mult, op1=ALU.add, reads=TR, writes=TR)
        dv("tensor_tensor", out=Dall, in0=Call, in1=pstart.rearrange("p (o e) -> p o e", o=1).broadcast_to([128, NSLOT, 32]), op=ALU.add,
           reads=TR + [t_rt], writes=TR)
        for k in range(2):
            dv("tensor_tensor", out=tmpE, in0=Eall[:, :, k, :], in1=Dall, op=ALU.mult, reads=TR + [t_rt], writes=TR)
            dv("tensor_reduce", out=dest_f[:, :, k], in_=tmpE, axis=AX.X, op=ALU.add, reads=TR, writes=[t_rt])
        dv("tensor_copy", out=dest_i, in_=dest_f, reads=[t_rt], writes=[t_rt])
        dv("tensor_tensor", out=tmpb, in0=pend.rearrange("p (o e) -> p o e", o=1).broadcast_to([128, NBLK, 32]),
           in1=c_j128.rearrange("p (b o) -> p b o", o=1).broadcast_to([128, NBLK, 32]), op=ALU.is_le, reads=TR + [t_const], writes=TR)
        dv("tensor_reduce", out=bef, in_=tmpb, axis=AX.X, op=ALU.add, reads=TR, writes=TR)
        dv("tensor_scalar", bef, bef, 31.0, None, ALU.min, reads=TR, writes=TR)
        dv("tensor_scalar", nef, c_j128, pend[:, 31:32], None, ALU.is_lt, reads=TR + [t_const], writes=TR)
        BIG = 1.0e6
        dv("tensor_scalar", nef, nef, -1.0, -BIG, ALU.add, ALU.mult, reads=TR, writes=TR)
        dv("tensor_scalar", bef, bef, 512.0, c_pidx[:, 0:1], ALU.mult, ALU.add, reads=TR + [t_const], writes=TR)
        dv("tensor_add", out=bef, in0=bef, in1=nef, reads=TR, writes=TR)
        for m in range(4):
            dv("tensor_scalar", idxw_f[:, :, m], bef, 128.0 * m, None, ALU.add, reads=TR, writes=TR)
        dv("tensor_copy", out=idxw_i, in_=idxw_f, reads=TR, writes=[t_rt])
        for i in range(NSLOT):
            for k in range(2):
                P.op("pool", ("indirect_dma_start", (), dict(out=xb_d, out_offset=bass.IndirectOffsetOnAxis(ap=dest_i[:, i, k:k + 1], axis=0),
                                                             in_=h2b[:, i, :], in_offset=None)),
                     reads=[t_h2b, t_rt, t_xbz], writes=[t_xbd], dma_out=t_xbd)
        P.barrier()
        if stage == 35:
            return finish_debug(nc, P, dt, [("dest", dest_i, [128, NSLOT, 2], I32), ("gall", gall, [128, NSLOT, 2], F32),
                                            ("idxw", idxw_i, [128, NBLK, 4], I32), ("Eall", Eall, [128, NSLOT, 2, 32], F32),
                                            ("xb", xb_d, [NBLK * 128, D], BF16)], [t_rt, t_xbd])

        A.reset(moe_mark)
        NWB = 3
        order_ = []
        for q_ in range(16):
            order_ += [q_, 32 + q_]
        order_ += list(range(16, 32))
        WG = [A.alloc([4, 2048], BF16) for _ in range(NWB)]
        WU = [A.alloc([4, 2048], BF16) for _ in range(NWB)]
        WD = [A.alloc([4, 2048], BF16) for _ in range(NWB)]
        W8_t = [P.tile("W8_%d" % i) for i in range(NWB)]
        xbs = [A.alloc([D], BF16) for _ in range(4)]
        xbs_t = [P.tile("xbs%d" % i) for i in range(4)]
        xbT = [A.alloc([16, 128], BF16) for _ in range(2)]
        xbT_t = [P.tile("xbT%d" % i) for i in range(2)]
        sgf = A.alloc([512], F32)
        t_sgf = P.tile("sgf")
        hid = A.alloc([512], BF16)
        t_hid = P.tile("hid")
        hidT = A.alloc([4, 128], BF16)
        t_hidT = P.tile("hidT")
        yst = [A.alloc([D], F32) for _ in range(2)]
        yst_t = [P.tile("yst%d" % i) for i in range(2)]
        t_ybd = P.tile("yb_d")

        def load_w8(n_, s):
            blk = order_[n_]
            for (wsrc, wdst) in ((wb_d[0], WG[s]), (wb_d[1], WU[s]), (wb_d[2], WD[s])):
                for m in range(4):
                    P.op("pool", ("indirect_dma_start", (), dict(out=wdst[:, m, :], out_offset=None, in_=wsrc,
                                                                 in_offset=bass.IndirectOffsetOnAxis(ap=idxw_i[:, blk, m:m + 1], axis=0),
                                                                 bounds_check=NE * 512 - 1, oob_is_err=False)),
                         reads=[t_rt, t_wb], writes=[W8_t[s]], dma_out=W8_t[s])

        sgf2 = [sgf, A.alloc([512], F32)]
        sgf2_t = [t_sgf, P.tile("sgf1")]
        hid2 = [hid, A.alloc([512], BF16)]
        hid2_t = [t_hid, P.tile("hid1")]
        hidT2 = [hidT, A.alloc([4, 128], BF16)]
        hidT2_t = [t_hidT, P.tile("hidT1")]

        def moe_L(n_):
            pb_ = order_[n_]
            P.DMA("sp", xbs[n_ % 4], xb_d[pb_ * 128:(pb_ + 1) * 128, :], [t_xbd, t_xbz], xbs_t[n_ % 4])

        def moe_S1(blk):
            s = blk % 2
            if blk + 3 < NBLK:
                moe_L(blk + 3)
            for half in range(2):
                for q in range(8):
                    c = half * 8 + q
                    m, kk = c // 4, c % 4
                    pe("transpose", psb(half)[:, q * 128:(q + 1) * 128], xbs[blk % 4][:, 512 * m + kk:512 * (m + 1):4], c_identb,
                       reads=[xbs_t[blk % 4], t_const], writes=[pst[half]])
                o_ = xbT[s][:, half * 8:(half + 1) * 8, :]
                i_ = psb(half).rearrange("p (a b) -> p a b", a=8)
                if half == 0:
                    act("activation", out=o_, in_=i_, func=AF.Copy, reads=[pst[half]], writes=[xbT_t[s]])
                else:
                    dv("tensor_copy", out=o_, in_=i_, reads=[pst[half]], writes=[xbT_t[s]])

        def moe_S2(blk):
            s = blk % 2
            ws = blk % NWB
            for (wt, b_) in ((WG[ws], 2), (WU[ws], 3)):
                for c in range(16):
                    m, kk = c // 4, c % 4
                    pe("matmul", psf(b_), lhsT=xbT[s][:, c, :], rhs=wt[:, m, kk * 512:(kk + 1) * 512], start=(c == 0), stop=(c == 15),
                       reads=[xbT_t[s], W8_t[ws]], writes=[pst[b_]])
            act("activation", out=sgf2[s], in_=psf(2), func=AF.Silu, reads=[pst[2]], writes=[sgf2_t[s]])
            dv("tensor_tensor", out=hid2[s], in0=sgf2[s], in1=psf(3), op=ALU.mult, reads=[sgf2_t[s], pst[3]], writes=[hid2_t[s]])

        def moe_S3(blk):
            s = blk % 2
            for m in range(4):
                pe("transpose", psb(4)[:, m * 128:(m + 1) * 128], hid2[s][:, m * 128:(m + 1) * 128], c_identb,
                   reads=[hid2_t[s], t_const], writes=[pst[4]])
            dv("tensor_copy", out=hidT2[s], in_=psb(4)[:, 0:512].rearrange("p (a b) -> p a b", a=4), reads=[pst[4]], writes=[hidT2_t[s]])

        def moe_S4(blk):
            s = blk % 2
            ws = blk % NWB
            for cg in range(4):
                bk = 5 + (blk * 4 + cg) % 3
                for m in range(4):
                    pe("matmul", psf(bk), lhsT=hidT2[s][:, m, :], rhs=WD[ws][:, m, cg * 512:(cg + 1) * 512], start=(m == 0), stop=(m == 3),
                       reads=[hidT2_t[s], W8_t[ws]], writes=[pst[bk]])
                if cg % 2 == 0:
                    act("activation", out=yst[s][:, cg * 512:(cg + 1) * 512], in_=psf(bk), func=AF.Copy, reads=[pst[bk]], writes=[yst_t[s]])
                else:
                    dv("tensor_copy", out=yst[s][:, cg * 512:(cg + 1) * 512], in_=psf(bk), reads=[pst[bk]], writes=[yst_t[s]])
            pb_ = order_[blk]
            P.DMA("sp", yb_d[pb_ * 128:(pb_ + 1) * 128, :], yst[s], [yst_t[s]], t_ybd, sem_tile=yst_t[s])

        order = []
        for q in range(16):
            order += [q, 32 + q]
        order += list(range(16, 32))
        assert sorted(order) == list(range(NBLK))
        for n in range(3):
            moe_L(n)
        load_w8(0, 0)
        load_w8(1, 1)
        moe_S1(0)
        moe_S2(0)
        for n in range(NBLK):
            if n + 2 < NBLK:
                load_w8(n + 2, (n + 2) % NWB)
            if n + 1 < NBLK:
                moe_S1(n + 1)
            moe_S3(n)
            if n + 1 < NBLK:
                moe_S2(n + 1)
            moe_S4(n)
        P.barrier()

        A.reset(moe_mark)
        full = stage >= 5
        x1t = [A.alloc([D], F32) for _ in range(2)]
        x1t_t = [P.tile("x1t%d" % i) for i in range(2)]
        ya = [A.alloc([D], F32)] * 2
        ya_t = [P.tile("ya")] * 2
        yb2 = [A.alloc([D], F32)] * 2
        yb2_t = [P.tile("yb2")] * 2
        x2 = [A.alloc([D], F32) for _ in range(2)]
        x2_t = [P.tile("x2_%d" % i) for i in range(2)]
        t_dbg2 = P.tile("dbgx2")
        if not full:
            dbg_x2 = dt("dbg_x2", [TOWN, D], F32, kind="ExternalOutput").ap()
        if full:
            gfin = A.alloc([D], F32)
            t_gfin = P.tile("gfin")
            gbc = A.alloc([D], F32)
            t_gbc = P.tile("gbc9")
            P.DMA("sp", gbc, bcast_row(g_ple), [], t_gbc)
            P.DMA("sp", gfin, bcast_row(g_final), [], t_gfin)
            Wpg = A.alloc([16, D], BF16)
            Wpp = A.alloc([2, D], BF16)
            t_Wp = P.tile("Wp")
            for kc4 in range(4):
                P.DMA("pool", Wpg[:, kc4 * 4:(kc4 + 1) * 4, :], w_pg[kc4 * 512:(kc4 + 1) * 512, :].rearrange("(kc p) n -> p kc n", p=128), [], t_Wp)
            P.DMA("pool", Wpp, w_pp.rearrange("(kc p) n -> p kc n", p=128), [], t_Wp)
            h3 = [A.alloc([D], BF16) for _ in range(2)]
            h3_t = [P.tile("h3_%d" % i) for i in range(2)]
            h3T = [A.alloc([16, 128], BF16) for _ in range(2)]
            h3T_t = [P.tile("h3T%d" % i) for i in range(2)]
            ptl = A.alloc([256], F32)
            t_ptl = P.tile("ptl")
            pb16 = A.alloc([256], BF16)
            t_pb = P.tile("pb16")
            pT = [A.alloc([2, 128], BF16) for _ in range(2)]
            pT_t = [P.tile("pT%d" % i) for i in range(2)]
            sgp = [A.alloc([512], F32) for _ in range(2)]
            sgp_t = [P.tile("sgp%d" % i) for i in range(2)]
            x3 = [A.alloc([D], F32) for _ in range(2)]
            x3_t = [P.tile("x3_%d" % i) for i in range(2)]
            ot = [A.alloc([D], F32)] * 2
            ot_t = [P.tile("ot")] * 2
            ssc = [A.alloc([4], F32) for _ in range(2)]
            ssc_t = [P.tile("ssc%d" % i) for i in range(2)]
            ssd = [A.alloc([4], F32) for _ in range(2)]
            ssd_t = [P.tile("ssd%d" % i) for i in range(2)]
            t_y = P.tile("y")
        itp = [0]

        def tail_front(i):
            u = i % 2
            P.DMA("sp", x1t[u], x1_d[i * 128:(i + 1) * 128, :], [t_x1d], x1t_t[u])
            for (k, dst, dst_t) in ((0, ya[u], ya_t[u]), (1, yb2[u], yb2_t[u])):
                P.op("pool", ("indirect_dma_start", (), dict(out=dst, out_offset=None, in_=yb_d,
                                                             in_offset=bass.IndirectOffsetOnAxis(ap=dest_i[:, i, k:k + 1], axis=0))),
                     reads=[t_rt, t_ybd], writes=[dst_t], dma_out=dst_t)
            dv("scalar_tensor_tensor", out=x2[u], in0=ya[u], scalar=gall[:, i, 0:1], in1=x1t[u], op0=ALU.mult, op1=ALU.add,
               reads=[ya_t[u], x1t_t[u], t_rt], writes=[x2_t[u]])
            dv("scalar_tensor_tensor", out=x2[u], in0=yb2[u], scalar=gall[:, i, 1:2], in1=x2[u], op0=ALU.mult, op1=ALU.add,
               reads=[yb2_t[u], x2_t[u], t_rt], writes=[x2_t[u]])
            if not full:
                P.DMA("sp", dbg_x2[i * 128:(i + 1) * 128, :], x2[u], [x2_t[u]], t_dbg2, output=True, sem_tile=x2_t[u])
                return
            norm_rstd(x2[u], x2_t[u], h3[u], h3_t[u], ssc[u], ssc_t[u])
            dv("scalar_tensor_tensor", out=h3[u], in0=x2[u], scalar=ssc[u][:, 2:3], in1=gbc, op0=ALU.mult, op1=ALU.mult,
               reads=[x2_t[u], ssc_t[u], t_gbc], writes=[h3_t[u]])
            transpose16(h3[u], h3_t[u], h3T[u], h3T_t[u], 0, (0, 1))
            P.DMA("sp", ptl, pown[i * 128:(i + 1) * 128, :], [], t_ptl)
            act("activation", out=pb16, in_=ptl, func=AF.Copy, reads=[t_ptl], writes=[t_pb])
            for c in range(2):
                pe("transpose", psb(2)[:, c * 128:(c + 1) * 128], pb16[:, c * 128:(c + 1) * 128], c_identb,
                   reads=[t_pb, t_const], writes=[pst[2]])
            dv("tensor_copy", out=pT[u], in_=psb(2)[:, 0:256].rearrange("p (a b) -> p a b", a=2), reads=[pst[2]], writes=[pT_t[u]])

        def tail_back(i):
            u = i % 2
            for cg in range(4):
                bg = 4 + (itp[0] % 2) * 2
                sg_, sg_t_ = sgp[itp[0] % 2], sgp_t[itp[0] % 2]
                itp[0] += 1
                cs = slice(cg * 512, (cg + 1) * 512)
                for kc in range(16):
                    pe("matmul", psf(bg), lhsT=h3T[u][:, kc, :], rhs=Wpg[:, kc, cs], start=(kc == 0), stop=(kc == 15),
                       reads=[h3T_t[u], t_Wp], writes=[pst[bg]])
                for c in range(2):
                    pe("matmul", psf(bg + 1), lhsT=pT[u][:, c, :], rhs=Wpp[:, c, cs], start=(c == 0), stop=(c == 1),
                       reads=[pT_t[u], t_Wp], writes=[pst[bg + 1]])
                act("activation", out=sg_, in_=psf(bg), func=AF.Sigmoid, reads=[pst[bg]], writes=[sg_t_])
                dv("tensor_tensor", out=sg_, in0=sg_, in1=psf(bg + 1), op=ALU.mult, reads=[sg_t_, pst[bg + 1]], writes=[sg_t_])
                if cg % 2 == 0:
                    P.I("pool", "tensor_tensor", out=x3[u][:, cs], in0=sg_, in1=x2[u][:, cs], op=ALU.add, reads=[sg_t_, x2_t[u]], writes=[x3_t[u]])
                else:
                    dv("tensor_tensor", out=x3[u][:, cs], in0=sg_, in1=x2[u][:, cs], op=ALU.add, reads=[sg_t_, x2_t[u]], writes=[x3_t[u]])
            norm_rstd(x3[u], x3_t[u], h3[u], h3_t[u], ssd[u], ssd_t[u])
            dv("scalar_tensor_tensor", out=ot[u], in0=x3[u], scalar=ssd[u][:, 2:3], in1=gfin, op0=ALU.mult, op1=ALU.mult,
               reads=[x3_t[u], ssd_t[u], t_gfin], writes=[ot_t[u]])
            P.DMA("sp", y[i * 128:(i + 1) * 128, :], ot[u], [ot_t[u]], t_y, output=True, sem_tile=ot_t[u])

        tail_front(0)
        for i in range(NSLOT):
            if i + 1 < NSLOT:
                tail_front(i + 1)
            if full:
                tail_back(i)
        P.finish()
    return nc


def finish_debug(nc, P, dt, items, t_dep):
    for name, ap, shape, dtype in items:
        o = dt("dbg_" + name, shape, dtype, kind="ExternalOutput").ap()
        t = P.tile("dbg_" + name)
        P.DMA("sp", o, ap, list(t_dep), t, output=True)
    P.finish()
    return nc


def _core_blocks(i):
    blks = []
    for m in range(4):
        blks += [8 * m + i, 8 * m + 7 - i]
    return blks


def make_in_maps(inputs):
    x = np.asarray(inputs["x"], dtype=np.float32)
    p = np.asarray(inputs["p"], dtype=np.float32)[0]
    f = lambda k: np.ascontiguousarray(np.asarray(inputs[k], dtype=np.float32))
    shared = {
        "w_in": f("w_in")[0], "b_forget": f("b_forget").reshape(1, 8),
        "w_branch_fox": f("w_branch_fox")[0], "w_branch_sb": f("w_branch_sb")[0], "w_mix_out": f("w_mix_out")[0],
        "g_mix": f("g_mix").reshape(1, D), "g_ffn": f("g_ffn").reshape(1, D),
        "w_rout": np.ascontiguousarray(np.concatenate([f("w_group")[0], f("w_expert")[0]], axis=1)),
        "w_gate": f("w_gate")[0].reshape(NE * 512, 2048), "w_up": f("w_up")[0].reshape(NE * 512, 2048),
        "w_down": f("w_down")[0].reshape(NE * 512, 2048),
        "g_ple": f("g_ple").reshape(1, D), "w_ple_proj": f("w_ple_proj")[0], "w_ple_gate": f("w_ple_gate")[0],
        "g_final": f("g_final").reshape(1, D),
    }
    in_maps, rows = [], []
    for c in range(8):
        b, i = c // 4, c % 4
        blks = _core_blocks(i)
        idx = np.concatenate([np.arange(k * 128, (k + 1) * 128) for k in blks])
        rows.append((b, idx))
        m = dict(shared)
        m["xall"] = np.ascontiguousarray(x[b])
        m["xown"] = np.ascontiguousarray(x[b][idx])
        m["pown"] = np.ascontiguousarray(p[b][idx])
        m["blk128"] = np.ascontiguousarray(np.tile(np.asarray(blks, np.float32)[None, :] * 128.0, (128, 1)))
        in_maps.append(m)
    return in_maps, rows


_NC_CACHE = {}


def kernel(**inputs):
    in_maps, rows = make_in_maps(inputs)
    if "nc" not in _NC_CACHE:
        _NC_CACHE["nc"] = build_nc()
    nc = _NC_CACHE["nc"]
    names = set(a.memorylocations[0].name for a in nc.allocations
                if isinstance(a, mybir.MemoryLocationSet) and a.kind == "ExternalInput")
    in_maps = [{k: v for k, v in m.items() if k in names} for m in in_maps]
    res = run_bass_kernel_spmd(nc, in_maps, core_ids=list(range(8)))
    out = np.zeros((2, S, D), np.float32)
    for c in range(8):
        b, idx = rows[c]
        out[b, idx] = np.asarray(res.results[c]["y"])
    return out
```

```python
import numpy as np
from contextlib import ExitStack
import concourse.bass as bass
import concourse.mybir as mybir
from concourse.bass_utils import run_bass_kernel_spmd

F32 = mybir.dt.float32
BF16 = mybir.dt.bfloat16
I32 = mybir.dt.int32
U32 = mybir.dt.uint32
U8 = mybir.dt.uint8
AF = mybir.ActivationFunctionType
ALU = mybir.AluOpType
AX = mybir.AxisListType

D = 2048
S = 4096
NB = 32
NSLOT = 8
TOWN = 1024
DIN = 10248
NE = 32
DEXP = 512
NBLK = 48
EPS = 1e-6
SCALE = 128 ** -0.5
NK = [4, 8, 12, 16, 20, 24, 28, 32]


class Tl:
    __slots__ = ("name", "writers", "readers", "sem", "dma_cnt")

    def __init__(self, name):
        self.name = name
        self.writers = []
        self.readers = []
        self.sem = {}
        self.dma_cnt = {}


class Op:
    __slots__ = ("eng", "emit", "deps", "marked", "semval", "idx", "dma")

    def __init__(self, eng, emit):
        self.eng = eng
        self.emit = emit
        self.deps = []
        self.marked = False
        self.semval = 0
        self.idx = 0
        self.dma = None


class Prog:
    ENGS = ("pe", "act", "dve", "pool", "sp")

    def __init__(self, nc, stack):
        self.nc = nc
        self.stack = stack
        self.ops = {e: [] for e in self.ENGS}
        self.nops = 0
        self.esem = {e: stack.enter_context(nc.semaphore("es_" + e)) for e in self.ENGS}
        self.dma_tokens = []
        self.out_tokens = []
        self.nsem = 0

    def tile(self, name):
        return Tl(name)

    def _tsem(self, t, kind):
        if kind not in t.sem:
            t.sem[kind] = self.stack.enter_context(self.nc.semaphore("ds_%d" % self.nsem))
            t.dma_cnt[kind] = 0
            self.nsem += 1
        return t.sem[kind]

    def I(self, eng, fname, *args, reads=(), writes=(), **kw):
        return self.op(eng, (fname, args, kw), reads=reads, writes=writes)

    def DMA(self, eng, out, in_, reads, wt, output=False, sem_tile=None, **kw):
        return self.op(eng, ("dma_start", (), dict(out=out, in_=in_, **kw)), reads=reads, writes=[wt], dma_out=wt, is_output=output,
                       sem_tile=sem_tile)

    def op(self, eng, emit, reads=(), writes=(), dma_out=None, is_output=False, sem_tile=None):
        o = Op(eng, emit)
        o.idx = self.nops
        self.nops += 1
        deps = []
        for t in reads:
            deps.extend(t.writers)
        for t in writes:
            deps.extend(t.readers)
            if dma_out is not None:
                deps.extend(tk for tk in t.writers if tk[0] != "dma")
            else:
                deps.extend(t.writers)
        o.deps = deps
        if dma_out is not None:
            kind = "sw" if eng == "pool" else "hw"
            st = sem_tile if sem_tile is not None else dma_out
            sem = self._tsem(st, kind)
            st.dma_cnt[kind] += 16
            tok = ("dma", sem, st.dma_cnt[kind])
            o.dma = sem
            self.dma_tokens.append(tok)
            if is_output:
                self.out_tokens.append(tok)
        else:
            tok = ("op", o)
        for t in reads:
            t.readers.append(tok)
        for t in writes:
            if t.readers:
                t.writers = [tok]
                t.readers = []
            else:
                t.writers.append(tok)
                if len(t.writers) > 64:
                    t.writers = self._compact(t.writers)
        for t in reads:
            if len(t.readers) > 64:
                t.readers = self._compact(t.readers)
        self.ops[eng].append(o)
        return o

    @staticmethod
    def _compact(toks):
        best = {}
        for tk in toks:
            if tk[0] == "op":
                k = ("op", tk[1].eng)
                if k not in best or best[k][1].idx < tk[1].idx:
                    best[k] = tk
            else:
                k = ("dma", id(tk[1]))
                if k not in best or best[k][2] < tk[2]:
                    best[k] = tk
        return list(best.values())

    def barrier(self):
        toks = list(self.dma_tokens)
        for e in self.ENGS:
            if self.ops[e]:
                last = self.ops[e][-1]
                if last.dma is None and (last.emit is not None or last.deps):
                    toks.append(("op", last))
        self.dma_tokens = []
        for e in self.ENGS:
            o = Op(e, None)
            o.idx = self.nops
            self.nops += 1
            o.deps = [t for t in toks if not (t[0] == "op" and t[1].eng == e and e in ("pe", "sp"))]
            self.ops[e].append(o)

    def finish(self):
        o = Op("sp", None)
        o.deps = list(self.out_tokens)
        self.ops["sp"].append(o)
        for e in self.ENGS:
            for o in self.ops[e]:
                for tk in o.deps:
                    if tk[0] == "op":
                        p = tk[1]
                        if p.eng == "pe" and e == "pe":
                            continue
                        p.marked = True
        for e in self.ENGS:
            c = 0
            for o in self.ops[e]:
                if o.marked:
                    c += 1
                    o.semval = c
        nc = self.nc
        hmap = {"pe": nc.tensor, "act": nc.scalar, "dve": nc.vector, "pool": nc.gpsimd, "sp": nc.sync}
        with nc.Block() as block:
            def run(e, h):
                waited = {}
                regcache = {}
                for o in self.ops[e]:
                    need = {}
                    for tk in o.deps:
                        if tk[0] == "op":
                            p = tk[1]
                            if p.eng == "pe" and e == "pe":
                                continue
                            sem = self.esem[p.eng]
                            val = p.semval
                        else:
                            sem = tk[1]
                            val = tk[2]
                        k = id(sem)
                        if waited.get(k, (None, 0))[1] >= val:
                            continue
                        if k not in need or need[k][1] < val:
                            need[k] = (sem, val)
                    for k, (sem, val) in need.items():
                        h.wait_ge(sem, val)
                        waited[k] = (sem, val)
                    ins = None
                    if o.emit is not None:
                        fname, args, kw = o.emit
                        if "bounds_check" in kw and isinstance(kw["bounds_check"], int):
                            bv = kw["bounds_check"]
                            if bv not in regcache:
                                regcache[bv] = h.to_reg(bv)
                            kw = dict(kw, bounds_check=regcache[bv])
                        try:
                            ins = getattr(h, fname)(*args, **kw)
                        except Exception:
                            print("EMIT FAIL", e, fname, {k: (getattr(v, "shape", v)) for k, v in kw.items()})
                            raise
                    elif o.marked:
                        ins = h.nop()
                    if ins is not None:
                        if o.dma is not None:
                            ins.then_inc(o.dma, 16)
                        elif o.marked:
                            ins.then_inc(self.esem[e], 1)

            @block.tensor
            def _(h):
                run("pe", h)

            @block.scalar
            def _(h):
                run("act", h)

            @block.vector
            def _(h):
                run("dve", h)

            @block.gpsimd
            def _(h):
                run("pool", h)

            @block.sync
            def _(h):
                run("sp", h)


class Arena:
    def __init__(self, nc, nbytes):
        self.big = nc.alloc_sbuf_tensor("arena", [128, nbytes], U8)
        self.nbytes = nbytes
        self.off = 0

    def mark(self):
        return self.off

    def reset(self, off):
        self.off = off

    def alloc(self, shape_free, dtype, nparts=128):
        esz = {F32: 4, BF16: 2, I32: 4, U32: 4}[dtype]
        n = 1
        for s in shape_free:
            n *= s
        nb = (n * esz + 63) // 64 * 64
        assert self.off + nb <= self.nbytes, ("sbuf overflow", self.off, nb)
        ap = self.big[:, self.off:self.off + n * esz].bitcast(dtype)
        self.off += nb
        if len(shape_free) == 2:
            ap = ap.rearrange("p (a b) -> p a b", a=shape_free[0])
        elif len(shape_free) == 3:
            ap = ap.rearrange("p (a b c) -> p a b c", a=shape_free[0], b=shape_free[1])
        return ap


def build_nc(stage=99):
    nc = bass.Bass("TRN2", target_bir_lowering=False)
    dt = nc.dram_tensor
    xall = dt("xall", [S, D], F32, kind="ExternalInput").ap()
    xown = dt("xown", [TOWN, D], F32, kind="ExternalInput").ap()
    blk128 = dt("blk128", [128, NSLOT], F32, kind="ExternalInput").ap()
    w_in = dt("w_in", [D, DIN], F32, kind="ExternalInput").ap()
    b_forget = dt("b_forget", [1, 8], F32, kind="ExternalInput").ap()
    g_mix = dt("g_mix", [1, D], F32, kind="ExternalInput").ap()
    if stage >= 3:
        w_bfox = dt("w_branch_fox", [1024, D], F32, kind="ExternalInput").ap()
        w_bsb = dt("w_branch_sb", [1024, D], F32, kind="ExternalInput").ap()
        w_mix = dt("w_mix_out", [D, D], F32, kind="ExternalInput").ap()
    if stage >= 4:
        g_ffn = dt("g_ffn", [1, D], F32, kind="ExternalInput").ap()
        w_rout = dt("w_rout", [D, 36], F32, kind="ExternalInput").ap()
        w_gate = dt("w_gate", [NE * 512, 2048], F32, kind="ExternalInput").ap()
        w_up = dt("w_up", [NE * 512, 2048], F32, kind="ExternalInput").ap()
        w_down = dt("w_down", [NE * 512, 2048], F32, kind="ExternalInput").ap()
    if stage >= 5:
        pown = dt("pown", [TOWN, 256], F32, kind="ExternalInput").ap()
        g_ple = dt("g_ple", [1, D], F32, kind="ExternalInput").ap()
        w_pp = dt("w_ple_proj", [256, D], F32, kind="ExternalInput").ap()
        w_pg = dt("w_ple_gate", [D, D], F32, kind="ExternalInput").ap()
        g_final = dt("g_final", [1, D], F32, kind="ExternalInput").ap()
    y = dt("y", [TOWN, D], F32, kind="ExternalOutput").ap()

    hT_d = dt("hT_d", [16, 128, S], BF16).ap()
    kaT_d = dt("kaT_d", [8, 128, S], BF16).ap()
    kbT_d = dt("kbT_d", [8, 128, S], BF16).ap()
    va_d = dt("va_d", [S, 1024], BF16).ap()
    vb_d = dt("vb_d", [S, 1024], BF16).ap()
    x1_d = dt("x1_d", [TOWN, D], F32).ap()
    xb_d = dt("xb_d", [NBLK * 128, D], BF16).ap()
    yb_d = dt("yb_d", [NBLK * 128, D], F32).ap()
    wb_d = [dt("wb%d_d" % i, [NE * 512, 2048], BF16).ap() for i in range(3)]

    stack = ExitStack()
    with stack:
        P = Prog(nc, stack)
        A = Arena(nc, 207 * 1024)
        ps = [nc.alloc_psum_tensor("ps%d" % i, [128, 512], F32) for i in range(8)]
        pst = [P.tile("ps%d" % i) for i in range(8)]

        def psf(i):
            return ps[i][:]

        def psb(i):
            return ps[i][:].bitcast(BF16)

        def bcast_row(ap1d):
            return ap1d.partition_broadcast(128).rearrange("p a n -> p (a n)")

        c_dmat = A.alloc([128], F32)
        c_identb = A.alloc([128], BF16)
        c_identf = A.alloc([128], F32)
        c_trif = A.alloc([128], F32)
        c_strif = A.alloc([128], F32)
        c_onesf = A.alloc([128], F32)
        c_ntri = A.alloc([128], BF16)
        c_nones = A.alloc([128], BF16)
        c_ones = A.alloc([128], BF16)
        c_j128 = A.alloc([NBLK], F32)
        c_pidx = A.alloc([1], F32)
        c_blk = A.alloc([NSLOT], F32)
        c_onehot = A.alloc([NSLOT, NB], F32)
        c_bf = A.alloc([8], F32)
        c_eps = A.alloc([1], F32)
        core_mark = A.mark()
        m_le = A.alloc([32, 128], BF16)
        m_lt = A.alloc([32, 128], BF16)
        t_const = P.tile("const")
        TC = [t_const]
        P.I("pool", "iota", c_dmat, [[1, 128]], base=0, channel_multiplier=-1, allow_small_or_imprecise_dtypes=True, writes=TC)
        P.I("pool", "iota", c_j128, [[128, NBLK]], base=0, channel_multiplier=0, allow_small_or_imprecise_dtypes=True, writes=TC)
        P.I("pool", "iota", c_pidx, [[0, 1]], base=0, channel_multiplier=1, allow_small_or_imprecise_dtypes=True, writes=TC)
        P.DMA("sp", c_blk, blk128, [], t_const)
        P.DMA("sp", c_bf, bcast_row(b_forget), [], t_const)

        def dv(fname, *args, reads=(), writes=(), **kw):
            return P.I("dve", fname, *args, reads=reads, writes=writes, **kw)

        def act(fname, *args, reads=(), writes=(), **kw):
            return P.I("act", fname, *args, reads=reads, writes=writes, **kw)

        def pe(fname, *args, reads=(), writes=(), **kw):
            return P.I("pe", fname, *args, reads=reads, writes=writes, **kw)

        dv("tensor_single_scalar", c_identb, c_dmat, 0.0, ALU.is_equal, reads=TC, writes=TC)
        dv("tensor_single_scalar", c_identf, c_dmat, 0.0, ALU.is_equal, reads=TC, writes=TC)
        dv("tensor_single_scalar", c_trif, c_dmat, 0.0, ALU.is_ge, reads=TC, writes=TC)
        dv("tensor_single_scalar", c_strif, c_dmat, 0.0, ALU.is_gt, reads=TC, writes=TC)
        dv("memset", c_onesf, 1.0, writes=TC)
        dv("memset", c_ones, 1.0, writes=TC)
        dv("memset", c_nones, -1.0, writes=TC)
        dv("memset", c_eps, EPS, writes=TC)
        dv("tensor_scalar", c_ntri, c_dmat, 0.0, -1.0, ALU.is_le, ALU.mult, reads=TC, writes=TC)
        for k in range(NSLOT):
            dv("tensor_scalar", c_onehot[:, k, :], c_j128[:, 0:NB], c_blk[:, k:k + 1], None, ALU.is_equal, reads=TC, writes=TC)
        for g in range(NSLOT):
            for j in range(4):
                kb = 4 * g + j
                dv("tensor_scalar", m_le[:, 4 * g + j, :], c_dmat, c_blk[:, g:g + 1], 128.0 * kb, ALU.add, ALU.is_ge, reads=TC, writes=TC)
                dv("tensor_scalar", m_lt[:, 4 * g + j, :], c_dmat, c_blk[:, g:g + 1], 128.0 * kb + 1.0, ALU.add, ALU.is_ge, reads=TC, writes=TC)
        if stage == 0:
            return finish_debug(nc, P, dt, [("m_le", m_le, [128, 32, 128], BF16), ("onehot", c_onehot, [128, NSLOT, NB], F32),
                                            ("ntri", c_ntri, [128, 128], BF16), ("bf", c_bf, [128, 8], F32)], TC)
        t_xbd = P.tile("xb_d")
        t_xbz = P.tile("xb_z")

        gbc = A.alloc([D], F32)
        t_gbc = P.tile("gbc")
        P.DMA("sp", gbc, bcast_row(g_mix), [], t_gbc)
        t_wb = P.tile("wb_d")
        cv_pieces = [(mat, e) for e in range(NE) for mat in range(3)]
        cv_state = {"l": 0}
        cv_on = stage >= 4
        zero_b = A.alloc([D], BF16)
        dv("memset", zero_b, 0.0, writes=TC)

        def conv_step(k=1):
            if not cv_on:
                return
            wsrc = (w_gate, w_up, w_down)
            for _ in range(k):
                if cv_state["l"] >= len(cv_pieces):
                    return
                mat, e = cv_pieces[cv_state["l"]]
                P.DMA("pool", wb_d[mat][e * 512:(e + 1) * 512, :], wsrc[mat][e * 512:(e + 1) * 512, :], [], t_wb)
                cv_state["l"] += 1

        def conv_flush():
            conv_step(len(cv_pieces))

        c_all = A.alloc([8, NB], F32)
        cbias = A.alloc([8, NSLOT], F32)
        t_c = P.tile("c")
        persist_mark = A.mark()

        def norm_rstd(xst, xst_t, junk, junk_t, ss, ss_t):
            act("activation", out=junk, in_=xst, func=AF.Square, accum_out=ss[:, 0:1], reads=[xst_t], writes=[junk_t, ss_t])
            act("activation", out=ss[:, 1:2], in_=ss[:, 0:1], func=AF.Sqrt, bias=c_eps[:, 0:1], scale=1.0 / D,
                reads=[ss_t, t_const], writes=[ss_t])
            dv("reciprocal", ss[:, 2:3], ss[:, 1:2], reads=[ss_t], writes=[ss_t])

        def transpose16(src_bf, src_t, dst, dst_t, col0, banks, stride=1, evac=("act", "dve")):
            for half in range(2):
                b = banks[half]
                for q in range(8):
                    kc = half * 8 + q
                    pe("transpose", psb(b)[:, q * 128:(q + 1) * 128], src_bf[:, kc * 128:(kc + 1) * 128], c_identb,
                       reads=[src_t, t_const], writes=[pst[b]])
                o_ = dst[:, half * 8:(half + 1) * 8, col0:col0 + 128]
                i_ = psb(b).rearrange("p (a b) -> p a b", a=8)
                if evac[half] == "act":
                    act("activation", out=o_, in_=i_, func=AF.Copy, reads=[pst[b]], writes=[dst_t])
                else:
                    dv("tensor_copy", out=o_, in_=i_, reads=[pst[b]], writes=[dst_t])

        def norm_transpose(x_src_ap, gb, gb_t, xst, xst_t, xn, xn_t, ss, ss_t, hT_dst, hT_t, col0, banks):
            P.DMA("sp", xst, x_src_ap, [], xst_t)
            norm_rstd(xst, xst_t, xn, xn_t, ss, ss_t)
            dv("scalar_tensor_tensor", out=xn, in0=xst, scalar=ss[:, 2:3], in1=gb, op0=ALU.mult, op1=ALU.mult,
               reads=[xst_t, ss_t, gb_t], writes=[xn_t])
            transpose16(xn, xn_t, hT_dst, hT_t, col0, banks)

        kv_tiles = []
        t_hd = P.tile("hd")
        for pss in range(2):
            A.reset(persist_mark)
            W = A.alloc([16, 2048], BF16)
            t_W = P.tile("W")
            c0 = 1024 if pss == 0 else 4104
            for kc4 in range(4):
                P.DMA("pool", W[:, kc4 * 4:(kc4 + 1) * 4, :],
                      w_in[kc4 * 512:(kc4 + 1) * 512, c0:c0 + 2048].rearrange("(kc p) n -> p kc n", p=128), [], t_W)
            if pss == 0:
                Wf32 = A.alloc([16, 8], F32)
                Wf = A.alloc([16, 8], BF16)
                fl_all = A.alloc([NB * 8], F32)
                t_fl = P.tile("fl")
                t_Wf = P.tile("Wf")
                P.DMA("sp", Wf32, w_in[:, 3072:3080].rearrange("(kc p) n -> p kc n", p=128), [], t_Wf)
                dv("tensor_copy", out=Wf, in_=Wf32, reads=[t_Wf], writes=[t_Wf])
            hT = [A.alloc([16, 512], BF16) for _ in range(2)]
            hT_t = [P.tile("hT%d" % i) for i in range(2)]
            kst = [A.alloc([512], BF16) for _ in range(4)]
            kst_t = [P.tile("kst%d" % i) for i in range(4)]
            vst = [A.alloc([1024], BF16) for _ in range(3)]
            vst_t = [P.tile("vst%d" % i) for i in range(3)]
            kT_d = kaT_d if pss == 0 else kbT_d
            v_d = va_d if pss == 0 else vb_d
            t_kd = P.tile("kd%d" % pss)
            t_vd = P.tile("vd%d" % pss)
            kv_tiles.extend([t_kd, t_vd])
            xn4 = [A.alloc([D], BF16) for _ in range(4)] if pss == 0 else None
            xn4_t = [P.tile("xn4_%d" % i) for i in range(4)]
            ss4 = [A.alloc([4], F32) for _ in range(4)] if pss == 0 else None
            ss4_t = [P.tile("ss4_%d" % i) for i in range(4)]

            xst4 = [A.alloc([D], F32) for _ in range(4)] if pss == 0 else None
            xst4_t = [P.tile("xst4_%d" % i) for i in range(4)]

            def load_group(G):
                for tt in range(4):
                    r0 = (G * 4 + tt) * 128
                    P.DMA("sp", xst4[tt], xall[r0:r0 + 128, :], [], xst4_t[tt])

            def norm_tile(G, tt):
                norm_rstd(xst4[tt], xst4_t[tt], xn4[tt], xn4_t[tt], ss4[tt], ss4_t[tt])
                dv("scalar_tensor_tensor", out=xn4[tt], in0=xst4[tt], scalar=ss4[tt][:, 2:3], in1=gbc, op0=ALU.mult, op1=ALU.mult,
                   reads=[xst4_t[tt], ss4_t[tt], t_gbc], writes=[xn4_t[tt]])

            def norm_group(G):
                load_group(G)
                for tt in range(4):
                    norm_tile(G, tt)

            def transpose_group(G):
                for tt in range(4):
                    transpose16(xn4[tt], xn4_t[tt], hT[G % 2], hT_t[G % 2], tt * 128, (0, 1))

            if pss == 0:
                norm_group(0)
                transpose_group(0)
            for G in range(8):
                hb = hT[G % 2]
                hbt = hT_t[G % 2]
                if pss == 0:
                    if G + 1 < 8:
                        load_group(G + 1)
                    P.DMA("sp", hT_d[:, :, G * 512:(G + 1) * 512].rearrange("kc p t -> p kc t"), hb, [hbt], t_hd, sem_tile=hbt)
                else:
                    P.DMA("sp", hb, hT_d[:, :, G * 512:(G + 1) * 512].rearrange("kc p t -> p kc t"), [t_hd], hbt)
                for hd in range(8):
                    b = (2, 3, 7)[(G * 8 + hd) % 3]
                    for kc in range(16):
                        pe("matmul", psf(b), lhsT=W[:, kc, hd * 128:(hd + 1) * 128], rhs=hb[:, kc, :], start=(kc == 0), stop=(kc == 15),
                           reads=[t_W, hbt], writes=[pst[b]])
                    ks = kst[(G * 8 + hd) % 4]
                    kst_ = kst_t[(G * 8 + hd) % 4]
                    dv("tensor_copy", out=ks, in_=psf(b), reads=[pst[b]], writes=[kst_])
                    P.DMA("sp", kT_d[hd, :, G * 512:(G + 1) * 512], ks, [kst_], t_kd, sem_tile=kst_)
                    if pss == 1 and hd % 4 == 0:
                        conv_step(1)
                    if pss == 0 and G + 1 < 8 and hd % 2 == 1:
                        norm_tile(G + 1, hd // 2)
                if pss == 0 and G + 1 < 8:
                    transpose_group(G + 1)
                if pss == 0 and G == 1 and stage >= 4:
                    for blk in range(NBLK):
                        P.DMA("sp", xb_d[blk * 128:(blk + 1) * 128, :], zero_b, TC, t_xbz)
                for tt in range(4):
                    vs = vst[(G * 4 + tt) % 3]
                    vs_t = vst_t[(G * 4 + tt) % 3]
                    for cg in range(2):
                        b = 4 + cg
                        for kc in range(16):
                            pe("matmul", psf(b), lhsT=hb[:, kc, tt * 128:(tt + 1) * 128], rhs=W[:, kc, 1024 + cg * 512:1024 + (cg + 1) * 512],
                               start=(kc == 0), stop=(kc == 15), reads=[t_W, hbt], writes=[pst[b]])
                        act("activation", out=vs[:, cg * 512:(cg + 1) * 512], in_=psf(b), func=AF.Copy, reads=[pst[b]], writes=[vs_t])
                    r0 = (G * 4 + tt) * 128
                    P.DMA("sp", v_d[r0:r0 + 128, :], vs, [vs_t], t_vd, sem_tile=vs_t)
                    if pss == 0:
                        for kc in range(16):
                            pe("matmul", psf(6)[:, 0:8], lhsT=hb[:, kc, tt * 128:(tt + 1) * 128], rhs=Wf[:, kc, :],
                               start=(kc == 0), stop=(kc == 15), reads=[t_Wf, hbt], writes=[pst[6]])
                        j = G * 4 + tt
                        dv("tensor_tensor", out=fl_all[:, j * 8:(j + 1) * 8], in0=psf(6)[:, 0:8], in1=c_bf, op=ALU.add,
                           reads=[pst[6], t_const], writes=[t_fl])
            if pss == 0:
                act("activation", out=fl_all, in_=fl_all, func=AF.Sigmoid, reads=[t_fl], writes=[t_fl])
                act("activation", out=fl_all, in_=fl_all, func=AF.Ln, reads=[t_fl], writes=[t_fl])
                pe("matmul", psf(6)[:, 0:256], lhsT=c_trif, rhs=fl_all, start=True, stop=True, reads=[t_fl, t_const], writes=[pst[6]])
                pe("matmul", psf(7)[:, 0:256], lhsT=c_onesf, rhs=fl_all, start=True, stop=True, reads=[t_fl, t_const], writes=[pst[7]])
                ctot = A.alloc([8, NB], F32)
                cpre = A.alloc([8, NB], F32)
                cm = A.alloc([8, NB], F32)
                ctmp = A.alloc([8, NB], F32)
                dv("tensor_copy", out=ctot, in_=psf(7)[:, 0:256].rearrange("p (j h) -> p h j", h=8), reads=[pst[7]], writes=[t_c])
                for hd in range(8):
                    dv("tensor_tensor_scan", out=cpre[:, hd, :], data0=c_onesf[:, 0:NB], data1=ctot[:, hd, :], initial=0.0,
                       op0=ALU.mult, op1=ALU.add, reads=[t_c, t_const], writes=[t_c])
                dv("tensor_sub", out=cpre, in0=cpre, in1=ctot, reads=[t_c], writes=[t_c])
                dv("tensor_tensor", out=c_all, in0=psf(6)[:, 0:256].rearrange("p (j h) -> p h j", h=8), in1=cpre, op=ALU.add,
                   reads=[pst[6], t_c], writes=[t_c])
                dv("scalar_tensor_tensor", out=cm, in0=ctot, scalar=0.5, in1=cpre, op0=ALU.mult, op1=ALU.add, reads=[t_c], writes=[t_c])
                for k in range(NSLOT):
                    dv("tensor_tensor", out=ctmp, in0=cm, in1=c_onehot[:, k:k + 1, :].broadcast_to([128, 8, NB]), op=ALU.mult,
                       reads=[t_c, t_const], writes=[t_c])
                    dv("tensor_reduce", out=cbias[:, :, k], in_=ctmp, axis=AX.X, op=ALU.add, reads=[t_c], writes=[t_c])
            P.barrier()

        if stage == 1:
            return finish_debug(nc, P, dt, [("c_all", c_all, [128, 8, NB], F32), ("cbias", cbias, [128, 8, NSLOT], F32),
                                            ("m_le", m_le, [128, 32, 128], BF16),
                                            ("kaT", kaT_d[0], [128, S], BF16), ("kbT", kbT_d[7], [128, S], BF16),
                                            ("va", va_d[0:512, :], [512, 1024], BF16),
                                            ("vb", vb_d[3584:4096, :], [512, 1024], BF16)], [t_const, t_c] + kv_tiles)

        A.reset(persist_mark)
        hTo = A.alloc([16, TOWN], BF16)
        t_hTo = P.tile("hTo")
        oaT = A.alloc([8, TOWN], BF16)
        obT = A.alloc([8, TOWN], BF16)
        t_oa = P.tile("oaT")
        t_ob = P.tile("obT")
        mid_mark = A.mark()
        QT = A.alloc([16, TOWN], BF16)
        t_QT = P.tile("QT")
        att_mark = A.mark()
        Wq = A.alloc([16, 1024], BF16)
        t_Wq = P.tile("Wq")
        xst = [A.alloc([D], F32) for _ in range(2)]
        xst_t = [P.tile("xst%d" % i) for i in range(2)]
        xn = [A.alloc([D], BF16) for _ in range(2)]
        xn_t = [P.tile("xn%d" % i) for i in range(2)]
        ssb = [A.alloc([4], F32) for _ in range(2)]
        ss_t = [P.tile("ss%d" % i) for i in range(2)]
        for i in range(8):
            norm_transpose(xown[i * 128:(i + 1) * 128, :], gbc, t_gbc, xst[i % 2], xst_t[i % 2], xn[i % 2], xn_t[i % 2], ssb[i % 2], ss_t[i % 2],
                           hTo, t_hTo, i * 128, (0, 1))
        for part, c0 in ((0, 0), (1, 3080)):
            for kc8 in range(2):
                P.DMA("pool", Wq[:, kc8 * 8:(kc8 + 1) * 8, :],
                      w_in[kc8 * 1024:(kc8 + 1) * 1024, c0:c0 + 1024].rearrange("(kc p) n -> p kc n", p=128), [], t_Wq)
            for hh in range(8):
                hd = part * 8 + hh
                for half in range(2):
                    b = 2 + (hd * 2 + half) % 4
                    for kc in range(16):
                        pe("matmul", psf(b), lhsT=Wq[:, kc, hh * 128:(hh + 1) * 128], rhs=hTo[:, kc, half * 512:(half + 1) * 512],
                           start=(kc == 0), stop=(kc == 15), reads=[t_Wq, t_hTo], writes=[pst[b]])
                    if half == 0 and hd % 2 == 0:
                        conv_step(1)
                    if half == 0:
                        act("activation", out=QT[:, hd, 0:512], in_=psf(b), func=AF.Copy, scale=SCALE, reads=[pst[b]], writes=[t_QT])
                    else:
                        dv("tensor_scalar", QT[:, hd, 512:1024], psf(b), SCALE, None, ALU.mult, reads=[pst[b]], writes=[t_QT])
        P.barrier()

        A.reset(att_mark)
        KT = [A.alloc([S], BF16) for _ in range(2)]
        KT_t = [P.tile("KT%d" % i) for i in range(2)]
        Vt = [A.alloc([NB, 128], BF16) for _ in range(2)]
        V_t = [P.tile("V%d" % i) for i in range(2)]
        Pt = [A.alloc([TOWN], BF16) for _ in range(2)]
        Pt_t = [P.tile("Pt%d" % i) for i in range(2)]
        clampt = A.alloc([128], F32)
        t_clamp = P.tile("clamp")
        Lr = A.alloc([TOWN], F32)
        t_Lr = P.tile("Lr")
        e1 = [A.alloc([TOWN], F32) for _ in range(2)]
        e1_t = [P.tile("e1%d" % i) for i in range(2)]
        spb = [A.alloc([TOWN], BF16) for _ in range(2)]
        sp_t = [P.tile("sp%d" % i) for i in range(2)]
        Nsum = A.alloc([TOWN], BF16)
        t_Ns = P.tile("Nsum")

        def load_kv(hd, kT_d, v_d, slot):
            P.DMA("sp", KT[slot], kT_d[hd], list(kv_tiles), KT_t[slot])
            P.DMA("sp", Vt[slot], v_d[:, hd * 128:(hd + 1) * 128].rearrange("(kb p) d -> p kb d", p=128), list(kv_tiles), V_t[slot])

        def col_splits(c0):
            out = []
            if c0 < 512:
                out.append((0, c0, 512))
                out.append((1, 512, 1024))
            else:
                out.append((1, c0, 1024))
            return out

        negc = [A.alloc([NB], F32) for _ in range(2)]
        negc_t = [P.tile("negc%d" % i) for i in range(2)]
        clampb = [clampt, A.alloc([128], F32)]
        clamp_t = [t_clamp, P.tile("clamp1")]

        def load_head(H):
            if H >= 16:
                return
            if H < 8:
                load_kv(H, kaT_d, va_d, H % 2)
            else:
                load_kv(H - 8, kbT_d, vb_d, H % 2)

        load_head(0)
        load_head(1)

        fox_iters = [(hd, kb) for hd in range(8) for kb in range(NB)]

        Pacc = A.alloc([TOWN], F32)
        t_Pacc = P.tile("Pacc")

        crow = [A.alloc([TOWN], BF16) for _ in range(2)]
        crow_t = [P.tile("crow%d" % i) for i in range(2)]

        def fox_A(j):
            hd, kb = fox_iters[j]
            sl = hd % 2
            if kb == 0:
                dv("tensor_scalar", negc[sl], c_all[:, hd, :], -1.0, None, ALU.mult, reads=[t_c], writes=[negc_t[sl]])
                dv("tensor_copy", out=crow[sl][0:1, :].rearrange("p (k c) -> p k c", k=NSLOT),
                   in_=cbias[0:1, hd, :].rearrange("p (k o) -> p k o", o=1).broadcast_to([1, NSLOT, 128]),
                   reads=[t_c], writes=[crow_t[sl]])
            c0 = (kb // 4) * 128
            sb = (j % 3) * 2
            for (bh, lo, hi) in col_splits(c0):
                dst = psf(sb + bh)[:, lo - bh * 512:hi - bh * 512]
                pe("matmul", dst, lhsT=KT[sl][:, kb * 128:(kb + 1) * 128], rhs=QT[:, hd, lo:hi],
                   start=True, stop=False, skip_group_check=True, reads=[KT_t[sl], t_QT], writes=[pst[sb + bh]])
                pe("matmul", dst, lhsT=c_ones[0:1, :], rhs=crow[sl][0:1, lo:hi],
                   start=False, stop=True, skip_group_check=True, reads=[t_const, crow_t[sl]], writes=[pst[sb + bh]])

        def fox_E1(j):
            hd, kb = fox_iters[j]
            sl = hd % 2
            g = kb // 4
            c0 = g * 128
            sb = (j % 3) * 2
            pt, ptt = Pt[j % 2], Pt_t[j % 2]
            cl, clt = clampb[j % 2], clamp_t[j % 2]
            bh = g // 4
            src = psf(sb + bh)[:, (g % 4) * 128:(g % 4 + 1) * 128]
            dv("tensor_scalar", cl, src, negc[sl][:, kb:kb + 1], 60.0, ALU.add, ALU.min, reads=[pst[sb + bh], negc_t[sl]], writes=[clt])
            act("activation", out=pt[:, c0:c0 + 128], in_=cl, func=AF.Exp, reads=[clt], writes=[ptt])
            if c0 + 128 < TOWN:
                for (bh2, lo, hi) in col_splits(c0 + 128):
                    act("activation", out=pt[:, lo:hi], in_=psf(sb + bh2)[:, lo - bh2 * 512:hi - bh2 * 512], func=AF.Exp,
                        bias=negc[sl][:, kb:kb + 1], reads=[pst[sb + bh2], negc_t[sl]], writes=[ptt])

        def fox_E2(j):
            hd, kb = fox_iters[j]
            c0 = (kb // 4) * 128
            pt, ptt = Pt[j % 2], Pt_t[j % 2]
            dv("tensor_mul", out=pt[:, c0:c0 + 128], in0=pt[:, c0:c0 + 128], in1=m_le[:, kb, :], reads=[ptt, t_const], writes=[ptt])
            if kb == 0:
                dv("tensor_copy", out=Pacc[:, 0:512], in_=pt[:, 0:512], reads=[ptt], writes=[t_Pacc])
                P.I("pool", "tensor_copy", out=Pacc[:, 512:TOWN], in_=pt[:, 512:TOWN], reads=[ptt], writes=[t_Pacc])
            else:
                if c0 < 512:
                    dv("tensor_tensor", out=Pacc[:, c0:512], in0=Pacc[:, c0:512], in1=pt[:, c0:512], op=ALU.add, reads=[ptt, t_Pacc], writes=[t_Pacc])
                lo2 = max(c0, 512)
                P.I("pool", "tensor_tensor", out=Pacc[:, lo2:TOWN], in0=Pacc[:, lo2:TOWN], in1=pt[:, lo2:TOWN], op=ALU.add,
                    reads=[ptt, t_Pacc], writes=[t_Pacc])

        def fox_C(j):
            hd, kb = fox_iters[j]
            sl = hd % 2
            c0 = (kb // 4) * 128
            pt, ptt = Pt[j % 2], Pt_t[j % 2]
            for (bh, lo, hi) in col_splits(c0):
                pe("matmul", psf(6 + bh)[:, lo - bh * 512:hi - bh * 512], lhsT=Vt[sl][:, kb, :], rhs=pt[:, lo:hi],
                   start=(kb == 0), stop=(kb == NB - 1), skip_group_check=True, reads=[V_t[sl], ptt], writes=[pst[6 + bh]])
            if kb == NB - 1:
                for bh in range(2):
                    lb = ((j + 1) % 3) * 2 + bh
                    pe("matmul", psf(lb), lhsT=c_onesf, rhs=Pacc[:, bh * 512:(bh + 1) * 512], start=True, stop=True,
                       reads=[t_const, t_Pacc], writes=[pst[lb]])
                    dv("reciprocal", Lr[:, bh * 512:(bh + 1) * 512], psf(lb), reads=[pst[lb]], writes=[t_Lr])
                    dv("tensor_tensor", out=oaT[:, hd, bh * 512:(bh + 1) * 512], in0=psf(6 + bh), in1=Lr[:, bh * 512:(bh + 1) * 512], op=ALU.mult,
                       reads=[pst[6 + bh], t_Lr], writes=[t_oa])
                load_head(hd + 2)

        nfx = len(fox_iters)
        fox_A(0)
        fox_A(1)
        fox_E1(0)
        for j in range(nfx):
            if j + 2 < nfx:
                fox_A(j + 2)
            if j + 1 < nfx:
                fox_E1(j + 1)
            fox_E2(j)
            fox_C(j)
            if j % 8 == 0:
                conv_step(1)

        sb_iters = [(hd, kb) for hd in range(8) for kb in range(NB - 1, -1, -1)]
        first_o = {}

        def sb_A(j):
            hd, kb = sb_iters[j]
            sl = hd % 2
            c0 = (kb // 4) * 128
            sb = (j % 3) * 2
            for (bh, lo, hi) in col_splits(c0):
                pe("matmul", psf(sb + bh)[:, lo - bh * 512:hi - bh * 512], lhsT=KT[sl][:, kb * 128:(kb + 1) * 128], rhs=QT[:, 8 + hd, lo:hi],
                   start=True, stop=False, skip_group_check=True, reads=[KT_t[sl], t_QT], writes=[pst[sb + bh]])

        def sb_S(j):
            hd, kb = sb_iters[j]
            c0 = (kb // 4) * 128
            sb = (j % 3) * 2
            ef, eft = e1[j % 2], e1_t[j % 2]
            spk, spt = spb[j % 2], sp_t[j % 2]
            for (bh, lo, hi) in col_splits(c0):
                act("activation", out=ef[:, lo:hi], in_=psf(sb + bh)[:, lo - bh * 512:hi - bh * 512], func=AF.Exp,
                    reads=[pst[sb + bh]], writes=[eft])
            act("activation", out=spk[:, c0:1024], in_=ef[:, c0:1024], func=AF.Ln, bias=1.0, reads=[eft], writes=[spt])
            dv("tensor_mul", out=spk[:, c0:c0 + 128], in0=spk[:, c0:c0 + 128], in1=m_lt[:, kb, :], reads=[spt, t_const], writes=[spt])

        def sb_B(j):
            hd, kb = sb_iters[j]
            c0 = (kb // 4) * 128
            sb = (j % 3) * 2
            spk, spt = spb[j % 2], sp_t[j % 2]
            if kb == NB - 1:
                P.I("pool", "memset", Nsum, 0.0, writes=[t_Ns])
            for (bh, lo, hi) in col_splits(c0):
                dst = psf(sb + bh)[:, lo - bh * 512:hi - bh * 512]
                pe("matmul", dst, lhsT=c_ntri, rhs=spk[:, lo:hi], start=False, stop=False, skip_group_check=True,
                   reads=[t_const, spt], writes=[pst[sb + bh]])
                pe("matmul", dst, lhsT=c_nones, rhs=Nsum[:, lo:hi], start=False, stop=True, skip_group_check=True,
                   reads=[t_const, t_Ns], writes=[pst[sb + bh]])

        def sb_X(j):
            hd, kb = sb_iters[j]
            c0 = (kb // 4) * 128
            sb = (j % 3) * 2
            spk, spt = spb[j % 2], sp_t[j % 2]
            at, att = Pt[j % 2], Pt_t[j % 2]
            for (bh, lo, hi) in col_splits(c0):
                act("activation", out=at[:, lo:hi], in_=psf(sb + bh)[:, lo - bh * 512:hi - bh * 512], func=AF.Exp,
                    reads=[pst[sb + bh]], writes=[att])
            dv("tensor_mul", out=at[:, c0:c0 + 128], in0=at[:, c0:c0 + 128], in1=m_lt[:, kb, :], reads=[att, t_const], writes=[att])
            P.I("pool", "tensor_tensor", out=Nsum[:, c0:1024], in0=Nsum[:, c0:1024], in1=spk[:, c0:1024], op=ALU.add,
                reads=[t_Ns, spt], writes=[t_Ns])

        def sb_C(j):
            hd, kb = sb_iters[j]
            sl = hd % 2
            c0 = (kb // 4) * 128
            at, att = Pt[j % 2], Pt_t[j % 2]
            for (bh, lo, hi) in col_splits(c0):
                fo = first_o.get((hd, bh), True)
                pe("matmul", psf(6 + bh)[:, lo - bh * 512:hi - bh * 512], lhsT=Vt[sl][:, kb, :], rhs=at[:, lo:hi],
                   start=fo, stop=(kb == 0), skip_group_check=True, reads=[V_t[sl], att], writes=[pst[6 + bh]])
                first_o[(hd, bh)] = False
            if kb == 0:
                act("activation", out=obT[:, hd, 0:512], in_=psf(6), func=AF.Copy, reads=[pst[6]], writes=[t_ob])
                dv("tensor_copy", out=obT[:, hd, 512:1024], in_=psf(7), reads=[pst[7]], writes=[t_ob])
                load_head(8 + hd + 2)

        nsb = len(sb_iters)
        sb_A(0)
        sb_S(0)
        sb_A(1)
        for j in range(nsb):
            sb_B(j)
            if j + 2 < nsb:
                sb_A(j + 2)
            if j + 1 < nsb:
                sb_S(j + 1)
            if j >= 1:
                sb_C(j - 1)
            sb_X(j)
            if j % 8 == 0:
                conv_step(1)
        sb_C(nsb - 1)
        P.barrier()
        if stage == 2:
            return finish_debug(nc, P, dt, [("oaT", oaT, [128, 8, TOWN], BF16), ("obT", obT, [128, 8, TOWN], BF16)], [t_oa, t_ob])

        A.reset(mid_mark)
        mixedT = A.alloc([16, TOWN], BF16)
        t_mx = P.tile("mixedT")
        p6_mark = A.mark()
        Wga = [A.alloc([16, 256], BF16) for _ in range(2)]
        Wgb = [A.alloc([16, 256], BF16) for _ in range(2)]
        Wfx = [A.alloc([8, 256], BF16) for _ in range(2)]
        Wsb = [A.alloc([8, 256], BF16) for _ in range(2)]
        Wg_t = [P.tile("Wg6_%d" % i) for i in range(2)]
        sa = [A.alloc([512], F32) for _ in range(2)]
        sb_ = [A.alloc([512], F32) for _ in range(2)]
        sg_t = [P.tile("sg%d" % i) for i in range(2)]

        def load_w6(ng, s):
            P.DMA("pool", Wga[s], w_in[:, 6152 + ng * 256:6152 + (ng + 1) * 256].rearrange("(kc p) n -> p kc n", p=128), [], Wg_t[s])
            P.DMA("pool", Wgb[s], w_in[:, 8200 + ng * 256:8200 + (ng + 1) * 256].rearrange("(kc p) n -> p kc n", p=128), [], Wg_t[s])
            P.DMA("pool", Wfx[s], w_bfox[:, ng * 256:(ng + 1) * 256].rearrange("(kc p) n -> p kc n", p=128), [], Wg_t[s])
            P.DMA("pool", Wsb[s], w_bsb[:, ng * 256:(ng + 1) * 256].rearrange("(kc p) n -> p kc n", p=128), [], Wg_t[s])

        load_w6(0, 0)
        it6 = 0
        for ng in range(8):
            s = ng % 2
            if ng + 1 < 8:
                load_w6(ng + 1, (ng + 1) % 2)
            for nsub in range(2):
                nch = ng * 2 + nsub
                for half in range(2):
                    pb = (it6 % 2) * 4
                    sa_, sbb, sgt = sa[it6 % 2], sb_[it6 % 2], sg_t[it6 % 2]
                    it6 += 1
                    cs = slice(half * 512, (half + 1) * 512)
                    ns = slice(nsub * 128, (nsub + 1) * 128)
                    for kc in range(16):
                        pe("matmul", psf(pb), lhsT=Wga[s][:, kc, ns], rhs=hTo[:, kc, cs], start=(kc == 0), stop=(kc == 15),
                           reads=[Wg_t[s], t_hTo], writes=[pst[pb]])
                    for kc in range(16):
                        pe("matmul", psf(pb + 1), lhsT=Wgb[s][:, kc, ns], rhs=hTo[:, kc, cs], start=(kc == 0), stop=(kc == 15),
                           reads=[Wg_t[s], t_hTo], writes=[pst[pb + 1]])
                    for hc in range(8):
                        pe("matmul", psf(pb + 2), lhsT=Wfx[s][:, hc, ns], rhs=oaT[:, hc, cs], start=(hc == 0), stop=(hc == 7),
                           reads=[Wg_t[s], t_oa], writes=[pst[pb + 2]])
                    for hc in range(8):
                        pe("matmul", psf(pb + 3), lhsT=Wsb[s][:, hc, ns], rhs=obT[:, hc, cs], start=(hc == 0), stop=(hc == 7),
                           reads=[Wg_t[s], t_ob], writes=[pst[pb + 3]])
                    act("activation", out=sa_, in_=psf(pb), func=AF.Sigmoid, reads=[pst[pb]], writes=[sgt])
                    act("activation", out=sbb, in_=psf(pb + 1), func=AF.Sigmoid, reads=[pst[pb + 1]], writes=[sgt])
                    dv("tensor_tensor", out=sa_, in0=sa_, in1=psf(pb + 2), op=ALU.mult, reads=[sgt, pst[pb + 2]], writes=[sgt])
                    dv("tensor_tensor", out=sbb, in0=sbb, in1=psf(pb + 3), op=ALU.mult, reads=[sgt, pst[pb + 3]], writes=[sgt])
                    P.I("pool", "tensor_tensor", out=mixedT[:, nch, cs], in0=sa_, in1=sbb, op=ALU.add, reads=[sgt], writes=[t_mx])
                    if it6 % 4 == 0:
                        conv_step(1)
        P.barrier()
        A.reset(p6_mark)
        Wm = [A.alloc([16, 512], BF16) for _ in range(2)]
        Wm_t = [P.tile("Wm%d" % i) for i in range(2)]
        xo = [A.alloc([512], F32) for _ in range(3)]
        xo_t = [P.tile("xo%d" % i) for i in range(3)]
        x1s = [A.alloc([512], F32) for _ in range(2)]
        x1s_t = [P.tile("x1s%d" % i) for i in range(2)]
        t_x1d = P.tile("x1_d")
        P.DMA("pool", Wm[0], w_mix[:, 0:512].rearrange("(kc p) n -> p kc n", p=128), [], Wm_t[0])
        mo_iters = [(cg, i) for cg in range(4) for i in range(8)]

        def xo_load(n):
            cg, i = mo_iters[n]
            P.DMA("sp", xo[n % 3], xown[i * 128:(i + 1) * 128, cg * 512:(cg + 1) * 512], [], xo_t[n % 3])

        xo_load(0)
        xo_load(1)
        for n, (cg, i) in enumerate(mo_iters):
            s_ = cg % 2
            if i == 0 and cg + 1 < 4:
                P.DMA("pool", Wm[(cg + 1) % 2], w_mix[:, (cg + 1) * 512:(cg + 2) * 512].rearrange("(kc p) n -> p kc n", p=128), [], Wm_t[(cg + 1) % 2])
            if n + 2 < len(mo_iters):
                xo_load(n + 2)
            b = n % 4
            u = n % 2
            for kc in range(16):
                pe("matmul", psf(b), lhsT=mixedT[:, kc, i * 128:(i + 1) * 128], rhs=Wm[s_][:, kc, :], start=(kc == 0), stop=(kc == 15),
                   reads=[t_mx, Wm_t[s_]], writes=[pst[b]])
            dv("tensor_tensor", out=x1s[u], in0=psf(b), in1=xo[n % 3], op=ALU.add, reads=[pst[b], xo_t[n % 3]], writes=[x1s_t[u]])
            P.DMA("sp", x1_d[i * 128:(i + 1) * 128, cg * 512:(cg + 1) * 512], x1s[u], [x1s_t[u]], t_x1d, sem_tile=x1s_t[u])
        conv_flush()
        P.barrier()
        if stage == 3:
            return finish_debug(nc, P, dt, [("x1", x1_d, [TOWN, D], F32)], [t_x1d])
        A.reset(core_mark)
        Wr = A.alloc([16, 36], F32)
        t_Wr = P.tile("Wr")
        P.DMA("sp", Wr, w_rout.rearrange("(kc p) n -> p kc n", p=128), [], t_Wr)
        Eall = A.alloc([NSLOT, 2, 32], F32)
        gall = A.alloc([NSLOT, 2], F32)
        Esum = A.alloc([NSLOT, 32], F32)
        Call = A.alloc([NSLOT, 32], F32)
        dest_f = A.alloc([NSLOT, 2], F32)
        dest_i = A.alloc([NSLOT, 2], I32)
        idxw_f = A.alloc([NBLK, 4], F32)
        idxw_i = A.alloc([NBLK, 4], I32)
        t_rt = P.tile("route")
        moe_mark = A.mark()
        gbc = A.alloc([D], F32)
        t_gbc = P.tile("gbc7")
        P.DMA("sp", gbc, bcast_row(g_ffn), [], t_gbc)
        h2b = A.alloc([NSLOT, D], BF16)
        t_h2b = P.tile("h2b")
        xst = [A.alloc([D], F32) for _ in range(2)]
        xst_t = [P.tile("x1t%d" % i) for i in range(2)]
        junk = A.alloc([D], BF16)
        junk_t = P.tile("junk")
        ssb = [A.alloc([4], F32) for _ in range(2)]
        ss_t = [P.tile("ss%d" % i) for i in range(2)]
        h2f = A.alloc([D], F32)
        t_h2f = P.tile("h2f")
        h2fT = A.alloc([16, 128], F32)
        t_h2fT = P.tile("h2fT")
        lg = A.alloc([36], F32)
        rs = A.alloc([16], F32)
        goh = A.alloc([4], F32)
        j4 = A.alloc([4], F32)
        t48 = A.alloc([4, 8], F32)
        esel = A.alloc([8], F32)
        v8 = A.alloc([8], F32)
        oh1 = A.alloc([8], F32)
        oh2 = A.alloc([8], F32)
        t_r = P.tile("rtmp")
        TR = [t_r]
        for i in range(NSLOT):
            u = i % 2
            P.DMA("sp", xst[u], x1_d[i * 128:(i + 1) * 128, :], [t_x1d], xst_t[u])
            norm_rstd(xst[u], xst_t[u], junk, junk_t, ssb[u], ss_t[u])
            dv("scalar_tensor_tensor", out=h2f, in0=xst[u], scalar=ssb[u][:, 2:3], in1=gbc, op0=ALU.mult, op1=ALU.mult,
               reads=[xst_t[u], ss_t[u], t_gbc], writes=[t_h2f])
            act("activation", out=h2b[:, i, :], in_=h2f, func=AF.Copy, reads=[t_h2f], writes=[t_h2b])
            for q4 in range(4):
                for q in range(4):
                    kc = q4 * 4 + q
                    pe("transpose", psf(q4)[:, q * 128:(q + 1) * 128], h2f[:, kc * 128:(kc + 1) * 128], c_identf,
                       reads=[t_h2f, t_const], writes=[pst[q4]])
                o_ = h2fT[:, q4 * 4:(q4 + 1) * 4, :]
                i_ = psf(q4).rearrange("p (a b) -> p a b", a=4)
                if q4 % 2 == 0:
                    act("activation", out=o_, in_=i_, func=AF.Copy, reads=[pst[q4]], writes=[t_h2fT])
                else:
                    dv("tensor_copy", out=o_, in_=i_, reads=[pst[q4]], writes=[t_h2fT])
            for kc in range(16):
                pe("matmul", psf(4)[:, 0:36], lhsT=h2fT[:, kc, :], rhs=Wr[:, kc, :], start=(kc == 0), stop=(kc == 15),
                   reads=[t_h2fT, t_Wr], writes=[pst[4]])
            dv("tensor_copy", out=lg, in_=psf(4)[:, 0:36], reads=[pst[4]], writes=TR)
            dv("tensor_reduce", out=rs[:, 0:1], in_=lg[:, 0:4], axis=AX.X, op=ALU.max, reads=TR, writes=TR)
            dv("tensor_scalar", goh, lg[:, 0:4], rs[:, 0:1], None, ALU.is_equal, reads=TR, writes=TR)
            dv("tensor_scalar", rs[:, 1:2], rs[:, 0:1], -1.0, None, ALU.mult, reads=TR, writes=TR)
            act("activation", out=j4, in_=lg[:, 0:4], func=AF.Exp, bias=rs[:, 1:2], accum_out=rs[:, 2:3], reads=TR, writes=TR)
            dv("reciprocal", rs[:, 3:4], rs[:, 2:3], reads=TR, writes=TR)
            dv("tensor_tensor", out=t48, in0=lg[:, 4:36].rearrange("p (g e) -> p g e", g=4),
               in1=goh.rearrange("p (g o) -> p g o", o=1).broadcast_to([128, 4, 8]), op=ALU.mult, reads=TR, writes=TR)
            dv("tensor_reduce", out=esel, in_=t48.rearrange("p g e -> p e g"), axis=AX.X, op=ALU.add, reads=TR, writes=TR)
            dv("max", v8, esel, reads=TR, writes=TR)
            dv("tensor_scalar", oh1, esel, v8[:, 0:1], None, ALU.is_equal, reads=TR, writes=TR)
            dv("tensor_scalar", oh2, esel, v8[:, 1:2], None, ALU.is_equal, reads=TR, writes=TR)
            dv("tensor_sub", out=rs[:, 4:5], in0=v8[:, 1:2], in1=v8[:, 0:1], reads=TR, writes=TR)
            act("activation", out=rs[:, 5:6], in_=rs[:, 4:5], func=AF.Exp, reads=TR, writes=TR)
            dv("tensor_scalar", rs[:, 6:7], rs[:, 5:6], 1.0, None, ALU.add, reads=TR, writes=TR)
            dv("reciprocal", rs[:, 7:8], rs[:, 6:7], reads=TR, writes=TR)
            dv("tensor_mul", out=gall[:, i, 0:1], in0=rs[:, 7:8], in1=rs[:, 3:4], reads=TR, writes=[t_rt])
            dv("tensor_mul", out=gall[:, i, 1:2], in0=rs[:, 5:6], in1=gall[:, i, 0:1], reads=TR + [t_rt], writes=[t_rt])
            for k, oh in ((0, oh1), (1, oh2)):
                dv("tensor_tensor", out=Eall[:, i, k, :].rearrange("p (g e) -> p g e", g=4),
                   in0=goh.rearrange("p (g o) -> p g o", o=1).broadcast_to([128, 4, 8]),
                   in1=oh.rearrange("p (o e) -> p o e", o=1).broadcast_to([128, 4, 8]), op=ALU.mult, reads=TR, writes=[t_rt])
            dv("tensor_add", out=Esum[:, i, :], in0=Eall[:, i, 0, :], in1=Eall[:, i, 1, :], reads=[t_rt], writes=[t_rt])
        for i in range(NSLOT):
            b = 5 + i % 2
            for i2 in range(i):
                pe("matmul", psf(b)[:, 0:32], lhsT=c_onesf, rhs=Esum[:, i2, :], start=(i2 == 0), stop=False,
                   reads=[t_rt, t_const], writes=[pst[b]])
            pe("matmul", psf(b)[:, 0:32], lhsT=c_strif, rhs=Esum[:, i, :], start=(i == 0), stop=True,
               reads=[t_rt, t_const], writes=[pst[b]])
            dv("tensor_copy", out=Call[:, i, :], in_=psf(b)[:, 0:32], reads=[pst[b]], writes=[t_rt])
        for i in range(NSLOT):
            pe("matmul", psf(7)[:, 0:32], lhsT=c_onesf, rhs=Esum[:, i, :], start=(i == 0), stop=(i == NSLOT - 1),
               reads=[t_rt, t_const], writes=[pst[7]])
        cnt = A.alloc([32], F32)
        nbk = A.alloc([32], F32)
        pend = A.alloc([32], F32)
        pstart = A.alloc([32], F32)
        Dall = A.alloc([NSLOT, 32], F32)
        tmpE = A.alloc([NSLOT, 32], F32)
        tmpb = A.alloc([NBLK, 32], F32)
        bef = A.alloc([NBLK], F32)
        nef = A.alloc([NBLK], F32)
        dv("tensor_copy", out=cnt, in_=psf(7)[:, 0:32], reads=[pst[7]], writes=TR)
        dv("tensor_single_scalar", nbk, cnt, 0.0, ALU.is_gt, reads=TR, writes=TR)
        for j in range(1, 8):
            dv("scalar_tensor_tensor", out=nbk, in0=cnt, scalar=128.0 * j, in1=nbk, op0=ALU.is_gt, op1=ALU.add, reads=TR, writes=TR)
        dv("tensor_tensor_scan", out=pend, data0=c_onesf[:, 0:32], data1=nbk, initial=0.0, op0=ALU.mult, op1=ALU.add,
           reads=TR + [t_const], writes=TR)
        dv("tensor_scalar", pend, pend, 128.0, None, ALU.mult, reads=TR, writes=TR)
        dv("scalar_tensor_tensor", out=pstart, in0=nbk, scalar=-128.0, in1=pend, op0=ALU.mult, op1=ALU.add, reads=TR, writes=TR)
        dv("tensor_tensor", out=Dall, in0=Call, in1=pstart.rearrange("p (o e) -> p o e", o=1).broadcast_to([128, NSLOT, 32]), op=ALU.add,
           reads=TR + [t_rt], writes=TR)
        for k in range(2):
            dv("tensor_tensor", out=tmpE, in0=Eall[:, :, k, :], in1=Dall, op=ALU.mult, reads=TR + [t_rt], writes=TR)
            dv("tensor_reduce", out=dest_f[:, :, k], in_=tmpE, axis=AX.X, op=ALU.add, reads=TR, writes=[t_rt])
        dv("tensor_copy", out=dest_i, in_=dest_f, reads=[t_rt], writes=[t_rt])
        dv("tensor_tensor", out=tmpb, in0=pend.rearrange("p (o e) -> p o e", o=1).broadcast_to([128, NBLK, 32]),
           in1=c_j128.rearrange("p (b o) -> p b o", o=1).broadcast_to([128, NBLK, 32]), op=ALU.is_le, reads=TR + [t_const], writes=TR)
        dv("tensor_reduce", out=bef, in_=tmpb, axis=AX.X, op=ALU.add, reads=TR, writes=TR)
        dv("tensor_scalar", bef, bef, 31.0, None, ALU.min, reads=TR, writes=TR)
        dv("tensor_scalar", nef, c_j128, pend[:, 31:32], None, ALU.is_lt, reads=TR + [t_const], writes=TR)
        BIG = 1.0e6
        dv("tensor_scalar", nef, nef, -1.0, -BIG, ALU.add, ALU.mult, reads=TR, writes=TR)
        dv("tensor_scalar", bef, bef, 512.0, c_pidx[:, 0:1], ALU.mult, ALU.add, reads=TR + [t_const], writes=TR)
        dv("tensor_add", out=bef, in0=bef, in1=nef, reads=TR, writes=TR)
        for m in range(4):
            dv("tensor_scalar", idxw_f[:, :, m], bef, 128.0 * m, None, ALU.add, reads=TR, writes=TR)
        dv("tensor_copy", out=idxw_i, in_=idxw_f, reads=TR, writes=[t_rt])
        for i in range(NSLOT):
            for k in range(2):
                P.op("pool", ("indirect_dma_start", (), dict(out=xb_d, out_offset=bass.IndirectOffsetOnAxis(ap=dest_i[:, i, k:k + 1], axis=0),
                                                             in_=h2b[:, i, :], in_offset=None)),
                     reads=[t_h2b, t_rt, t_xbz], writes=[t_xbd], dma_out=t_xbd)
        P.barrier()
        if stage == 35:
            return finish_debug(nc, P, dt, [("dest", dest_i, [128, NSLOT, 2], I32), ("gall", gall, [128, NSLOT, 2], F32),
                                            ("idxw", idxw_i, [128, NBLK, 4], I32), ("Eall", Eall, [128, NSLOT, 2, 32], F32),
                                            ("xb", xb_d, [NBLK * 128, D], BF16)], [t_rt, t_xbd])

        A.reset(moe_mark)
        NWB = 3
        order_ = []
        for q_ in range(16):
            order_ += [q_, 32 + q_]
        order_ += list(range(16, 32))
        WG = [A.alloc([4, 2048], BF16) for _ in range(NWB)]
        WU = [A.alloc([4, 2048], BF16) for _ in range(NWB)]
        WD = [A.alloc([4, 2048], BF16) for _ in range(NWB)]
        W8_t = [P.tile("W8_%d" % i) for i in range(NWB)]
        xbs = [A.alloc([D], BF16) for _ in range(4)]
        xbs_t = [P.tile("xbs%d" % i) for i in range(4)]
        xbT = [A.alloc([16, 128], BF16) for _ in range(2)]
        xbT_t = [P.tile("xbT%d" % i) for i in range(2)]
        sgf = A.alloc([512], F32)
        t_sgf = P.tile("sgf")
        hid = A.alloc([512], BF16)
        t_hid = P.tile("hid")
        hidT = A.alloc([4, 128], BF16)
        t_hidT = P.tile("hidT")
        yst = [A.alloc([D], F32) for _ in range(2)]
        yst_t = [P.tile("yst%d" % i) for i in range(2)]
        t_ybd = P.tile("yb_d")

        def load_w8(n_, s):
            blk = order_[n_]
            for (wsrc, wdst) in ((wb_d[0], WG[s]), (wb_d[1], WU[s]), (wb_d[2], WD[s])):
                for m in range(4):
                    P.op("pool", ("indirect_dma_start", (), dict(out=wdst[:, m, :], out_offset=None, in_=wsrc,
                                                                 in_offset=bass.IndirectOffsetOnAxis(ap=idxw_i[:, blk, m:m + 1], axis=0),
                                                                 bounds_check=NE * 512 - 1, oob_is_err=False)),
                         reads=[t_rt, t_wb], writes=[W8_t[s]], dma_out=W8_t[s])

        sgf2 = [sgf, A.alloc([512], F32)]
        sgf2_t = [t_sgf, P.tile("sgf1")]
        hid2 = [hid, A.alloc([512], BF16)]
        hid2_t = [t_hid, P.tile("hid1")]
        hidT2 = [hidT, A.alloc([4, 128], BF16)]
        hidT2_t = [t_hidT, P.tile("hidT1")]

        def moe_L(n_):
            pb_ = order_[n_]
            P.DMA("sp", xbs[n_ % 4], xb_d[pb_ * 128:(pb_ + 1) * 128, :], [t_xbd, t_xbz], xbs_t[n_ % 4])

        def moe_S1(blk):
            s = blk % 2
            if blk + 3 < NBLK:
                moe_L(blk + 3)
            for half in range(2):
                for q in range(8):
                    c = half * 8 + q
                    m, kk = c // 4, c % 4
                    pe("transpose", psb(half)[:, q * 128:(q + 1) * 128], xbs[blk % 4][:, 512 * m + kk:512 * (m + 1):4], c_identb,
                       reads=[xbs_t[blk % 4], t_const], writes=[pst[half]])
                o_ = xbT[s][:, half * 8:(half + 1) * 8, :]
                i_ = psb(half).rearrange("p (a b) -> p a b", a=8)
                if half == 0:
                    act("activation", out=o_, in_=i_, func=AF.Copy, reads=[pst[half]], writes=[xbT_t[s]])
                else:
                    dv("tensor_copy", out=o_, in_=i_, reads=[pst[half]], writes=[xbT_t[s]])

        def moe_S2(blk):
            s = blk % 2
            ws = blk % NWB
            for (wt, b_) in ((WG[ws], 2), (WU[ws], 3)):
                for c in range(16):
                    m, kk = c // 4, c % 4
                    pe("matmul", psf(b_), lhsT=xbT[s][:, c, :], rhs=wt[:, m, kk * 512:(kk + 1) * 512], start=(c == 0), stop=(c == 15),
                       reads=[xbT_t[s], W8_t[ws]], writes=[pst[b_]])
            act("activation", out=sgf2[s], in_=psf(2), func=AF.Silu, reads=[pst[2]], writes=[sgf2_t[s]])
            dv("tensor_tensor", out=hid2[s], in0=sgf2[s], in1=psf(3), op=ALU.mult, reads=[sgf2_t[s], pst[3]], writes=[hid2_t[s]])

        def moe_S3(blk):
            s = blk % 2
            for m in range(4):
                pe("transpose", psb(4)[:, m * 128:(m + 1) * 128], hid2[s][:, m * 128:(m + 1) * 128], c_identb,
                   reads=[hid2_t[s], t_const], writes=[pst[4]])
            dv("tensor_copy", out=hidT2[s], in_=psb(4)[:, 0:512].rearrange("p (a b) -> p a b", a=4), reads=[pst[4]], writes=[hidT2_t[s]])

        def moe_S4(blk):
            s = blk % 2
            ws = blk % NWB
            for cg in range(4):
                bk = 5 + (blk * 4 + cg) % 3
                for m in range(4):
                    pe("matmul", psf(bk), lhsT=hidT2[s][:, m, :], rhs=WD[ws][:, m, cg * 512:(cg + 1) * 512], start=(m == 0), stop=(m == 3),
                       reads=[hidT2_t[s], W8_t[ws]], writes=[pst[bk]])
                if cg % 2 == 0:
                    act("activation", out=yst[s][:, cg * 512:(cg + 1) * 512], in_=psf(bk), func=AF.Copy, reads=[pst[bk]], writes=[yst_t[s]])
                else:
                    dv("tensor_copy", out=yst[s][:, cg * 512:(cg + 1) * 512], in_=psf(bk), reads=[pst[bk]], writes=[yst_t[s]])
            pb_ = order_[blk]
            P.DMA("sp", yb_d[pb_ * 128:(pb_ + 1) * 128, :], yst[s], [yst_t[s]], t_ybd, sem_tile=yst_t[s])

        order = []
        for q in range(16):
            order += [q, 32 + q]
        order += list(range(16, 32))
        assert sorted(order) == list(range(NBLK))
        for n in range(3):
            moe_L(n)
        load_w8(0, 0)
        load_w8(1, 1)
        moe_S1(0)
        moe_S2(0)
        for n in range(NBLK):
            if n + 2 < NBLK:
                load_w8(n + 2, (n + 2) % NWB)
            if n + 1 < NBLK:
                moe_S1(n + 1)
            moe_S3(n)
            if n + 1 < NBLK:
                moe_S2(n + 1)
            moe_S4(n)
        P.barrier()

        A.reset(moe_mark)
        full = stage >= 5
        x1t = [A.alloc([D], F32) for _ in range(2)]
        x1t_t = [P.tile("x1t%d" % i) for i in range(2)]
        ya = [A.alloc([D], F32)] * 2
        ya_t = [P.tile("ya")] * 2
        yb2 = [A.alloc([D], F32)] * 2
        yb2_t = [P.tile("yb2")] * 2
        x2 = [A.alloc([D], F32) for _ in range(2)]
        x2_t = [P.tile("x2_%d" % i) for i in range(2)]
        t_dbg2 = P.tile("dbgx2")
        if not full:
            dbg_x2 = dt("dbg_x2", [TOWN, D], F32, kind="ExternalOutput").ap()
        if full:
            gfin = A.alloc([D], F32)
            t_gfin = P.tile("gfin")
            gbc = A.alloc([D], F32)
            t_gbc = P.tile("gbc9")
            P.DMA("sp", gbc, bcast_row(g_ple), [], t_gbc)
            P.DMA("sp", gfin, bcast_row(g_final), [], t_gfin)
            Wpg = A.alloc([16, D], BF16)
            Wpp = A.alloc([2, D], BF16)
            t_Wp = P.tile("Wp")
            for kc4 in range(4):
                P.DMA("pool", Wpg[:, kc4 * 4:(kc4 + 1) * 4, :], w_pg[kc4 * 512:(kc4 + 1) * 512, :].rearrange("(kc p) n -> p kc n", p=128), [], t_Wp)
            P.DMA("pool", Wpp, w_pp.rearrange("(kc p) n -> p kc n", p=128), [], t_Wp)
            h3 = [A.alloc([D], BF16) for _ in range(2)]
            h3_t = [P.tile("h3_%d" % i) for i in range(2)]
            h3T = [A.alloc([16, 128], BF16) for _ in range(2)]
            h3T_t = [P.tile("h3T%d" % i) for i in range(2)]
            ptl = A.alloc([256], F32)
            t_ptl = P.tile("ptl")
            pb16 = A.alloc([256], BF16)
            t_pb = P.tile("pb16")
            pT = [A.alloc([2, 128], BF16) for _ in range(2)]
            pT_t = [P.tile("pT%d" % i) for i in range(2)]
            sgp = [A.alloc([512], F32) for _ in range(2)]
            sgp_t = [P.tile("sgp%d" % i) for i in range(2)]
            x3 = [A.alloc([D], F32) for _ in range(2)]
            x3_t = [P.tile("x3_%d" % i) for i in range(2)]
            ot = [A.alloc([D], F32)] * 2
            ot_t = [P.tile("ot")] * 2
            ssc = [A.alloc([4], F32) for _ in range(2)]
            ssc_t = [P.tile("ssc%d" % i) for i in range(2)]
            ssd = [A.alloc([4], F32) for _ in range(2)]
            ssd_t = [P.tile("ssd%d" % i) for i in range(2)]
            t_y = P.tile("y")
        itp = [0]

        def tail_front(i):
            u = i % 2
            P.DMA("sp", x1t[u], x1_d[i * 128:(i + 1) * 128, :], [t_x1d], x1t_t[u])
            for (k, dst, dst_t) in ((0, ya[u], ya_t[u]), (1, yb2[u], yb2_t[u])):
                P.op("pool", ("indirect_dma_start", (), dict(out=dst, out_offset=None, in_=yb_d,
                                                             in_offset=bass.IndirectOffsetOnAxis(ap=dest_i[:, i, k:k + 1], axis=0))),
                     reads=[t_rt, t_ybd], writes=[dst_t], dma_out=dst_t)
            dv("scalar_tensor_tensor", out=x2[u], in0=ya[u], scalar=gall[:, i, 0:1], in1=x1t[u], op0=ALU.mult, op1=ALU.add,
               reads=[ya_t[u], x1t_t[u], t_rt], writes=[x2_t[u]])
            dv("scalar_tensor_tensor", out=x2[u], in0=yb2[u], scalar=gall[:, i, 1:2], in1=x2[u], op0=ALU.mult, op1=ALU.add,
               reads=[yb2_t[u], x2_t[u], t_rt], writes=[x2_t[u]])
            if not full:
                P.DMA("sp", dbg_x2[i * 128:(i + 1) * 128, :], x2[u], [x2_t[u]], t_dbg2, output=True, sem_tile=x2_t[u])
                return
            norm_rstd(x2[u], x2_t[u], h3[u], h3_t[u], ssc[u], ssc_t[u])
            dv("scalar_tensor_tensor", out=h3[u], in0=x2[u], scalar=ssc[u][:, 2:3], in1=gbc, op0=ALU.mult, op1=ALU.mult,
               reads=[x2_t[u], ssc_t[u], t_gbc], writes=[h3_t[u]])
            transpose16(h3[u], h3_t[u], h3T[u], h3T_t[u], 0, (0, 1))
            P.DMA("sp", ptl, pown[i * 128:(i + 1) * 128, :], [], t_ptl)
            act("activation", out=pb16, in_=ptl, func=AF.Copy, reads=[t_ptl], writes=[t_pb])
            for c in range(2):
                pe("transpose", psb(2)[:, c * 128:(c + 1) * 128], pb16[:, c * 128:(c + 1) * 128], c_identb,
                   reads=[t_pb, t_const], writes=[pst[2]])
            dv("tensor_copy", out=pT[u], in_=psb(2)[:, 0:256].rearrange("p (a b) -> p a b", a=2), reads=[pst[2]], writes=[pT_t[u]])

        def tail_back(i):
            u = i % 2
            for cg in range(4):
                bg = 4 + (itp[0] % 2) * 2
                sg_, sg_t_ = sgp[itp[0] % 2], sgp_t[itp[0] % 2]
                itp[0] += 1
                cs = slice(cg * 512, (cg + 1) * 512)
                for kc in range(16):
                    pe("matmul", psf(bg), lhsT=h3T[u][:, kc, :], rhs=Wpg[:, kc, cs], start=(kc == 0), stop=(kc == 15),
                       reads=[h3T_t[u], t_Wp], writes=[pst[bg]])
                for c in range(2):
                    pe("matmul", psf(bg + 1), lhsT=pT[u][:, c, :], rhs=Wpp[:, c, cs], start=(c == 0), stop=(c == 1),
                       reads=[pT_t[u], t_Wp], writes=[pst[bg + 1]])
                act("activation", out=sg_, in_=psf(bg), func=AF.Sigmoid, reads=[pst[bg]], writes=[sg_t_])
                dv("tensor_tensor", out=sg_, in0=sg_, in1=psf(bg + 1), op=ALU.mult, reads=[sg_t_, pst[bg + 1]], writes=[sg_t_])
                if cg % 2 == 0:
                    P.I("pool", "tensor_tensor", out=x3[u][:, cs], in0=sg_, in1=x2[u][:, cs], op=ALU.add, reads=[sg_t_, x2_t[u]], writes=[x3_t[u]])
                else:
                    dv("tensor_tensor", out=x3[u][:, cs], in0=sg_, in1=x2[u][:, cs], op=ALU.add, reads=[sg_t_, x2_t[u]], writes=[x3_t[u]])
            norm_rstd(x3[u], x3_t[u], h3[u], h3_t[u], ssd[u], ssd_t[u])
            dv("scalar_tensor_tensor", out=ot[u], in0=x3[u], scalar=ssd[u][:, 2:3], in1=gfin, op0=ALU.mult, op1=ALU.mult,
               reads=[x3_t[u], ssd_t[u], t_gfin], writes=[ot_t[u]])
            P.DMA("sp", y[i * 128:(i + 1) * 128, :], ot[u], [ot_t[u]], t_y, output=True, sem_tile=ot_t[u])

        tail_front(0)
        for i in range(NSLOT):
            if i + 1 < NSLOT:
                tail_front(i + 1)
            if full:
                tail_back(i)
        P.finish()
    return nc


def finish_debug(nc, P, dt, items, t_dep):
    for name, ap, shape, dtype in items:
        o = dt("dbg_" + name, shape, dtype, kind="ExternalOutput").ap()
        t = P.tile("dbg_" + name)
        P.DMA("sp", o, ap, list(t_dep), t, output=True)
    P.finish()
    return nc


def _core_blocks(i):
    blks = []
    for m in range(4):
        blks += [8 * m + i, 8 * m + 7 - i]
    return blks


def make_in_maps(inputs):
    x = np.asarray(inputs["x"], dtype=np.float32)
    p = np.asarray(inputs["p"], dtype=np.float32)[0]
    f = lambda k: np.ascontiguousarray(np.asarray(inputs[k], dtype=np.float32))
    shared = {
        "w_in": f("w_in")[0], "b_forget": f("b_forget").reshape(1, 8),
        "w_branch_fox": f("w_branch_fox")[0], "w_branch_sb": f("w_branch_sb")[0], "w_mix_out": f("w_mix_out")[0],
        "g_mix": f("g_mix").reshape(1, D), "g_ffn": f("g_ffn").reshape(1, D),
        "w_rout": np.ascontiguousarray(np.concatenate([f("w_group")[0], f("w_expert")[0]], axis=1)),
        "w_gate": f("w_gate")[0].reshape(NE * 512, 2048), "w_up": f("w_up")[0].reshape(NE * 512, 2048),
        "w_down": f("w_down")[0].reshape(NE * 512, 2048),
        "g_ple": f("g_ple").reshape(1, D), "w_ple_proj": f("w_ple_proj")[0], "w_ple_gate": f("w_ple_gate")[0],
        "g_final": f("g_final").reshape(1, D),
    }
    in_maps, rows = [], []
    for c in range(8):
        b, i = c // 4, c % 4
        blks = _core_blocks(i)
        idx = np.concatenate([np.arange(k * 128, (k + 1) * 128) for k in blks])
        rows.append((b, idx))
        m = dict(shared)
        m["xall"] = np.ascontiguousarray(x[b])
        m["xown"] = np.ascontiguousarray(x[b][idx])
        m["pown"] = np.ascontiguousarray(p[b][idx])
        m["blk128"] = np.ascontiguousarray(np.tile(np.asarray(blks, np.float32)[None, :] * 128.0, (128, 1)))
        in_maps.append(m)
    return in_maps, rows


_NC_CACHE = {}


def kernel(**inputs):
    in_maps, rows = make_in_maps(inputs)
    if "nc" not in _NC_CACHE:
        _NC_CACHE["nc"] = build_nc()
    nc = _NC_CACHE["nc"]
    names = set(a.memorylocations[0].name for a in nc.allocations
                if isinstance(a, mybir.MemoryLocationSet) and a.kind == "ExternalInput")
    in_maps = [{k: v for k, v in m.items() if k in names} for m in in_maps]
    res = run_bass_kernel_spmd(nc, in_maps, core_ids=list(range(8)))
    out = np.zeros((2, S, D), np.float32)
    for c in range(8):
        b, idx = rows[c]
        out[b, idx] = np.asarray(res.results[c]["y"])
    return out
```

```python
import numpy as np
from contextlib import ExitStack
import concourse.bass as bass
import concourse.mybir as mybir
from concourse.bass_utils import run_bass_kernel_spmd

F32 = mybir.dt.float32
BF16 = mybir.dt.bfloat16
I32 = mybir.dt.int32
U32 = mybir.dt.uint32
U8 = mybir.dt.uint8
AF = mybir.ActivationFunctionType
ALU = mybir.AluOpType
AX = mybir.AxisListType

D = 2048
S = 4096
NB = 32
NSLOT = 8
TOWN = 1024
DIN = 10248
NE = 32
DEXP = 512
NBLK = 48
EPS = 1e-6
SCALE = 128 ** -0.5
NK = [4, 8, 12, 16, 20, 24, 28, 32]


class Tl:
    __slots__ = ("name", "writers", "readers", "sem", "dma_cnt")

    def __init__(self, name):
        self.name = name
        self.writers = []
        self.readers = []
        self.sem = {}
        self.dma_cnt = {}


class Op:
    __slots__ = ("eng", "emit", "deps", "marked", "semval", "idx", "dma")

    def __init__(self, eng, emit):
        self.eng = eng
        self.emit = emit
        self.deps = []
        self.marked = False
        self.semval = 0
        self.idx = 0
        self.dma = None


class Prog:
    ENGS = ("pe", "act", "dve", "pool", "sp")

    def __init__(self, nc, stack):
        self.nc = nc
        self.stack = stack
        self.ops = {e: [] for e in self.ENGS}
        self.nops = 0
        self.esem = {e: stack.enter_context(nc.semaphore("es_" + e)) for e in self.ENGS}
        self.dma_tokens = []
        self.out_tokens = []
        self.nsem = 0

    def tile(self, name):
        return Tl(name)

    def _tsem(self, t, kind):
        if kind not in t.sem:
            t.sem[kind] = self.stack.enter_context(self.nc.semaphore("ds_%d" % self.nsem))
            t.dma_cnt[kind] = 0
            self.nsem += 1
        return t.sem[kind]

    def I(self, eng, fname, *args, reads=(), writes=(), **kw):
        return self.op(eng, (fname, args, kw), reads=reads, writes=writes)

    def DMA(self, eng, out, in_, reads, wt, output=False, sem_tile=None, **kw):
        return self.op(eng, ("dma_start", (), dict(out=out, in_=in_, **kw)), reads=reads, writes=[wt], dma_out=wt, is_output=output,
                       sem_tile=sem_tile)

    def op(self, eng, emit, reads=(), writes=(), dma_out=None, is_output=False, sem_tile=None):
        o = Op(eng, emit)
        o.idx = self.nops
        self.nops += 1
        deps = []
        for t in reads:
            deps.extend(t.writers)
        for t in writes:
            deps.extend(t.readers)
            if dma_out is not None:
                deps.extend(tk for tk in t.writers if tk[0] != "dma")
            else:
                deps.extend(t.writers)
        o.deps = deps
        if dma_out is not None:
            kind = "sw" if eng == "pool" else "hw"
            st = sem_tile if sem_tile is not None else dma_out
            sem = self._tsem(st, kind)
            st.dma_cnt[kind] += 16
            tok = ("dma", sem, st.dma_cnt[kind])
            o.dma = sem
            self.dma_tokens.append(tok)
            if is_output:
                self.out_tokens.append(tok)
        else:
            tok = ("op", o)
        for t in reads:
            t.readers.append(tok)
        for t in writes:
            if t.readers:
                t.writers = [tok]
                t.readers = []
            else:
                t.writers.append(tok)
                if len(t.writers) > 64:
                    t.writers = self._compact(t.writers)
        for t in reads:
            if len(t.readers) > 64:
                t.readers = self._compact(t.readers)
        self.ops[eng].append(o)
        return o

    @staticmethod
    def _compact(toks):
        best = {}
        for tk in toks:
            if tk[0] == "op":
                k = ("op", tk[1].eng)
                if k not in best or best[k][1].idx < tk[1].idx:
                    best[k] = tk
            else:
                k = ("dma", id(tk[1]))
                if k not in best or best[k][2] < tk[2]:
                    best[k] = tk
        return list(best.values())

    def barrier(self):
        toks = list(self.dma_tokens)
        for e in self.ENGS:
            if self.ops[e]:
                last = self.ops[e][-1]
                if last.dma is None and (last.emit is not None or last.deps):
                    toks.append(("op", last))
        self.dma_tokens = []
        for e in self.ENGS:
            o = Op(e, None)
            o.idx = self.nops
            self.nops += 1
            o.deps = [t for t in toks if not (t[0] == "op" and t[1].eng == e and e in ("pe", "sp"))]
            self.ops[e].append(o)

    def finish(self):
        o = Op("sp", None)
        o.deps = list(self.out_tokens)
        self.ops["sp"].append(o)
        for e in self.ENGS:
            for o in self.ops[e]:
                for tk in o.deps:
                    if tk[0] == "op":
                        p = tk[1]
                        if p.eng == "pe" and e == "pe":
                            continue
                        p.marked = True
        for e in self.ENGS:
            c = 0
            for o in self.ops[e]:
                if o.marked:
                    c += 1
                    o.semval = c
        nc = self.nc
        hmap = {"pe": nc.tensor, "act": nc.scalar, "dve": nc.vector, "pool": nc.gpsimd, "sp": nc.sync}
        with nc.Block() as block:
            def run(e, h):
                waited = {}
                regcache = {}
                for o in self.ops[e]:
                    need = {}
                    for tk in o.deps:
                        if tk[0] == "op":
                            p = tk[1]
                            if p.eng == "pe" and e == "pe":
                                continue
                            sem = self.esem[p.eng]
                            val = p.semval
                        else:
                            sem = tk[1]
                            val = tk[2]
                        k = id(sem)
                        if waited.get(k, (None, 0))[1] >= val:
                            continue
                        if k not in need or need[k][1] < val:
                            need[k] = (sem, val)
                    for k, (sem, val) in need.items():
                        h.wait_ge(sem, val)
                        waited[k] = (sem, val)
                    ins = None
                    if o.emit is not None:
                        fname, args, kw = o.emit
                        if "bounds_check" in kw and isinstance(kw["bounds_check"], int):
                            bv = kw["bounds_check"]
                            if bv not in regcache:
                                regcache[bv] = h.to_reg(bv)
                            kw = dict(kw, bounds_check=regcache[bv])
                        try:
                            ins = getattr(h, fname)(*args, **kw)
                        except Exception:
                            print("EMIT FAIL", e, fname, {k: (getattr(v, "shape", v)) for k, v in kw.items()})
                            raise
                    elif o.marked:
                        ins = h.nop()
                    if ins is not None:
                        if o.dma is not None:
                            ins.then_inc(o.dma, 16)
                        elif o.marked:
                            ins.then_inc(self.esem[e], 1)

            @block.tensor
            def _(h):
                run("pe", h)

            @block.scalar
            def _(h):
                run("act", h)

            @block.vector
            def _(h):
                run("dve", h)

            @block.gpsimd
            def _(h):
                run("pool", h)

            @block.sync
            def _(h):
                run("sp", h)


class Arena:
    def __init__(self, nc, nbytes):
        self.big = nc.alloc_sbuf_tensor("arena", [128, nbytes], U8)
        self.nbytes = nbytes
        self.off = 0

    def mark(self):
        return self.off

    def reset(self, off):
        self.off = off

    def alloc(self, shape_free, dtype, nparts=128):
        esz = {F32: 4, BF16: 2, I32: 4, U32: 4}[dtype]
        n = 1
        for s in shape_free:
            n *= s
        nb = (n * esz + 63) // 64 * 64
        assert self.off + nb <= self.nbytes, ("sbuf overflow", self.off, nb)
        ap = self.big[:, self.off:self.off + n * esz].bitcast(dtype)
        self.off += nb
        if len(shape_free) == 2:
            ap = ap.rearrange("p (a b) -> p a b", a=shape_free[0])
        elif len(shape_free) == 3:
            ap = ap.rearrange("p (a b c) -> p a b c", a=shape_free[0], b=shape_free[1])
        return ap


def build_nc(stage=99):
    nc = bass.Bass("TRN2", target_bir_lowering=False)
    dt = nc.dram_tensor
    xall = dt("xall", [S, D], F32, kind="ExternalInput").ap()
    xown = dt("xown", [TOWN, D], F32, kind="ExternalInput").ap()
    blk128 = dt("blk128", [128, NSLOT], F32, kind="ExternalInput").ap()
    w_in = dt("w_in", [D, DIN], F32, kind="ExternalInput").ap()
    b_forget = dt("b_forget", [1, 8], F32, kind="ExternalInput").ap()
    g_mix = dt("g_mix", [1, D], F32, kind="ExternalInput").ap()
    if stage >= 3:
        w_bfox = dt("w_branch_fox", [1024, D], F32, kind="ExternalInput").ap()
        w_bsb = dt("w_branch_sb", [1024, D], F32, kind="ExternalInput").ap()
        w_mix = dt("w_mix_out", [D, D], F32, kind="ExternalInput").ap()
    if stage >= 4:
        g_ffn = dt("g_ffn", [1, D], F32, kind="ExternalInput").ap()
        w_rout = dt("w_rout", [D, 36], F32, kind="ExternalInput").ap()
        w_gate = dt("w_gate", [NE * 512, 2048], F32, kind="ExternalInput").ap()
        w_up = dt("w_up", [NE * 512, 2048], F32, kind="ExternalInput").ap()
        w_down = dt("w_down", [NE * 512, 2048], F32, kind="ExternalInput").ap()
    if stage >= 5:
        pown = dt("pown", [TOWN, 256], F32, kind="ExternalInput").ap()
        g_ple = dt("g_ple", [1, D], F32, kind="ExternalInput").ap()
        w_pp = dt("w_ple_proj", [256, D], F32, kind="ExternalInput").ap()
        w_pg = dt("w_ple_gate", [D, D], F32, kind="ExternalInput").ap()
        g_final = dt("g_final", [1, D], F32, kind="ExternalInput").ap()
    y = dt("y", [TOWN, D], F32, kind="ExternalOutput").ap()

    hT_d = dt("hT_d", [16, 128, S], BF16).ap()
    kaT_d = dt("kaT_d", [8, 128, S], BF16).ap()
    kbT_d = dt("kbT_d", [8, 128, S], BF16).ap()
    va_d = dt("va_d", [S, 1024], BF16).ap()
    vb_d = dt("vb_d", [S, 1024], BF16).ap()
    x1_d = dt("x1_d", [TOWN, D], F32).ap()
    xb_d = dt("xb_d", [NBLK * 128, D], BF16).ap()
    yb_d = dt("yb_d", [NBLK * 128, D], F32).ap()
    wb_d = [dt("wb%d_d" % i, [NE * 512, 2048], BF16).ap() for i in range(3)]

    stack = ExitStack()
    with stack:
        P = Prog(nc, stack)
        A = Arena(nc, 207 * 1024)
        ps = [nc.alloc_psum_tensor("ps%d" % i, [128, 512], F32) for i in range(8)]
        pst = [P.tile("ps%d" % i) for i in range(8)]

        def psf(i):
            return ps[i][:]

        def psb(i):
            return ps[i][:].bitcast(BF16)

        def bcast_row(ap1d):
            return ap1d.partition_broadcast(128).rearrange("p a n -> p (a n)")

        c_dmat = A.alloc([128], F32)
        c_identb = A.alloc([128], BF16)
        c_identf = A.alloc([128], F32)
        c_trif = A.alloc([128], F32)
        c_strif = A.alloc([128], F32)
        c_onesf = A.alloc([128], F32)
        c_ntri = A.alloc([128], BF16)
        c_nones = A.alloc([128], BF16)
        c_ones = A.alloc([128], BF16)
        c_j128 = A.alloc([NBLK], F32)
        c_pidx = A.alloc([1], F32)
        c_blk = A.alloc([NSLOT], F32)
        c_onehot = A.alloc([NSLOT, NB], F32)
        c_bf = A.alloc([8], F32)
        c_eps = A.alloc([1], F32)
        core_mark = A.mark()
        m_le = A.alloc([32, 128], BF16)
        m_lt = A.alloc([32, 128], BF16)
        t_const = P.tile("const")
        TC = [t_const]
        P.I("pool", "iota", c_dmat, [[1, 128]], base=0, channel_multiplier=-1, allow_small_or_imprecise_dtypes=True, writes=TC)
        P.I("pool", "iota", c_j128, [[128, NBLK]], base=0, channel_multiplier=0, allow_small_or_imprecise_dtypes=True, writes=TC)
        P.I("pool", "iota", c_pidx, [[0, 1]], base=0, channel_multiplier=1, allow_small_or_imprecise_dtypes=True, writes=TC)
        P.DMA("sp", c_blk, blk128, [], t_const)
        P.DMA("sp", c_bf, bcast_row(b_forget), [], t_const)

        def dv(fname, *args, reads=(), writes=(), **kw):
            return P.I("dve", fname, *args, reads=reads, writes=writes, **kw)

        def act(fname, *args, reads=(), writes=(), **kw):
            return P.I("act", fname, *args, reads=reads, writes=writes, **kw)

        def pe(fname, *args, reads=(), writes=(), **kw):
            return P.I("pe", fname, *args, reads=reads, writes=writes, **kw)

        dv("tensor_single_scalar", c_identb, c_dmat, 0.0, ALU.is_equal, reads=TC, writes=TC)
        dv("tensor_single_scalar", c_identf, c_dmat, 0.0, ALU.is_equal, reads=TC, writes=TC)
        dv("tensor_single_scalar", c_trif, c_dmat, 0.0, ALU.is_ge, reads=TC, writes=TC)
        dv("tensor_single_scalar", c_strif, c_dmat, 0.0, ALU.is_gt, reads=TC, writes=TC)
        dv("memset", c_onesf, 1.0, writes=TC)
        dv("memset", c_ones, 1.0, writes=TC)
        dv("memset", c_nones, -1.0, writes=TC)
        dv("memset", c_eps, EPS, writes=TC)
        dv("tensor_scalar", c_ntri, c_dmat, 0.0, -1.0, ALU.is_le, ALU.mult, reads=TC, writes=TC)
        for k in range(NSLOT):
            dv("tensor_scalar", c_onehot[:, k, :], c_j128[:, 0:NB], c_blk[:, k:k + 1], None, ALU.is_equal, reads=TC, writes=TC)
        for g in range(NSLOT):
            for j in range(4):
                kb = 4 * g + j
                dv("tensor_scalar", m_le[:, 4 * g + j, :], c_dmat, c_blk[:, g:g + 1], 128.0 * kb, ALU.add, ALU.is_ge, reads=TC, writes=TC)
                dv("tensor_scalar", m_lt[:, 4 * g + j, :], c_dmat, c_blk[:, g:g + 1], 128.0 * kb + 1.0, ALU.add, ALU.is_ge, reads=TC, writes=TC)
        if stage == 0:
            return finish_debug(nc, P, dt, [("m_le", m_le, [128, 32, 128], BF16), ("onehot", c_onehot, [128, NSLOT, NB], F32),
                                            ("ntri", c_ntri, [128, 128], BF16), ("bf", c_bf, [128, 8], F32)], TC)
        t_xbd = P.tile("xb_d")
        t_xbz = P.tile("xb_z")

        gbc = A.alloc([D], F32)
        t_gbc = P.tile("gbc")
        P.DMA("sp", gbc, bcast_row(g_mix), [], t_gbc)
        t_wb = P.tile("wb_d")
        cv_pieces = [(mat, e) for e in range(NE) for mat in range(3)]
        cv_state = {"l": 0}
        cv_on = stage >= 4
        zero_b = A.alloc([D], BF16)
        dv("memset", zero_b, 0.0, writes=TC)

        def conv_step(k=1):
            if not cv_on:
                return
            wsrc = (w_gate, w_up, w_down)
            for _ in range(k):
                if cv_state["l"] >= len(cv_pieces):
                    return
                mat, e = cv_pieces[cv_state["l"]]
                P.DMA("pool", wb_d[mat][e * 512:(e + 1) * 512, :], wsrc[mat][e * 512:(e + 1) * 512, :], [], t_wb)
                cv_state["l"] += 1

        def conv_flush():
            conv_step(len(cv_pieces))

        c_all = A.alloc([8, NB], F32)
        cbias = A.alloc([8, NSLOT], F32)
        t_c = P.tile("c")
        persist_mark = A.mark()

        def norm_rstd(xst, xst_t, junk, junk_t, ss, ss_t):
            act("activation", out=junk, in_=xst, func=AF.Square, accum_out=ss[:, 0:1], reads=[xst_t], writes=[junk_t, ss_t])
            act("activation", out=ss[:, 1:2], in_=ss[:, 0:1], func=AF.Sqrt, bias=c_eps[:, 0:1], scale=1.0 / D,
                reads=[ss_t, t_const], writes=[ss_t])
            dv("reciprocal", ss[:, 2:3], ss[:, 1:2], reads=[ss_t], writes=[ss_t])

        def transpose16(src_bf, src_t, dst, dst_t, col0, banks, stride=1, evac=("act", "dve")):
            for half in range(2):
                b = banks[half]
                for q in range(8):
                    kc = half * 8 + q
                    pe("transpose", psb(b)[:, q * 128:(q + 1) * 128], src_bf[:, kc * 128:(kc + 1) * 128], c_identb,
                       reads=[src_t, t_const], writes=[pst[b]])
                o_ = dst[:, half * 8:(half + 1) * 8, col0:col0 + 128]
                i_ = psb(b).rearrange("p (a b) -> p a b", a=8)
                if evac[half] == "act":
                    act("activation", out=o_, in_=i_, func=AF.Copy, reads=[pst[b]], writes=[dst_t])
                else:
                    dv("tensor_copy", out=o_, in_=i_, reads=[pst[b]], writes=[dst_t])

        def norm_transpose(x_src_ap, gb, gb_t, xst, xst_t, xn, xn_t, ss, ss_t, hT_dst, hT_t, col0, banks):
            P.DMA("sp", xst, x_src_ap, [], xst_t)
            norm_rstd(xst, xst_t, xn, xn_t, ss, ss_t)
            dv("scalar_tensor_tensor", out=xn, in0=xst, scalar=ss[:, 2:3], in1=gb, op0=ALU.mult, op1=ALU.mult,
               reads=[xst_t, ss_t, gb_t], writes=[xn_t])
            transpose16(xn, xn_t, hT_dst, hT_t, col0, banks)

        kv_tiles = []
        t_hd = P.tile("hd")
        for pss in range(2):
            A.reset(persist_mark)
            W = A.alloc([16, 2048], BF16)
            t_W = P.tile("W")
            c0 = 1024 if pss == 0 else 4104
            for kc4 in range(4):
                P.DMA("pool", W[:, kc4 * 4:(kc4 + 1) * 4, :],
                      w_in[kc4 * 512:(kc4 + 1) * 512, c0:c0 + 2048].rearrange("(kc p) n -> p kc n", p=128), [], t_W)
            if pss == 0:
                Wf32 = A.alloc([16, 8], F32)
                Wf = A.alloc([16, 8], BF16)
                fl_all = A.alloc([NB * 8], F32)
                t_fl = P.tile("fl")
                t_Wf = P.tile("Wf")
                P.DMA("sp", Wf32, w_in[:, 3072:3080].rearrange("(kc p) n -> p kc n", p=128), [], t_Wf)
                dv("tensor_copy", out=Wf, in_=Wf32, reads=[t_Wf], writes=[t_Wf])
            hT = [A.alloc([16, 512], BF16) for _ in range(2)]
            hT_t = [P.tile("hT%d" % i) for i in range(2)]
            kst = [A.alloc([512], BF16) for _ in range(4)]
            kst_t = [P.tile("kst%d" % i) for i in range(4)]
            vst = [A.alloc([1024], BF16) for _ in range(3)]
            vst_t = [P.tile("vst%d" % i) for i in range(3)]
            kT_d = kaT_d if pss == 0 else kbT_d
            v_d = va_d if pss == 0 else vb_d
            t_kd = P.tile("kd%d" % pss)
            t_vd = P.tile("vd%d" % pss)
            kv_tiles.extend([t_kd, t_vd])
            xn4 = [A.alloc([D], BF16) for _ in range(4)] if pss == 0 else None
            xn4_t = [P.tile("xn4_%d" % i) for i in range(4)]
            ss4 = [A.alloc([4], F32) for _ in range(4)] if pss == 0 else None
            ss4_t = [P.tile("ss4_%d" % i) for i in range(4)]

            xst4 = [A.alloc([D], F32) for _ in range(4)] if pss == 0 else None
            xst4_t = [P.tile("xst4_%d" % i) for i in range(4)]

            def load_group(G):
                for tt in range(4):
                    r0 = (G * 4 + tt) * 128
                    P.DMA("sp", xst4[tt], xall[r0:r0 + 128, :], [], xst4_t[tt])

            def norm_tile(G, tt):
                norm_rstd(xst4[tt], xst4_t[tt], xn4[tt], xn4_t[tt], ss4[tt], ss4_t[tt])
                dv("scalar_tensor_tensor", out=xn4[tt], in0=xst4[tt], scalar=ss4[tt][:, 2:3], in1=gbc, op0=ALU.mult, op1=ALU.mult,
                   reads=[xst4_t[tt], ss4_t[tt], t_gbc], writes=[xn4_t[tt]])

            def norm_group(G):
                load_group(G)
                for tt in range(4):
                    norm_tile(G, tt)

            def transpose_group(G):
                for tt in range(4):
                    transpose16(xn4[tt], xn4_t[tt], hT[G % 2], hT_t[G % 2], tt * 128, (0, 1))

            if pss == 0:
                norm_group(0)
                transpose_group(0)
            for G in range(8):
                hb = hT[G % 2]
                hbt = hT_t[G % 2]
                if pss == 0:
                    if G + 1 < 8:
                        load_group(G + 1)
                    P.DMA("sp", hT_d[:, :, G * 512:(G + 1) * 512].rearrange("kc p t -> p kc t"), hb, [hbt], t_hd, sem_tile=hbt)
                else:
                    P.DMA("sp", hb, hT_d[:, :, G * 512:(G + 1) * 512].rearrange("kc p t -> p kc t"), [t_hd], hbt)
                for hd in range(8):
                    b = (2, 3, 7)[(G * 8 + hd) % 3]
                    for kc in range(16):
                        pe("matmul", psf(b), lhsT=W[:, kc, hd * 128:(hd + 1) * 128], rhs=hb[:, kc, :], start=(kc == 0), stop=(kc == 15),
                           reads=[t_W, hbt], writes=[pst[b]])
                    ks = kst[(G * 8 + hd) % 4]
                    kst_ = kst_t[(G * 8 + hd) % 4]
                    dv("tensor_copy", out=ks, in_=psf(b), reads=[pst[b]], writes=[kst_])
                    P.DMA("sp", kT_d[hd, :, G * 512:(G + 1) * 512], ks, [kst_], t_kd, sem_tile=kst_)
                    if pss == 0 and G + 1 < 8 and hd % 2 == 1:
                        norm_tile(G + 1, hd // 2)
                if pss == 0 and G + 1 < 8:
                    transpose_group(G + 1)
                if pss == 0 and G == 1 and stage >= 4:
                    for blk in range(NBLK):
                        P.DMA("sp", xb_d[blk * 128:(blk + 1) * 128, :], zero_b, TC, t_xbz)
                for tt in range(4):
                    vs = vst[(G * 4 + tt) % 3]
                    vs_t = vst_t[(G * 4 + tt) % 3]
                    for cg in range(2):
                        b = 4 + cg
                        for kc in range(16):
                            pe("matmul", psf(b), lhsT=hb[:, kc, tt * 128:(tt + 1) * 128], rhs=W[:, kc, 1024 + cg * 512:1024 + (cg + 1) * 512],
                               start=(kc == 0), stop=(kc == 15), reads=[t_W, hbt], writes=[pst[b]])
                        act("activation", out=vs[:, cg * 512:(cg + 1) * 512], in_=psf(b), func=AF.Copy, reads=[pst[b]], writes=[vs_t])
                    r0 = (G * 4 + tt) * 128
                    P.DMA("sp", v_d[r0:r0 + 128, :], vs, [vs_t], t_vd, sem_tile=vs_t)
                    if pss == 0:
                        for kc in range(16):
                            pe("matmul", psf(6)[:, 0:8], lhsT=hb[:, kc, tt * 128:(tt + 1) * 128], rhs=Wf[:, kc, :],
                               start=(kc == 0), stop=(kc == 15), reads=[t_Wf, hbt], writes=[pst[6]])
                        j = G * 4 + tt
                        dv("tensor_tensor", out=fl_all[:, j * 8:(j + 1) * 8], in0=psf(6)[:, 0:8], in1=c_bf, op=ALU.add,
                           reads=[pst[6], t_const], writes=[t_fl])
            if pss == 0:
                act("activation", out=fl_all, in_=fl_all, func=AF.Sigmoid, reads=[t_fl], writes=[t_fl])
                act("activation", out=fl_all, in_=fl_all, func=AF.Ln, reads=[t_fl], writes=[t_fl])
                pe("matmul", psf(6)[:, 0:256], lhsT=c_trif, rhs=fl_all, start=True, stop=True, reads=[t_fl, t_const], writes=[pst[6]])
                pe("matmul", psf(7)[:, 0:256], lhsT=c_onesf, rhs=fl_all, start=True, stop=True, reads=[t_fl, t_const], writes=[pst[7]])
                ctot = A.alloc([8, NB], F32)
                cpre = A.alloc([8, NB], F32)
                cm = A.alloc([8, NB], F32)
                ctmp = A.alloc([8, NB], F32)
                dv("tensor_copy", out=ctot, in_=psf(7)[:, 0:256].rearrange("p (j h) -> p h j", h=8), reads=[pst[7]], writes=[t_c])
                for hd in range(8):
                    dv("tensor_tensor_scan", out=cpre[:, hd, :], data0=c_onesf[:, 0:NB], data1=ctot[:, hd, :], initial=0.0,
                       op0=ALU.mult, op1=ALU.add, reads=[t_c, t_const], writes=[t_c])
                dv("tensor_sub", out=cpre, in0=cpre, in1=ctot, reads=[t_c], writes=[t_c])
                dv("tensor_tensor", out=c_all, in0=psf(6)[:, 0:256].rearrange("p (j h) -> p h j", h=8), in1=cpre, op=ALU.add,
                   reads=[pst[6], t_c], writes=[t_c])
                dv("scalar_tensor_tensor", out=cm, in0=ctot, scalar=0.5, in1=cpre, op0=ALU.mult, op1=ALU.add, reads=[t_c], writes=[t_c])
                for k in range(NSLOT):
                    dv("tensor_tensor", out=ctmp, in0=cm, in1=c_onehot[:, k:k + 1, :].broadcast_to([128, 8, NB]), op=ALU.mult,
                       reads=[t_c, t_const], writes=[t_c])
                    dv("tensor_reduce", out=cbias[:, :, k], in_=ctmp, axis=AX.X, op=ALU.add, reads=[t_c], writes=[t_c])
            P.barrier()

        if stage == 1:
            return finish_debug(nc, P, dt, [("c_all", c_all, [128, 8, NB], F32), ("cbias", cbias, [128, 8, NSLOT], F32),
                                            ("m_le", m_le, [128, 32, 128], BF16),
                                            ("kaT", kaT_d[0], [128, S], BF16), ("kbT", kbT_d[7], [128, S], BF16),
                                            ("va", va_d[0:512, :], [512, 1024], BF16),
                                            ("vb", vb_d[3584:4096, :], [512, 1024], BF16)], [t_const, t_c] + kv_tiles)

        A.reset(persist_mark)
        hTo = A.alloc([16, TOWN], BF16)
        t_hTo = P.tile("hTo")
        oaT = A.alloc([8, TOWN], BF16)
        obT = A.alloc([8, TOWN], BF16)
        t_oa = P.tile("oaT")
        t_ob = P.tile("obT")
        mid_mark = A.mark()
        QT = A.alloc([16, TOWN], BF16)
        t_QT = P.tile("QT")
        att_mark = A.mark()
        Wq = A.alloc([16, 1024], BF16)
        t_Wq = P.tile("Wq")
        xst = [A.alloc([D], F32) for _ in range(2)]
        xst_t = [P.tile("xst%d" % i) for i in range(2)]
        xn = [A.alloc([D], BF16) for _ in range(2)]
        xn_t = [P.tile("xn%d" % i) for i in range(2)]
        ssb = [A.alloc([4], F32) for _ in range(2)]
        ss_t = [P.tile("ss%d" % i) for i in range(2)]
        for i in range(8):
            norm_transpose(xown[i * 128:(i + 1) * 128, :], gbc, t_gbc, xst[i % 2], xst_t[i % 2], xn[i % 2], xn_t[i % 2], ssb[i % 2], ss_t[i % 2],
                           hTo, t_hTo, i * 128, (0, 1))
        for part, c0 in ((0, 0), (1, 3080)):
            for kc8 in range(2):
                P.DMA("pool", Wq[:, kc8 * 8:(kc8 + 1) * 8, :],
                      w_in[kc8 * 1024:(kc8 + 1) * 1024, c0:c0 + 1024].rearrange("(kc p) n -> p kc n", p=128), [], t_Wq)
            for hh in range(8):
                hd = part * 8 + hh
                for half in range(2):
                    b = 2 + (hd * 2 + half) % 4
                    for kc in range(16):
                        pe("matmul", psf(b), lhsT=Wq[:, kc, hh * 128:(hh + 1) * 128], rhs=hTo[:, kc, half * 512:(half + 1) * 512],
                           start=(kc == 0), stop=(kc == 15), reads=[t_Wq, t_hTo], writes=[pst[b]])
                    if half == 0:
                        act("activation", out=QT[:, hd, 0:512], in_=psf(b), func=AF.Copy, scale=SCALE, reads=[pst[b]], writes=[t_QT])
                    else:
                        dv("tensor_scalar", QT[:, hd, 512:1024], psf(b), SCALE, None, ALU.mult, reads=[pst[b]], writes=[t_QT])
        P.barrier()

        A.reset(att_mark)
        KT = [A.alloc([S], BF16) for _ in range(2)]
        KT_t = [P.tile("KT%d" % i) for i in range(2)]
        Vt = [A.alloc([NB, 128], BF16) for _ in range(2)]
        V_t = [P.tile("V%d" % i) for i in range(2)]
        Pt = [A.alloc([TOWN], BF16) for _ in range(2)]
        Pt_t = [P.tile("Pt%d" % i) for i in range(2)]
        clampt = A.alloc([128], F32)
        t_clamp = P.tile("clamp")
        Lr = A.alloc([TOWN], F32)
        t_Lr = P.tile("Lr")
        e1 = [A.alloc([TOWN], F32) for _ in range(2)]
        e1_t = [P.tile("e1%d" % i) for i in range(2)]
        spb = [A.alloc([TOWN], BF16) for _ in range(2)]
        sp_t = [P.tile("sp%d" % i) for i in range(2)]
        Nsum = A.alloc([TOWN], BF16)
        t_Ns = P.tile("Nsum")

        def load_kv(hd, kT_d, v_d, slot):
            P.DMA("sp", KT[slot], kT_d[hd], list(kv_tiles), KT_t[slot])
            P.DMA("sp", Vt[slot], v_d[:, hd * 128:(hd + 1) * 128].rearrange("(kb p) d -> p kb d", p=128), list(kv_tiles), V_t[slot])

        def col_splits(c0):
            out = []
            if c0 < 512:
                out.append((0, c0, 512))
                out.append((1, 512, 1024))
            else:
                out.append((1, c0, 1024))
            return out

        negc = [A.alloc([NB], F32) for _ in range(2)]
        negc_t = [P.tile("negc%d" % i) for i in range(2)]
        clampb = [clampt, A.alloc([128], F32)]
        clamp_t = [t_clamp, P.tile("clamp1")]

        def load_head(H):
            if H >= 16:
                return
            if H < 8:
                load_kv(H, kaT_d, va_d, H % 2)
            else:
                load_kv(H - 8, kbT_d, vb_d, H % 2)

        load_head(0)
        load_head(1)

        fox_iters = [(hd, kb) for hd in range(8) for kb in range(NB)]

        Pacc = A.alloc([TOWN], F32)
        t_Pacc = P.tile("Pacc")

        crow = [A.alloc([TOWN], BF16) for _ in range(2)]
        crow_t = [P.tile("crow%d" % i) for i in range(2)]

        def fox_A(j):
            hd, kb = fox_iters[j]
            sl = hd % 2
            if kb == 0:
                dv("tensor_scalar", negc[sl], c_all[:, hd, :], -1.0, None, ALU.mult, reads=[t_c], writes=[negc_t[sl]])
                dv("tensor_copy", out=crow[sl][0:1, :].rearrange("p (k c) -> p k c", k=NSLOT),
                   in_=cbias[0:1, hd, :].rearrange("p (k o) -> p k o", o=1).broadcast_to([1, NSLOT, 128]),
                   reads=[t_c], writes=[crow_t[sl]])
            c0 = (kb // 4) * 128
            sb = (j % 3) * 2
            for (bh, lo, hi) in col_splits(c0):
                dst = psf(sb + bh)[:, lo - bh * 512:hi - bh * 512]
                pe("matmul", dst, lhsT=KT[sl][:, kb * 128:(kb + 1) * 128], rhs=QT[:, hd, lo:hi],
                   start=True, stop=False, skip_group_check=True, reads=[KT_t[sl], t_QT], writes=[pst[sb + bh]])
                pe("matmul", dst, lhsT=c_ones[0:1, :], rhs=crow[sl][0:1, lo:hi],
                   start=False, stop=True, skip_group_check=True, reads=[t_const, crow_t[sl]], writes=[pst[sb + bh]])

        def fox_E1(j):
            hd, kb = fox_iters[j]
            sl = hd % 2
            g = kb // 4
            c0 = g * 128
            sb = (j % 3) * 2
            pt, ptt = Pt[j % 2], Pt_t[j % 2]
            cl, clt = clampb[j % 2], clamp_t[j % 2]
            bh = g // 4
            src = psf(sb + bh)[:, (g % 4) * 128:(g % 4 + 1) * 128]
            dv("tensor_scalar", cl, src, negc[sl][:, kb:kb + 1], 60.0, ALU.add, ALU.min, reads=[pst[sb + bh], negc_t[sl]], writes=[clt])
            act("activation", out=pt[:, c0:c0 + 128], in_=cl, func=AF.Exp, reads=[clt], writes=[ptt])
            if c0 + 128 < TOWN:
                for (bh2, lo, hi) in col_splits(c0 + 128):
                    act("activation", out=pt[:, lo:hi], in_=psf(sb + bh2)[:, lo - bh2 * 512:hi - bh2 * 512], func=AF.Exp,
                        bias=negc[sl][:, kb:kb + 1], reads=[pst[sb + bh2], negc_t[sl]], writes=[ptt])

        def fox_E2(j):
            hd, kb = fox_iters[j]
            c0 = (kb // 4) * 128
            pt, ptt = Pt[j % 2], Pt_t[j % 2]
            dv("tensor_mul", out=pt[:, c0:c0 + 128], in0=pt[:, c0:c0 + 128], in1=m_le[:, kb, :], reads=[ptt, t_const], writes=[ptt])
            if kb == 0:
                dv("tensor_copy", out=Pacc[:, 0:512], in_=pt[:, 0:512], reads=[ptt], writes=[t_Pacc])
                P.I("pool", "tensor_copy", out=Pacc[:, 512:TOWN], in_=pt[:, 512:TOWN], reads=[ptt], writes=[t_Pacc])
            else:
                if c0 < 512:
                    dv("tensor_tensor", out=Pacc[:, c0:512], in0=Pacc[:, c0:512], in1=pt[:, c0:512], op=ALU.add, reads=[ptt, t_Pacc], writes=[t_Pacc])
                lo2 = max(c0, 512)
                P.I("pool", "tensor_tensor", out=Pacc[:, lo2:TOWN], in0=Pacc[:, lo2:TOWN], in1=pt[:, lo2:TOWN], op=ALU.add,
                    reads=[ptt, t_Pacc], writes=[t_Pacc])

        def fox_C(j):
            hd, kb = fox_iters[j]
            sl = hd % 2
            c0 = (kb // 4) * 128
            pt, ptt = Pt[j % 2], Pt_t[j % 2]
            for (bh, lo, hi) in col_splits(c0):
                pe("matmul", psf(6 + bh)[:, lo - bh * 512:hi - bh * 512], lhsT=Vt[sl][:, kb, :], rhs=pt[:, lo:hi],
                   start=(kb == 0), stop=(kb == NB - 1), skip_group_check=True, reads=[V_t[sl], ptt], writes=[pst[6 + bh]])
            if kb == NB - 1:
                for bh in range(2):
                    lb = ((j + 1) % 3) * 2 + bh
                    pe("matmul", psf(lb), lhsT=c_onesf, rhs=Pacc[:, bh * 512:(bh + 1) * 512], start=True, stop=True,
                       reads=[t_const, t_Pacc], writes=[pst[lb]])
                    dv("reciprocal", Lr[:, bh * 512:(bh + 1) * 512], psf(lb), reads=[pst[lb]], writes=[t_Lr])
                    dv("tensor_tensor", out=oaT[:, hd, bh * 512:(bh + 1) * 512], in0=psf(6 + bh), in1=Lr[:, bh * 512:(bh + 1) * 512], op=ALU.mult,
                       reads=[pst[6 + bh], t_Lr], writes=[t_oa])
                load_head(hd + 2)

        nfx = len(fox_iters)
        fox_A(0)
        fox_A(1)
        fox_E1(0)
        for j in range(nfx):
            if j + 2 < nfx:
                fox_A(j + 2)
            if j + 1 < nfx:
                fox_E1(j + 1)
            fox_E2(j)
            fox_C(j)
            if j % 8 == 0:
                conv_step(1)

        sb_iters = [(hd, kb) for hd in range(8) for kb in range(NB - 1, -1, -1)]
        first_o = {}

        def sb_A(j):
            hd, kb = sb_iters[j]
            sl = hd % 2
            c0 = (kb // 4) * 128
            sb = (j % 3) * 2
            for (bh, lo, hi) in col_splits(c0):
                pe("matmul", psf(sb + bh)[:, lo - bh * 512:hi - bh * 512], lhsT=KT[sl][:, kb * 128:(kb + 1) * 128], rhs=QT[:, 8 + hd, lo:hi],
                   start=True, stop=False, skip_group_check=True, reads=[KT_t[sl], t_QT], writes=[pst[sb + bh]])

        def sb_S(j):
            hd, kb = sb_iters[j]
            c0 = (kb // 4) * 128
            sb = (j % 3) * 2
            ef, eft = e1[j % 2], e1_t[j % 2]
            spk, spt = spb[j % 2], sp_t[j % 2]
            for (bh, lo, hi) in col_splits(c0):
                act("activation", out=ef[:, lo:hi], in_=psf(sb + bh)[:, lo - bh * 512:hi - bh * 512], func=AF.Exp,
                    reads=[pst[sb + bh]], writes=[eft])
            act("activation", out=spk[:, c0:1024], in_=ef[:, c0:1024], func=AF.Ln, bias=1.0, reads=[eft], writes=[spt])
            dv("tensor_mul", out=spk[:, c0:c0 + 128], in0=spk[:, c0:c0 + 128], in1=m_lt[:, kb, :], reads=[spt, t_const], writes=[spt])

        def sb_B(j):
            hd, kb = sb_iters[j]
            c0 = (kb // 4) * 128
            sb = (j % 3) * 2
            spk, spt = spb[j % 2], sp_t[j % 2]
            if kb == NB - 1:
                P.I("pool", "memset", Nsum, 0.0, writes=[t_Ns])
            for (bh, lo, hi) in col_splits(c0):
                dst = psf(sb + bh)[:, lo - bh * 512:hi - bh * 512]
                pe("matmul", dst, lhsT=c_ntri, rhs=spk[:, lo:hi], start=False, stop=False, skip_group_check=True,
                   reads=[t_const, spt], writes=[pst[sb + bh]])
                pe("matmul", dst, lhsT=c_nones, rhs=Nsum[:, lo:hi], start=False, stop=True, skip_group_check=True,
                   reads=[t_const, t_Ns], writes=[pst[sb + bh]])

        def sb_X(j):
            hd, kb = sb_iters[j]
            c0 = (kb // 4) * 128
            sb = (j % 3) * 2
            spk, spt = spb[j % 2], sp_t[j % 2]
            at, att = Pt[j % 2], Pt_t[j % 2]
            for (bh, lo, hi) in col_splits(c0):
                act("activation", out=at[:, lo:hi], in_=psf(sb + bh)[:, lo - bh * 512:hi - bh * 512], func=AF.Exp,
                    reads=[pst[sb + bh]], writes=[att])
            dv("tensor_mul", out=at[:, c0:c0 + 128], in0=at[:, c0:c0 + 128], in1=m_lt[:, kb, :], reads=[att, t_const], writes=[att])
            P.I("pool", "tensor_tensor", out=Nsum[:, c0:1024], in0=Nsum[:, c0:1024], in1=spk[:, c0:1024], op=ALU.add,
                reads=[t_Ns, spt], writes=[t_Ns])

        def sb_C(j):
            hd, kb = sb_iters[j]
            sl = hd % 2
            c0 = (kb // 4) * 128
            at, att = Pt[j % 2], Pt_t[j % 2]
            for (bh, lo, hi) in col_splits(c0):
                fo = first_o.get((hd, bh), True)
                pe("matmul", psf(6 + bh)[:, lo - bh * 512:hi - bh * 512], lhsT=Vt[sl][:, kb, :], rhs=at[:, lo:hi],
                   start=fo, stop=(kb == 0), skip_group_check=True, reads=[V_t[sl], att], writes=[pst[6 + bh]])
                first_o[(hd, bh)] = False
            if kb == 0:
                act("activation", out=obT[:, hd, 0:512], in_=psf(6), func=AF.Copy, reads=[pst[6]], writes=[t_ob])
                dv("tensor_copy", out=obT[:, hd, 512:1024], in_=psf(7), reads=[pst[7]], writes=[t_ob])
                load_head(8 + hd + 2)

        nsb = len(sb_iters)
        sb_A(0)
        sb_S(0)
        sb_A(1)
        for j in range(nsb):
            sb_B(j)
            if j + 2 < nsb:
                sb_A(j + 2)
            if j + 1 < nsb:
                sb_S(j + 1)
            if j >= 1:
                sb_C(j - 1)
            sb_X(j)
            if j % 6 == 0:
                conv_step(1)
        sb_C(nsb - 1)
        P.barrier()
        if stage == 2:
            return finish_debug(nc, P, dt, [("oaT", oaT, [128, 8, TOWN], BF16), ("obT", obT, [128, 8, TOWN], BF16)], [t_oa, t_ob])

        A.reset(mid_mark)
        mixedT = A.alloc([16, TOWN], BF16)
        t_mx = P.tile("mixedT")
        p6_mark = A.mark()
        Wga = [A.alloc([16, 256], BF16) for _ in range(2)]
        Wgb = [A.alloc([16, 256], BF16) for _ in range(2)]
        Wfx = [A.alloc([8, 256], BF16) for _ in range(2)]
        Wsb = [A.alloc([8, 256], BF16) for _ in range(2)]
        Wg_t = [P.tile("Wg6_%d" % i) for i in range(2)]
        sa = [A.alloc([512], F32) for _ in range(2)]
        sb_ = [A.alloc([512], F32) for _ in range(2)]
        sg_t = [P.tile("sg%d" % i) for i in range(2)]

        def load_w6(ng, s):
            P.DMA("pool", Wga[s], w_in[:, 6152 + ng * 256:6152 + (ng + 1) * 256].rearrange("(kc p) n -> p kc n", p=128), [], Wg_t[s])
            P.DMA("pool", Wgb[s], w_in[:, 8200 + ng * 256:8200 + (ng + 1) * 256].rearrange("(kc p) n -> p kc n", p=128), [], Wg_t[s])
            P.DMA("pool", Wfx[s], w_bfox[:, ng * 256:(ng + 1) * 256].rearrange("(kc p) n -> p kc n", p=128), [], Wg_t[s])
            P.DMA("pool", Wsb[s], w_bsb[:, ng * 256:(ng + 1) * 256].rearrange("(kc p) n -> p kc n", p=128), [], Wg_t[s])

        load_w6(0, 0)
        it6 = 0
        for ng in range(8):
            s = ng % 2
            if ng + 1 < 8:
                load_w6(ng + 1, (ng + 1) % 2)
            for nsub in range(2):
                nch = ng * 2 + nsub
                for half in range(2):
                    pb = (it6 % 2) * 4
                    sa_, sbb, sgt = sa[it6 % 2], sb_[it6 % 2], sg_t[it6 % 2]
                    it6 += 1
                    cs = slice(half * 512, (half + 1) * 512)
                    ns = slice(nsub * 128, (nsub + 1) * 128)
                    for kc in range(16):
                        pe("matmul", psf(pb), lhsT=Wga[s][:, kc, ns], rhs=hTo[:, kc, cs], start=(kc == 0), stop=(kc == 15),
                           reads=[Wg_t[s], t_hTo], writes=[pst[pb]])
                    for kc in range(16):
                        pe("matmul", psf(pb + 1), lhsT=Wgb[s][:, kc, ns], rhs=hTo[:, kc, cs], start=(kc == 0), stop=(kc == 15),
                           reads=[Wg_t[s], t_hTo], writes=[pst[pb + 1]])
                    for hc in range(8):
                        pe("matmul", psf(pb + 2), lhsT=Wfx[s][:, hc, ns], rhs=oaT[:, hc, cs], start=(hc == 0), stop=(hc == 7),
                           reads=[Wg_t[s], t_oa], writes=[pst[pb + 2]])
                    for hc in range(8):
                        pe("matmul", psf(pb + 3), lhsT=Wsb[s][:, hc, ns], rhs=obT[:, hc, cs], start=(hc == 0), stop=(hc == 7),
                           reads=[Wg_t[s], t_ob], writes=[pst[pb + 3]])
                    act("activation", out=sa_, in_=psf(pb), func=AF.Sigmoid, reads=[pst[pb]], writes=[sgt])
                    act("activation", out=sbb, in_=psf(pb + 1), func=AF.Sigmoid, reads=[pst[pb + 1]], writes=[sgt])
                    dv("tensor_tensor", out=sa_, in0=sa_, in1=psf(pb + 2), op=ALU.mult, reads=[sgt, pst[pb + 2]], writes=[sgt])
                    dv("tensor_tensor", out=sbb, in0=sbb, in1=psf(pb + 3), op=ALU.mult, reads=[sgt, pst[pb + 3]], writes=[sgt])
                    P.I("pool", "tensor_tensor", out=mixedT[:, nch, cs], in0=sa_, in1=sbb, op=ALU.add, reads=[sgt], writes=[t_mx])
                    if it6 % 2 == 0:
                        conv_step(1)
        P.barrier()
        A.reset(p6_mark)
        Wm = [A.alloc([16, 512], BF16) for _ in range(2)]
        Wm_t = [P.tile("Wm%d" % i) for i in range(2)]
        xo = [A.alloc([512], F32) for _ in range(3)]
        xo_t = [P.tile("xo%d" % i) for i in range(3)]
        x1s = [A.alloc([512], F32) for _ in range(2)]
        x1s_t = [P.tile("x1s%d" % i) for i in range(2)]
        t_x1d = P.tile("x1_d")
        P.DMA("pool", Wm[0], w_mix[:, 0:512].rearrange("(kc p) n -> p kc n", p=128), [], Wm_t[0])
        mo_iters = [(cg, i) for cg in range(4) for i in range(8)]

        def xo_load(n):
            cg, i = mo_iters[n]
            P.DMA("sp", xo[n % 3], xown[i * 128:(i + 1) * 128, cg * 512:(cg + 1) * 512], [], xo_t[n % 3])

        xo_load(0)
        xo_load(1)
        for n, (cg, i) in enumerate(mo_iters):
            s_ = cg % 2
            if i == 0 and cg + 1 < 4:
                P.DMA("pool", Wm[(cg + 1) % 2], w_mix[:, (cg + 1) * 512:(cg + 2) * 512].rearrange("(kc p) n -> p kc n", p=128), [], Wm_t[(cg + 1) % 2])
            if n + 2 < len(mo_iters):
                xo_load(n + 2)
            b = n % 4
            u = n % 2
            for kc in range(16):
                pe("matmul", psf(b), lhsT=mixedT[:, kc, i * 128:(i + 1) * 128], rhs=Wm[s_][:, kc, :], start=(kc == 0), stop=(kc == 15),
                   reads=[t_mx, Wm_t[s_]], writes=[pst[b]])
            dv("tensor_tensor", out=x1s[u], in0=psf(b), in1=xo[n % 3], op=ALU.add, reads=[pst[b], xo_t[n % 3]], writes=[x1s_t[u]])
            P.DMA("sp", x1_d[i * 128:(i + 1) * 128, cg * 512:(cg + 1) * 512], x1s[u], [x1s_t[u]], t_x1d, sem_tile=x1s_t[u])
        conv_flush()
        P.barrier()
        if stage == 3:
            return finish_debug(nc, P, dt, [("x1", x1_d, [TOWN, D], F32)], [t_x1d])
        A.reset(core_mark)
        Wr = A.alloc([16, 36], F32)
        t_Wr = P.tile("Wr")
        P.DMA("sp", Wr, w_rout.rearrange("(kc p) n -> p kc n", p=128), [], t_Wr)
        Eall = A.alloc([NSLOT, 2, 32], F32)
        gall = A.alloc([NSLOT, 2], F32)
        Esum = A.alloc([NSLOT, 32], F32)
        Call = A.alloc([NSLOT, 32], F32)
        dest_f = A.alloc([NSLOT, 2], F32)
        dest_i = A.alloc([NSLOT, 2], I32)
        idxw_f = A.alloc([NBLK, 4], F32)
        idxw_i = A.alloc([NBLK, 4], I32)
        t_rt = P.tile("route")
        moe_mark = A.mark()
        gbc = A.alloc([D], F32)
        t_gbc = P.tile("gbc7")
        P.DMA("sp", gbc, bcast_row(g_ffn), [], t_gbc)
        h2b = A.alloc([NSLOT, D], BF16)
        t_h2b = P.tile("h2b")
        xst = [A.alloc([D], F32) for _ in range(2)]
        xst_t = [P.tile("x1t%d" % i) for i in range(2)]
        junk = A.alloc([D], BF16)
        junk_t = P.tile("junk")
        ssb = [A.alloc([4], F32) for _ in range(2)]
        ss_t = [P.tile("ss%d" % i) for i in range(2)]
        h2f = A.alloc([D], F32)
        t_h2f = P.tile("h2f")
        h2fT = A.alloc([16, 128], F32)
        t_h2fT = P.tile("h2fT")
        lg = A.alloc([36], F32)
        rs = A.alloc([16], F32)
        goh = A.alloc([4], F32)
        j4 = A.alloc([4], F32)
        t48 = A.alloc([4, 8], F32)
        esel = A.alloc([8], F32)
        v8 = A.alloc([8], F32)
        oh1 = A.alloc([8], F32)
        oh2 = A.alloc([8], F32)
        t_r = P.tile("rtmp")
        TR = [t_r]
        for i in range(NSLOT):
            u = i % 2
            P.DMA("sp", xst[u], x1_d[i * 128:(i + 1) * 128, :], [t_x1d], xst_t[u])
            norm_rstd(xst[u], xst_t[u], junk, junk_t, ssb[u], ss_t[u])
            dv("scalar_tensor_tensor", out=h2f, in0=xst[u], scalar=ssb[u][:, 2:3], in1=gbc, op0=ALU.mult, op1=ALU.mult,
               reads=[xst_t[u], ss_t[u], t_gbc], writes=[t_h2f])
            act("activation", out=h2b[:, i, :], in_=h2f, func=AF.Copy, reads=[t_h2f], writes=[t_h2b])
            for q4 in range(4):
                for q in range(4):
                    kc = q4 * 4 + q
                    pe("transpose", psf(q4)[:, q * 128:(q + 1) * 128], h2f[:, kc * 128:(kc + 1) * 128], c_identf,
                       reads=[t_h2f, t_const], writes=[pst[q4]])
                o_ = h2fT[:, q4 * 4:(q4 + 1) * 4, :]
                i_ = psf(q4).rearrange("p (a b) -> p a b", a=4)
                if q4 % 2 == 0:
                    act("activation", out=o_, in_=i_, func=AF.Copy, reads=[pst[q4]], writes=[t_h2fT])
                else:
                    dv("tensor_copy", out=o_, in_=i_, reads=[pst[q4]], writes=[t_h2fT])
            for kc in range(16):
                pe("matmul", psf(4)[:, 0:36], lhsT=h2fT[:, kc, :], rhs=Wr[:, kc, :], start=(kc == 0), stop=(kc == 15),
                   reads=[t_h2fT, t_Wr], writes=[pst[4]])
            dv("tensor_copy", out=lg, in_=psf(4)[:, 0:36], reads=[pst[4]], writes=TR)
            dv("tensor_reduce", out=rs[:, 0:1], in_=lg[:, 0:4], axis=AX.X, op=ALU.max, reads=TR, writes=TR)
            dv("tensor_scalar", goh, lg[:, 0:4], rs[:, 0:1], None, ALU.is_equal, reads=TR, writes=TR)
            dv("tensor_scalar", rs[:, 1:2], rs[:, 0:1], -1.0, None, ALU.mult, reads=TR, writes=TR)
            act("activation", out=j4, in_=lg[:, 0:4], func=AF.Exp, bias=rs[:, 1:2], accum_out=rs[:, 2:3], reads=TR, writes=TR)
            dv("reciprocal", rs[:, 3:4], rs[:, 2:3], reads=TR, writes=TR)
            dv("tensor_tensor", out=t48, in0=lg[:, 4:36].rearrange("p (g e) -> p g e", g=4),
               in1=goh.rearrange("p (g o) -> p g o", o=1).broadcast_to([128, 4, 8]), op=ALU.mult, reads=TR, writes=TR)
            dv("tensor_reduce", out=esel, in_=t48.rearrange("p g e -> p e g"), axis=AX.X, op=ALU.add, reads=TR, writes=TR)
            dv("max", v8, esel, reads=TR, writes=TR)
            dv("tensor_scalar", oh1, esel, v8[:, 0:1], None, ALU.is_equal, reads=TR, writes=TR)
            dv("tensor_scalar", oh2, esel, v8[:, 1:2], None, ALU.is_equal, reads=TR, writes=TR)
            dv("tensor_sub", out=rs[:, 4:5], in0=v8[:, 1:2], in1=v8[:, 0:1], reads=TR, writes=TR)
            act("activation", out=rs[:, 5:6], in_=rs[:, 4:5], func=AF.Exp, reads=TR, writes=TR)
            dv("tensor_scalar", rs[:, 6:7], rs[:, 5:6], 1.0, None, ALU.add, reads=TR, writes=TR)
            dv("reciprocal", rs[:, 7:8], rs[:, 6:7], reads=TR, writes=TR)
            dv("tensor_mul", out=gall[:, i, 0:1], in0=rs[:, 7:8], in1=rs[:, 3:4], reads=TR, writes=[t_rt])
            dv("tensor_mul", out=gall[:, i, 1:2], in0=rs[:, 5:6], in1=gall[:, i, 0:1], reads=TR + [t_rt], writes=[t_rt])
            for k, oh in ((0, oh1), (1, oh2)):
                dv("tensor_tensor", out=Eall[:, i, k, :].rearrange("p (g e) -> p g e", g=4),
                   in0=goh.rearrange("p (g o) -> p g o", o=1).broadcast_to([128, 4, 8]),
                   in1=oh.rearrange("p (o e) -> p o e", o=1).broadcast_to([128, 4, 8]), op=ALU.mult, reads=TR, writes=[t_rt])
            dv("tensor_add", out=Esum[:, i, :], in0=Eall[:, i, 0, :], in1=Eall[:, i, 1, :], reads=[t_rt], writes=[t_rt])
        for i in range(NSLOT):
            b = 5 + i % 2
            for i2 in range(i):
                pe("matmul", psf(b)[:, 0:32], lhsT=c_onesf, rhs=Esum[:, i2, :], start=(i2 == 0), stop=False,
                   reads=[t_rt, t_const], writes=[pst[b]])
            pe("matmul", psf(b)[:, 0:32], lhsT=c_strif, rhs=Esum[:, i, :], start=(i == 0), stop=True,
               reads=[t_rt, t_const], writes=[pst[b]])
            dv("tensor_copy", out=Call[:, i, :], in_=psf(b)[:, 0:32], reads=[pst[b]], writes=[t_rt])
        for i in range(NSLOT):
            pe("matmul", psf(7)[:, 0:32], lhsT=c_onesf, rhs=Esum[:, i, :], start=(i == 0), stop=(i == NSLOT - 1),
               reads=[t_rt, t_const], writes=[pst[7]])
        cnt = A.alloc([32], F32)
        nbk = A.alloc([32], F32)
        pend = A.alloc([32], F32)
        pstart = A.alloc([32], F32)
        Dall = A.alloc([NSLOT, 32], F32)
        tmpE = A.alloc([NSLOT, 32], F32)
        tmpb = A.alloc([NBLK, 32], F32)
        bef = A.alloc([NBLK], F32)
        nef = A.alloc([NBLK], F32)
        dv("tensor_copy", out=cnt, in_=psf(7)[:, 0:32], reads=[pst[7]], writes=TR)
        dv("tensor_single_scalar", nbk, cnt, 0.0, ALU.is_gt, reads=TR, writes=TR)
        for j in range(1, 8):
            dv("scalar_tensor_tensor", out=nbk, in0=cnt, scalar=128.0 * j, in1=nbk, op0=ALU.is_gt, op1=ALU.add, reads=TR, writes=TR)
        dv("tensor_tensor_scan", out=pend, data0=c_onesf[:, 0:32], data1=nbk, initial=0.0, op0=ALU.mult, op1=ALU.add,
           reads=TR + [t_const], writes=TR)
        dv("tensor_scalar", pend, pend, 128.0, None, ALU.mult, reads=TR, writes=TR)
        dv("scalar_tensor_tensor", out=pstart, in0=nbk, scalar=-128.0, in1=pend, op0=ALU.mult, op1=ALU.add, reads=TR, writes=TR)
        dv("tensor_tensor", out=Dall, in0=Call, in1=pstart.rearrange("p (o e) -> p o e", o=1).broadcast_to([128, NSLOT, 32]), op=ALU.add,
           reads=TR + [t_rt], writes=TR)
        for k in range(2):
            dv("tensor_tensor", out=tmpE, in0=Eall[:, :, k, :], in1=Dall, op=ALU.mult, reads=TR + [t_rt], writes=TR)
            dv("tensor_reduce", out=dest_f[:, :, k], in_=tmpE, axis=AX.X, op=ALU.add, reads=TR, writes=[t_rt])
        dv("tensor_copy", out=dest_i, in_=dest_f, reads=[t_rt], writes=[t_rt])
        dv("tensor_tensor", out=tmpb, in0=pend.rearrange("p (o e) -> p o e", o=1).broadcast_to([128, NBLK, 32]),
           in1=c_j128.rearrange("p (b o) -> p b o", o=1).broadcast_to([128, NBLK, 32]), op=ALU.is_le, reads=TR + [t_const], writes=TR)
        dv("tensor_reduce", out=bef, in_=tmpb, axis=AX.X, op=ALU.add, reads=TR, writes=TR)
        dv("tensor_scalar", bef, bef, 31.0, None, ALU.min, reads=TR, writes=TR)
        dv("tensor_scalar", nef, c_j128, pend[:, 31:32], None, ALU.is_lt, reads=TR + [t_const], writes=TR)
        BIG = 1.0e6
        dv("tensor_scalar", nef, nef, -1.0, -BIG, ALU.add, ALU.mult, reads=TR, writes=TR)
        dv("tensor_scalar", bef, bef, 512.0, c_pidx[:, 0:1], ALU.mult, ALU.add, reads=TR + [t_const], writes=TR)
        dv("tensor_add", out=bef, in0=bef, in1=nef, reads=TR, writes=TR)
        for m in range(4):
            dv("tensor_scalar", idxw_f[:, :, m], bef, 128.0 * m, None, ALU.add, reads=TR, writes=TR)
        dv("tensor_copy", out=idxw_i, in_=idxw_f, reads=TR, writes=[t_rt])
        for i in range(NSLOT):
            for k in range(2):
                P.op("pool", ("indirect_dma_start", (), dict(out=xb_d, out_offset=bass.IndirectOffsetOnAxis(ap=dest_i[:, i, k:k + 1], axis=0),
                                                             in_=h2b[:, i, :], in_offset=None)),
                     reads=[t_h2b, t_rt, t_xbz], writes=[t_xbd], dma_out=t_xbd)
        P.barrier()
        if stage == 35:
            return finish_debug(nc, P, dt, [("dest", dest_i, [128, NSLOT, 2], I32), ("gall", gall, [128, NSLOT, 2], F32),
                                            ("idxw", idxw_i, [128, NBLK, 4], I32), ("Eall", Eall, [128, NSLOT, 2, 32], F32),
                                            ("xb", xb_d, [NBLK * 128, D], BF16)], [t_rt, t_xbd])

        A.reset(moe_mark)
        NWB = 3
        order_ = []
        for q_ in range(16):
            order_ += [q_, 32 + q_]
        order_ += list(range(16, 32))
        WG = [A.alloc([4, 2048], BF16) for _ in range(NWB)]
        WU = [A.alloc([4, 2048], BF16) for _ in range(NWB)]
        WD = [A.alloc([4, 2048], BF16) for _ in range(NWB)]
        W8_t = [P.tile("W8_%d" % i) for i in range(NWB)]
        xbs = [A.alloc([D], BF16) for _ in range(4)]
        xbs_t = [P.tile("xbs%d" % i) for i in range(4)]
        xbT = [A.alloc([16, 128], BF16) for _ in range(2)]
        xbT_t = [P.tile("xbT%d" % i) for i in range(2)]
        sgf = A.alloc([512], F32)
        t_sgf = P.tile("sgf")
        hid = A.alloc([512], BF16)
        t_hid = P.tile("hid")
        hidT = A.alloc([4, 128], BF16)
        t_hidT = P.tile("hidT")
        yst = [A.alloc([D], F32) for _ in range(2)]
        yst_t = [P.tile("yst%d" % i) for i in range(2)]
        t_ybd = P.tile("yb_d")

        def load_w8(n_, s):
            blk = order_[n_]
            for (wsrc, wdst) in ((wb_d[0], WG[s]), (wb_d[1], WU[s]), (wb_d[2], WD[s])):
                for m in range(4):
                    P.op("pool", ("indirect_dma_start", (), dict(out=wdst[:, m, :], out_offset=None, in_=wsrc,
                                                                 in_offset=bass.IndirectOffsetOnAxis(ap=idxw_i[:, blk, m:m + 1], axis=0),
                                                                 bounds_check=NE * 512 - 1, oob_is_err=False)),
                         reads=[t_rt, t_wb], writes=[W8_t[s]], dma_out=W8_t[s])

        sgf2 = [sgf, A.alloc([512], F32)]
        sgf2_t = [t_sgf, P.tile("sgf1")]
        hid2 = [hid, A.alloc([512], BF16)]
        hid2_t = [t_hid, P.tile("hid1")]
        hidT2 = [hidT, A.alloc([4, 128], BF16)]
        hidT2_t = [t_hidT, P.tile("hidT1")]

        def moe_L(n_):
            pb_ = order_[n_]
            P.DMA("sp", xbs[n_ % 4], xb_d[pb_ * 128:(pb_ + 1) * 128, :], [t_xbd, t_xbz], xbs_t[n_ % 4])

        def moe_S1(blk):
            s = blk % 2
            if blk + 3 < NBLK:
                moe_L(blk + 3)
            for half in range(2):
                for q in range(8):
                    c = half * 8 + q
                    m, kk = c // 4, c % 4
                    pe("transpose", psb(half)[:, q * 128:(q + 1) * 128], xbs[blk % 4][:, 512 * m + kk:512 * (m + 1):4], c_identb,
                       reads=[xbs_t[blk % 4], t_const], writes=[pst[half]])
                o_ = xbT[s][:, half * 8:(half + 1) * 8, :]
                i_ = psb(half).rearrange("p (a b) -> p a b", a=8)
                if half == 0:
                    act("activation", out=o_, in_=i_, func=AF.Copy, reads=[pst[half]], writes=[xbT_t[s]])
                else:
                    dv("tensor_copy", out=o_, in_=i_, reads=[pst[half]], writes=[xbT_t[s]])

        def moe_S2(blk):
            s = blk % 2
            ws = blk % NWB
            for (wt, b_) in ((WG[ws], 2), (WU[ws], 3)):
                for c in range(16):
                    m, kk = c // 4, c % 4
                    pe("matmul", psf(b_), lhsT=xbT[s][:, c, :], rhs=wt[:, m, kk * 512:(kk + 1) * 512], start=(c == 0), stop=(c == 15),
                       reads=[xbT_t[s], W8_t[ws]], writes=[pst[b_]])
            act("activation", out=sgf2[s], in_=psf(2), func=AF.Silu, reads=[pst[2]], writes=[sgf2_t[s]])
            dv("tensor_tensor", out=hid2[s], in0=sgf2[s], in1=psf(3), op=ALU.mult, reads=[sgf2_t[s], pst[3]], writes=[hid2_t[s]])

        def moe_S3(blk):
            s = blk % 2
            for m in range(4):
                pe("transpose", psb(4)[:, m * 128:(m + 1) * 128], hid2[s][:, m * 128:(m + 1) * 128], c_identb,
                   reads=[hid2_t[s], t_const], writes=[pst[4]])
            dv("tensor_copy", out=hidT2[s], in_=psb(4)[:, 0:512].rearrange("p (a b) -> p a b", a=4), reads=[pst[4]], writes=[hidT2_t[s]])

        def moe_S4(blk):
            s = blk % 2
            ws = blk % NWB
            for cg in range(4):
                bk = 5 + (blk * 4 + cg) % 3
                for m in range(4):
                    pe("matmul", psf(bk), lhsT=hidT2[s][:, m, :], rhs=WD[ws][:, m, cg * 512:(cg + 1) * 512], start=(m == 0), stop=(m == 3),
                       reads=[hidT2_t[s], W8_t[ws]], writes=[pst[bk]])
                if cg % 2 == 0:
                    act("activation", out=yst[s][:, cg * 512:(cg + 1) * 512], in_=psf(bk), func=AF.Copy, reads=[pst[bk]], writes=[yst_t[s]])
                else:
                    dv("tensor_copy", out=yst[s][:, cg * 512:(cg + 1) * 512], in_=psf(bk), reads=[pst[bk]], writes=[yst_t[s]])
            pb_ = order_[blk]
            P.DMA("sp", yb_d[pb_ * 128:(pb_ + 1) * 128, :], yst[s], [yst_t[s]], t_ybd, sem_tile=yst_t[s])

        order = []
        for q in range(16):
            order += [q, 32 + q]
        order += list(range(16, 32))
        assert sorted(order) == list(range(NBLK))
        for n in range(3):
            moe_L(n)
        load_w8(0, 0)
        load_w8(1, 1)
        moe_S1(0)
        moe_S2(0)
        for n in range(NBLK):
            if n + 2 < NBLK:
                load_w8(n + 2, (n + 2) % NWB)
            if n + 1 < NBLK:
                moe_S1(n + 1)
            moe_S3(n)
            if n + 1 < NBLK:
                moe_S2(n + 1)
            moe_S4(n)
        P.barrier()

        A.reset(moe_mark)
        full = stage >= 5
        x1t = [A.alloc([D], F32) for _ in range(2)]
        x1t_t = [P.tile("x1t%d" % i) for i in range(2)]
        ya = [A.alloc([D], F32)] * 2
        ya_t = [P.tile("ya")] * 2
        yb2 = [A.alloc([D], F32)] * 2
        yb2_t = [P.tile("yb2")] * 2
        x2 = [A.alloc([D], F32) for _ in range(2)]
        x2_t = [P.tile("x2_%d" % i) for i in range(2)]
        t_dbg2 = P.tile("dbgx2")
        if not full:
            dbg_x2 = dt("dbg_x2", [TOWN, D], F32, kind="ExternalOutput").ap()
        if full:
            gfin = A.alloc([D], F32)
            t_gfin = P.tile("gfin")
            gbc = A.alloc([D], F32)
            t_gbc = P.tile("gbc9")
            P.DMA("sp", gbc, bcast_row(g_ple), [], t_gbc)
            P.DMA("sp", gfin, bcast_row(g_final), [], t_gfin)
            Wpg = A.alloc([16, D], BF16)
            Wpp = A.alloc([2, D], BF16)
            t_Wp = P.tile("Wp")
            for kc4 in range(4):
                P.DMA("pool", Wpg[:, kc4 * 4:(kc4 + 1) * 4, :], w_pg[kc4 * 512:(kc4 + 1) * 512, :].rearrange("(kc p) n -> p kc n", p=128), [], t_Wp)
            P.DMA("pool", Wpp, w_pp.rearrange("(kc p) n -> p kc n", p=128), [], t_Wp)
            h3 = [A.alloc([D], BF16) for _ in range(2)]
            h3_t = [P.tile("h3_%d" % i) for i in range(2)]
            h3T = [A.alloc([16, 128], BF16) for _ in range(2)]
            h3T_t = [P.tile("h3T%d" % i) for i in range(2)]
            ptl = A.alloc([256], F32)
            t_ptl = P.tile("ptl")
            pb16 = A.alloc([256], BF16)
            t_pb = P.tile("pb16")
            pT = [A.alloc([2, 128], BF16) for _ in range(2)]
            pT_t = [P.tile("pT%d" % i) for i in range(2)]
            sgp = [A.alloc([512], F32) for _ in range(2)]
            sgp_t = [P.tile("sgp%d" % i) for i in range(2)]
            x3 = [A.alloc([D], F32) for _ in range(2)]
            x3_t = [P.tile("x3_%d" % i) for i in range(2)]
            ot = [A.alloc([D], F32)] * 2
            ot_t = [P.tile("ot")] * 2
            ssc = [A.alloc([4], F32) for _ in range(2)]
            ssc_t = [P.tile("ssc%d" % i) for i in range(2)]
            ssd = [A.alloc([4], F32) for _ in range(2)]
            ssd_t = [P.tile("ssd%d" % i) for i in range(2)]
            t_y = P.tile("y")
        itp = [0]

        def tail_front(i):
            u = i % 2
            P.DMA("sp", x1t[u], x1_d[i * 128:(i + 1) * 128, :], [t_x1d], x1t_t[u])
            for (k, dst, dst_t) in ((0, ya[u], ya_t[u]), (1, yb2[u], yb2_t[u])):
                P.op("pool", ("indirect_dma_start", (), dict(out=dst, out_offset=None, in_=yb_d,
                                                             in_offset=bass.IndirectOffsetOnAxis(ap=dest_i[:, i, k:k + 1], axis=0))),
                     reads=[t_rt, t_ybd], writes=[dst_t], dma_out=dst_t)
            dv("scalar_tensor_tensor", out=x2[u], in0=ya[u], scalar=gall[:, i, 0:1], in1=x1t[u], op0=ALU.mult, op1=ALU.add,
               reads=[ya_t[u], x1t_t[u], t_rt], writes=[x2_t[u]])
            dv("scalar_tensor_tensor", out=x2[u], in0=yb2[u], scalar=gall[:, i, 1:2], in1=x2[u], op0=ALU.mult, op1=ALU.add,
               reads=[yb2_t[u], x2_t[u], t_rt], writes=[x2_t[u]])
            if not full:
                P.DMA("sp", dbg_x2[i * 128:(i + 1) * 128, :], x2[u], [x2_t[u]], t_dbg2, output=True, sem_tile=x2_t[u])
                return
            norm_rstd(x2[u], x2_t[u], h3[u], h3_t[u], ssc[u], ssc_t[u])
            dv("scalar_tensor_tensor", out=h3[u], in0=x2[u], scalar=ssc[u][:, 2:3], in1=gbc, op0=ALU.mult, op1=ALU.mult,
               reads=[x2_t[u], ssc_t[u], t_gbc], writes=[h3_t[u]])
            transpose16(h3[u], h3_t[u], h3T[u], h3T_t[u], 0, (0, 1))
            P.DMA("sp", ptl, pown[i * 128:(i + 1) * 128, :], [], t_ptl)
            act("activation", out=pb16, in_=ptl, func=AF.Copy, reads=[t_ptl], writes=[t_pb])
            for c in range(2):
                pe("transpose", psb(2)[:, c * 128:(c + 1) * 128], pb16[:, c * 128:(c + 1) * 128], c_identb,
                   reads=[t_pb, t_const], writes=[pst[2]])
            dv("tensor_copy", out=pT[u], in_=psb(2)[:, 0:256].rearrange("p (a b) -> p a b", a=2), reads=[pst[2]], writes=[pT_t[u]])

        def tail_back(i):
            u = i % 2
            for cg in range(4):
                bg = 4 + (itp[0] % 2) * 2
                sg_, sg_t_ = sgp[itp[0] % 2], sgp_t[itp[0] % 2]
                itp[0] += 1
                cs = slice(cg * 512, (cg + 1) * 512)
                for kc in range(16):
                    pe("matmul", psf(bg), lhsT=h3T[u][:, kc, :], rhs=Wpg[:, kc, cs], start=(kc == 0), stop=(kc == 15),
                       reads=[h3T_t[u], t_Wp], writes=[pst[bg]])
                for c in range(2):
                    pe("matmul", psf(bg + 1), lhsT=pT[u][:, c, :], rhs=Wpp[:, c, cs], start=(c == 0), stop=(c == 1),
                       reads=[pT_t[u], t_Wp], writes=[pst[bg + 1]])
                act("activation", out=sg_, in_=psf(bg), func=AF.Sigmoid, reads=[pst[bg]], writes=[sg_t_])
                dv("tensor_tensor", out=sg_, in0=sg_, in1=psf(bg + 1), op=ALU.mult, reads=[sg_t_, pst[bg + 1]], writes=[sg_t_])
                if cg % 2 == 0:
                    P.I("pool", "tensor_tensor", out=x3[u][:, cs], in0=sg_, in1=x2[u][:, cs], op=ALU.add, reads=[sg_t_, x2_t[u]], writes=[x3_t[u]])
                else:
                    dv("tensor_tensor", out=x3[u][:, cs], in0=sg_, in1=x2[u][:, cs], op=ALU.add, reads=[sg_t_, x2_t[u]], writes=[x3_t[u]])
            norm_rstd(x3[u], x3_t[u], h3[u], h3_t[u], ssd[u], ssd_t[u])
            dv("scalar_tensor_tensor", out=ot[u], in0=x3[u], scalar=ssd[u][:, 2:3], in1=gfin, op0=ALU.mult, op1=ALU.mult,
               reads=[x3_t[u], ssd_t[u], t_gfin], writes=[ot_t[u]])
            P.DMA("sp", y[i * 128:(i + 1) * 128, :], ot[u], [ot_t[u]], t_y, output=True, sem_tile=ot_t[u])

        tail_front(0)
        for i in range(NSLOT):
            if i + 1 < NSLOT:
                tail_front(i + 1)
            if full:
                tail_back(i)
        P.finish()
    return nc


def finish_debug(nc, P, dt, items, t_dep):
    for name, ap, shape, dtype in items:
        o = dt("dbg_" + name, shape, dtype, kind="ExternalOutput").ap()
        t = P.tile("dbg_" + name)
        P.DMA("sp", o, ap, list(t_dep), t, output=True)
    P.finish()
    return nc


def _core_blocks(i):
    blks = []
    for m in range(4):
        blks += [8 * m + i, 8 * m + 7 - i]
    return blks


def make_in_maps(inputs):
    x = np.asarray(inputs["x"], dtype=np.float32)
    p = np.asarray(inputs["p"], dtype=np.float32)[0]
    f = lambda k: np.ascontiguousarray(np.asarray(inputs[k], dtype=np.float32))
    shared = {
        "w_in": f("w_in")[0], "b_forget": f("b_forget").reshape(1, 8),
        "w_branch_fox": f("w_branch_fox")[0], "w_branch_sb": f("w_branch_sb")[0], "w_mix_out": f("w_mix_out")[0],
        "g_mix": f("g_mix").reshape(1, D), "g_ffn": f("g_ffn").reshape(1, D),
        "w_rout": np.ascontiguousarray(np.concatenate([f("w_group")[0], f("w_expert")[0]], axis=1)),
        "w_gate": f("w_gate")[0].reshape(NE * 512, 2048), "w_up": f("w_up")[0].reshape(NE * 512, 2048),
        "w_down": f("w_down")[0].reshape(NE * 512, 2048),
        "g_ple": f("g_ple").reshape(1, D), "w_ple_proj": f("w_ple_proj")[0], "w_ple_gate": f("w_ple_gate")[0],
        "g_final": f("g_final").reshape(1, D),
    }
    in_maps, rows = [], []
    for c in range(8):
        b, i = c // 4, c % 4
        blks = _core_blocks(i)
        idx = np.concatenate([np.arange(k * 128, (k + 1) * 128) for k in blks])
        rows.append((b, idx))
        m = dict(shared)
        m["xall"] = np.ascontiguousarray(x[b])
        m["xown"] = np.ascontiguousarray(x[b][idx])
        m["pown"] = np.ascontiguousarray(p[b][idx])
        m["blk128"] = np.ascontiguousarray(np.tile(np.asarray(blks, np.float32)[None, :] * 128.0, (128, 1)))
        in_maps.append(m)
    return in_maps, rows


_NC_CACHE = {}


def kernel(**inputs):
    in_maps, rows = make_in_maps(inputs)
    if "nc" not in _NC_CACHE:
        _NC_CACHE["nc"] = build_nc()
    nc = _NC_CACHE["nc"]
    names = set(a.memorylocations[0].name for a in nc.allocations
                if isinstance(a, mybir.MemoryLocationSet) and a.kind == "ExternalInput")
    in_maps = [{k: v for k, v in m.items() if k in names} for m in in_maps]
    res = run_bass_kernel_spmd(nc, in_maps, core_ids=list(range(8)))
    out = np.zeros((2, S, D), np.float32)
    for c in range(8):
        b, idx = rows[c]
        out[b, idx] = np.asarray(res.results[c]["y"])
    return out
```
